# Optimizing a Trainium2 kernel written in Bass

```python
import jax, jax.numpy as jnp
from jax import lax
import numpy as np

D_MODEL = 4096
BATCH = 4
SEQ = 2048
DEPTH = 2

HEAD_DIM = 128
ROT_DIM = HEAD_DIM // 4
ROPE_THETA = 500000.0
BLOCK = 128
NEG_INF = -1e30
EPS = 1e-6

A_HEADS = D_MODEL // (2 * HEAD_DIM)
A_WIDTH = A_HEADS * HEAD_DIM
DILATED_PAIRS = ((128, 1), (512, 4), (2048, 16))
B_HEADS = D_MODEL // (2 * HEAD_DIM)
Q_LORA = 1536
KV_LORA = 512
NOPE_DIM = 128
ROPE_DIM = 64
V_DIM = 128
QK_DIM = NOPE_DIM + ROPE_DIM
MIX_WIDTH = A_WIDTH + B_HEADS * V_DIM
IN_COLS = 3 * A_WIDTH + Q_LORA + KV_LORA + ROPE_DIM
C_HEADS = D_MODEL // HEAD_DIM
C_WIDTH = C_HEADS * HEAD_DIM
MEM_LEN = 256
X_HEADS = 4
X_DIM = X_HEADS * HEAD_DIM
D_FF = 14336
N_EXPERTS = 8
TOP_K = 2
D_FF_EXPERT = 4096

N_EVEN = (DEPTH + 1) // 2
N_ODD = DEPTH // 2

kernel_name = 'hybrid_dilated_mla_stickbreak_moe'


def rms_norm(x, g):
    xf = x.astype(jnp.float32)
    y = xf * lax.rsqrt(jnp.mean(xf * xf, axis=-1, keepdims=True) + EPS)
    return (y * g.astype(jnp.float32)).astype(x.dtype)


def rope_cos_sin(positions, dim):
    inv_freq = ROPE_THETA ** (-jnp.arange(0, dim, 2, dtype=jnp.float32) / dim)
    ang = positions.astype(jnp.float32)[..., None] * inv_freq
    return jnp.cos(ang)[:, :, None, :], jnp.sin(ang)[:, :, None, :]


def apply_rope(x, cos, sin):
    half = x.shape[-1] // 2
    x1 = x[..., :half].astype(jnp.float32)
    x2 = x[..., half:].astype(jnp.float32)
    return jnp.concatenate([x1 * cos - x2 * sin, x2 * cos + x1 * sin], axis=-1).astype(x.dtype)


def partial_rope(x, cos, sin):
    return jnp.concatenate([apply_rope(x[..., :ROT_DIM], cos, sin), x[..., ROT_DIM:]], axis=-1)


def band_attention(q, k, v, n_back):
    G, N, H, hd = q.shape
    nb = -(-N // BLOCK)
    pad = nb * BLOCK - N
    qp = jnp.pad(q, ((0, 0), (0, pad), (0, 0), (0, 0)))
    kp = jnp.pad(k, ((0, 0), (BLOCK, pad), (0, 0), (0, 0)))
    vp = jnp.pad(v, ((0, 0), (BLOCK, pad), (0, 0), (0, 0)))
    qb = qp.reshape(G, nb, BLOCK, H, hd)
    kb = kp.reshape(G, nb + 1, BLOCK, H, hd)
    vb = vp.reshape(G, nb + 1, BLOCK, H, hd)
    kw = jnp.concatenate([kb[:, :-1], kb[:, 1:]], axis=2)
    vw = jnp.concatenate([vb[:, :-1], vb[:, 1:]], axis=2)
    s = jnp.einsum('gnqhd,gnkhd->gnhqk', qb, kw).astype(jnp.float32) * (hd ** -0.5)
    qi = jnp.arange(BLOCK)[:, None]
    kj = jnp.arange(2 * BLOCK)[None, :]
    dist = qi + BLOCK - kj
    kpos = jnp.arange(nb)[:, None, None] * BLOCK - BLOCK + kj[None]
    valid = (dist >= 0) & (dist <= n_back) & (kpos >= 0)
    s = jnp.where(valid[None, :, None], s, NEG_INF)
    m = jnp.max(s, axis=-1, keepdims=True)
    lse = (m + jnp.log(jnp.sum(jnp.exp(s - m), axis=-1, keepdims=True)))[..., 0]
    p = jnp.exp(s - lse[..., None])
    o = jnp.einsum('gnhqk,gnkhd->gnqhd', p.astype(v.dtype), vw)
    o = o.reshape(G, nb * BLOCK, H, hd)[:, :N]
    lse = lse.transpose(0, 1, 3, 2).reshape(G, nb * BLOCK, H)[:, :N]
    return o, lse


def dilated_attention(q, k, v):
    B, S, H, hd = q.shape
    outs, lses = [], []
    for window, dil in DILATED_PAIRS:
        N = S // dil
        def stride(t):
            return t.reshape(B, N, dil, H, hd).transpose(0, 2, 1, 3, 4).reshape(B * dil, N, H, hd)
        o, lse = band_attention(stride(q), stride(k), stride(v), window // dil)
        outs.append(o.reshape(B, dil, N, H, hd).transpose(0, 2, 1, 3, 4).reshape(B, S, H, hd))
        lses.append(lse.reshape(B, dil, N, H).transpose(0, 2, 1, 3).reshape(B, S, H))
    w = jax.nn.softmax(jnp.stack(lses), axis=0)
    o = jnp.einsum('rbsh,rbshd->bshd', w, jnp.stack(outs).astype(jnp.float32))
    return o.astype(q.dtype)


def causal_block_attention(q, k, v, scale):
    B, S, H, dq = q.shape
    nb = S // BLOCK
    qb = q.reshape(B, nb, BLOCK, H, dq).swapaxes(0, 1)
    kpos = jnp.arange(S)

    def block(args):
        qblk, i = args
        s = jnp.einsum('bqhd,bkhd->bhqk', qblk, k).astype(jnp.float32) * scale
        qpos = i * BLOCK + jnp.arange(BLOCK)
        s = jnp.where(kpos[None, :] <= qpos[:, None], s, NEG_INF)
        p = jax.nn.softmax(s, axis=-1)
        return jnp.einsum('bhqk,bkhd->bqhd', p.astype(v.dtype), v)

    o = lax.map(block, (qb, jnp.arange(nb)))
    return o.swapaxes(0, 1).reshape(B, S, H, v.shape[-1])


def stick_breaking_attention(q, k, v):
    B, S, H, hd = q.shape
    nb = S // BLOCK
    scale = hd ** -0.5
    qb = q.reshape(B, nb, BLOCK, H, hd).swapaxes(0, 1)
    kpos = jnp.arange(S)

    def block(args):
        qblk, i = args
        z = jnp.einsum('bqhd,bkhd->bhqk', qblk, k).astype(jnp.float32) * scale
        qpos = i * BLOCK + jnp.arange(BLOCK)
        earlier = kpos[None, :] < qpos[:, None]
        log_beta = jax.nn.log_sigmoid(z)
        log_keep = jnp.where(earlier, jax.nn.log_sigmoid(-z), 0.0)
        later = lax.cumsum(log_keep, axis=3, reverse=True) - log_keep
        a = jnp.where(earlier, jnp.exp(log_beta + later), 0.0)
        return jnp.einsum('bhqk,bkhd->bqhd', a.astype(v.dtype), v)

    o = lax.map(block, (qb, jnp.arange(nb)))
    return o.swapaxes(0, 1).reshape(B, S, H, hd)


def even_mixer(h, cos_a, sin_a, cos_b, sin_b, w_in, ga_q, ga_k, g_cq, w_uq, g_ckv, w_ukv,
               gb_q, gb_kn, gb_kr, w_o):
    B, S, _ = h.shape
    proj = h @ w_in
    offs = [A_WIDTH, 2 * A_WIDTH, 3 * A_WIDTH, 3 * A_WIDTH + Q_LORA, 3 * A_WIDTH + Q_LORA + KV_LORA]
    qa, ka, va, cq, ckv, kr = jnp.split(proj, offs, axis=-1)
    qa = partial_rope(rms_norm(qa.reshape(B, S, A_HEADS, HEAD_DIM), ga_q), cos_a, sin_a)
    ka = partial_rope(rms_norm(ka.reshape(B, S, A_HEADS, HEAD_DIM), ga_k), cos_a, sin_a)
    va = va.reshape(B, S, A_HEADS, HEAD_DIM)
    o_a = dilated_attention(qa, ka, va).reshape(B, S, A_WIDTH)
    q = (rms_norm(cq, g_cq) @ w_uq).reshape(B, S, B_HEADS, QK_DIM)
    q = rms_norm(q, gb_q)
    q = jnp.concatenate([q[..., :NOPE_DIM], apply_rope(q[..., NOPE_DIM:], cos_b, sin_b)], axis=-1)
    kv = (rms_norm(ckv, g_ckv) @ w_ukv).reshape(B, S, B_HEADS, NOPE_DIM + V_DIM)
    k_nope = rms_norm(kv[..., :NOPE_DIM], gb_kn)
    vb = kv[..., NOPE_DIM:]
    k_rope = apply_rope(rms_norm(kr.reshape(B, S, 1, ROPE_DIM), gb_kr), cos_b, sin_b)
    kb = jnp.concatenate([k_nope, jnp.broadcast_to(k_rope, (B, S, B_HEADS, ROPE_DIM))], axis=-1)
    o_b = causal_block_attention(q, kb, vb, QK_DIM ** -0.5).reshape(B, S, B_HEADS * V_DIM)
    return jnp.concatenate([o_a, o_b], axis=-1) @ w_o


def odd_mixer(h, w_qkv, w_o):
    B, S, _ = h.shape
    qkv = (h @ w_qkv).reshape(B, S, 3, C_HEADS, HEAD_DIM)
    o = stick_breaking_attention(qkv[:, :, 0], qkv[:, :, 1], qkv[:, :, 2])
    return o.reshape(B, S, C_WIDTH) @ w_o


def memory_cross_attention(h, mem_k, mem_v, w_xq, g_xq, w_xo):
    B, S, _ = h.shape
    q = rms_norm((h @ w_xq).reshape(B, S, X_HEADS, HEAD_DIM), g_xq)
    s = jnp.einsum('bshd,bmhd->bhsm', q, mem_k).astype(jnp.float32) * (HEAD_DIM ** -0.5)
    p = jax.nn.softmax(s, axis=-1)
    o = jnp.einsum('bhsm,bmhd->bshd', p.astype(mem_v.dtype), mem_v).reshape(B, S, X_DIM)
    return o @ w_xo


def swiglu(t, w_gate, w_up, w_down):
    return (jax.nn.silu(t @ w_gate) * (t @ w_up)) @ w_down


def moe_swiglu(h, w_router, b_router, w_egate, w_eup, w_edown):
    B, S, D = h.shape
    t = h.reshape(B * S, D)
    logits = (t @ w_router).astype(jnp.float32) + b_router.astype(jnp.float32)
    top_logit, top_idx = lax.top_k(logits, TOP_K)
    top_w = jax.nn.softmax(top_logit, axis=-1)
    gates = jnp.einsum('tk,tke->te', top_w, jax.nn.one_hot(top_idx, N_EXPERTS, dtype=jnp.float32))
    y = jnp.zeros_like(t)
    for e in range(N_EXPERTS):
        y = y + gates[:, e:e + 1].astype(t.dtype) * swiglu(t, w_egate[e], w_eup[e], w_edown[e])
    return y.reshape(B, S, D)


def setup_inputs(seed: int = 0) -> dict:
    key = jax.random.key(seed)
    ks = jax.random.split(key, 40)
    f32 = jnp.float32

    def w(i, shape, fan_in):
        return jax.random.normal(ks[i], shape, f32) * (fan_in ** -0.5)

    def g(i, shape):
        return 1.0 + 0.02 * jax.random.normal(ks[i], shape, f32)

    E, O = N_EVEN, N_ODD
    positions = (jax.random.randint(ks[2], (BATCH, 1), 0, 4096, dtype=jnp.int32)
                 + jnp.arange(SEQ, dtype=jnp.int32)[None, :])
    return {
        'x': jax.random.normal(ks[0], (BATCH, SEQ, D_MODEL), f32),
        'mem': jax.random.normal(ks[1], (BATCH, MEM_LEN, D_MODEL), f32),
        'positions': positions,
        'g_mem': g(3, (D_MODEL,)),
        'w_mem_kv': w(4, (D_MODEL, 2 * X_DIM), D_MODEL),
        'g_mem_k': g(5, (HEAD_DIM,)),
        'g_mix': g(6, (DEPTH, D_MODEL)),
        'g_x': g(7, (DEPTH, D_MODEL)),
        'w_xq': w(8, (DEPTH, D_MODEL, X_DIM), D_MODEL),
        'g_xq': g(9, (DEPTH, HEAD_DIM)),
        'w_xo': w(10, (DEPTH, X_DIM, D_MODEL), X_DIM),
        'g_ffn': g(11, (DEPTH, D_MODEL)),
        'w_in': w(12, (E, D_MODEL, IN_COLS), D_MODEL),
        'ga_q': g(13, (E, HEAD_DIM)),
        'ga_k': g(14, (E, HEAD_DIM)),
        'g_cq': g(15, (E, Q_LORA)),
        'w_uq': w(16, (E, Q_LORA, B_HEADS * QK_DIM), Q_LORA),
        'g_ckv': g(17, (E, KV_LORA)),
        'w_ukv': w(18, (E, KV_LORA, B_HEADS * (NOPE_DIM + V_DIM)), KV_LORA),
        'gb_q': g(19, (E, QK_DIM)),
        'gb_kn': g(20, (E, NOPE_DIM)),
        'gb_kr': g(21, (E, ROPE_DIM)),
        'w_o_even': w(22, (E, MIX_WIDTH, D_MODEL), MIX_WIDTH),
        'w_gate': w(23, (E, D_MODEL, D_FF), D_MODEL),
        'w_up': w(24, (E, D_MODEL, D_FF), D_MODEL),
        'w_down': w(25, (E, D_FF, D_MODEL), D_FF),
        'w_qkv': w(26, (O, D_MODEL, 3 * C_WIDTH), D_MODEL),
        'w_o_odd': w(27, (O, C_WIDTH, D_MODEL), C_WIDTH),
        'w_router': w(28, (O, D_MODEL, N_EXPERTS), D_MODEL),
        'b_router': 0.01 * jax.random.normal(ks[29], (O, N_EXPERTS), f32),
        'w_egate': w(30, (O, N_EXPERTS, D_MODEL, D_FF_EXPERT), D_MODEL),
        'w_eup': w(31, (O, N_EXPERTS, D_MODEL, D_FF_EXPERT), D_MODEL),
        'w_edown': w(32, (O, N_EXPERTS, D_FF_EXPERT, D_MODEL), D_FF_EXPERT),
    }


def reference(x, mem, positions, g_mem, w_mem_kv, g_mem_k, g_mix, g_x, w_xq, g_xq, w_xo, g_ffn,
              w_in, ga_q, ga_k, g_cq, w_uq, g_ckv, w_ukv, gb_q, gb_kn, gb_kr, w_o_even,
              w_gate, w_up, w_down, w_qkv, w_o_odd, w_router, b_router, w_egate, w_eup, w_edown):
    B, M = mem.shape[0], mem.shape[1]
    cos_a, sin_a = rope_cos_sin(positions, ROT_DIM)
    cos_b, sin_b = rope_cos_sin(positions, ROPE_DIM)
    mk, mv = jnp.split(rms_norm(mem, g_mem) @ w_mem_kv, 2, axis=-1)
    mem_k = rms_norm(mk.reshape(B, M, X_HEADS, HEAD_DIM), g_mem_k)
    mem_v = mv.reshape(B, M, X_HEADS, HEAD_DIM)
    for layer in range(DEPTH):
        i = layer // 2
        h = rms_norm(x, g_mix[layer])
        if layer % 2 == 0:
            x = x + even_mixer(h, cos_a, sin_a, cos_b, sin_b, w_in[i], ga_q[i], ga_k[i], g_cq[i],
                               w_uq[i], g_ckv[i], w_ukv[i], gb_q[i], gb_kn[i], gb_kr[i], w_o_even[i])
        else:
            x = x + odd_mixer(h, w_qkv[i], w_o_odd[i])
        h = rms_norm(x, g_x[layer])
        x = x + memory_cross_attention(h, mem_k, mem_v, w_xq[layer], g_xq[layer], w_xo[layer])
        h = rms_norm(x, g_ffn[layer])
        if layer % 2 == 0:
            x = x + swiglu(h, w_gate[i], w_up[i], w_down[i])
        else:
            x = x + moe_swiglu(h, w_router[i], b_router[i], w_egate[i], w_eup[i], w_edown[i])
    return x
```

```python
import contextlib
import math
import numpy as np
import ml_dtypes
import concourse.bass as bass
import concourse.mybir as mybir
from concourse.bass_utils import run_bass_kernel_spmd

F32 = mybir.dt.float32
BF16 = mybir.dt.bfloat16
I32 = mybir.dt.int32
AF = mybir.ActivationFunctionType
ALU = mybir.AluOpType
AX = mybir.AxisListType
NPBF = ml_dtypes.bfloat16

EPS = 1e-6
NCORES = 8
D = 4096
KT = 32
SEQ = 2048
TOKC = 1024
TOK = 256
DMA_ROT = 8


class Buf:
    __slots__ = ("ap", "name", "last_w", "readers")

    def __init__(self, ap, name=""):
        self.ap = ap
        self.name = name
        self.last_w = None
        self.readers = []


class Op:
    __slots__ = ("eng", "fn", "deps", "signal", "val", "dma_sem", "dma_val", "is_dma")

    def __init__(self, eng, fn):
        self.eng = eng
        self.fn = fn
        self.deps = []
        self.signal = False
        self.val = None
        self.is_dma = False
        self.dma_sem = None
        self.dma_val = None


class Prog:
    ENGS = ("pe", "act", "dve", "pool", "sp")

    def __init__(self, nc):
        self.nc = nc
        self.ops = {e: [] for e in self.ENGS}
        self.dma_count = {e: 0 for e in self.ENGS}
        self.final_dma = []
        self.fence_ops = []
        self.fenced = set()

    def fence(self):
        f = []
        for e in self.ENGS:
            comp = [o for o in self.ops[e] if not o.is_dma]
            if comp:
                f.append(comp[-1])
            f.extend([o for o in self.ops[e] if o.is_dma][-DMA_ROT:])
        self.fence_ops = f
        self.fenced = set()

    def _add(self, eng, fn, reads, writes, is_dma=False):
        op = Op(eng, fn)
        op.is_dma = is_dma
        deps = []
        if self.fence_ops and eng not in self.fenced:
            self.fenced.add(eng)
            deps.extend(self.fence_ops)
        for b in reads:
            if b.last_w is not None:
                deps.append(b.last_w)
        for b in writes:
            if b.last_w is not None:
                deps.append(b.last_w)
            last = {}
            for r in b.readers:
                if r.is_dma:
                    deps.append(r)
                else:
                    last[r.eng] = r
            deps.extend(last.values())
        if is_dma:
            k = self.dma_count[eng]
            self.dma_count[eng] += 1
            op.dma_sem = (eng, k % DMA_ROT)
            op.dma_val = 16 * (k // DMA_ROT + 1)
        seen = set()
        for d in deps:
            if id(d) in seen or d is op:
                continue
            seen.add(id(d))
            op.deps.append(d)
        for b in writes:
            b.last_w = op
            b.readers = []
        for b in reads:
            if b.last_w is op:
                continue
            b.readers.append(op)
        self.ops[eng].append(op)
        return op

    def op(self, eng, fn, reads=(), writes=()):
        return self._add(eng, fn, list(reads), list(writes))

    def dma(self, eng, out_ap, in_ap, reads=(), writes=(), final=False):
        def fn(e, out_ap=out_ap, in_ap=in_ap):
            return e.dma_start(out=out_ap, in_=in_ap)
        op = self._add(eng, fn, list(reads), list(writes), is_dma=True)
        if final:
            self.final_dma.append(op)
        return op

    def emit(self):
        nc = self.nc
        es = contextlib.ExitStack()
        with es:
            eng_sem = {e: es.enter_context(nc.semaphore(f"es_{e}")) for e in self.ENGS}
            dma_sems = {}
            for e in self.ENGS:
                for r in range(min(DMA_ROT, self.dma_count[e])):
                    dma_sems[(e, r)] = es.enter_context(nc.semaphore(f"ds_{e}{r}"))
            for e in self.ENGS:
                for op in self.ops[e]:
                    for d in op.deps:
                        if d.is_dma:
                            continue
                        if d.eng == op.eng and not op.is_dma and d.eng == "pe":
                            continue
                        d.signal = True
            for e in self.ENGS:
                v = 0
                for op in self.ops[e]:
                    if op.is_dma:
                        continue
                    if op.signal:
                        v += 1
                        op.val = v
            prog = self

            def run_engine(ename, e):
                seen = {}
                for op in prog.ops[ename]:
                    waits = {}
                    for d in op.deps:
                        if d.is_dma:
                            key = ("d",) + d.dma_sem
                            val = d.dma_val
                        else:
                            if d.eng == ename and not op.is_dma and ename == "pe":
                                continue
                            key = ("e", d.eng)
                            val = d.val
                        if waits.get(key, 0) < val:
                            waits[key] = val
                    if op.is_dma and op.dma_val > 16:
                        key = ("d",) + op.dma_sem
                        pv = op.dma_val - 16
                        if waits.get(key, 0) < pv:
                            waits[key] = pv
                    for key, val in waits.items():
                        if seen.get(key, 0) >= val:
                            continue
                        seen[key] = val
                        sem = eng_sem[key[1]] if key[0] == "e" else dma_sems[(key[1], key[2])]
                        e.wait_ge(sem, val)
                    ins = op.fn(e)
                    if op.is_dma:
                        ins.then_inc(dma_sems[op.dma_sem], 16)
                    elif op.signal:
                        ins.then_inc(eng_sem[ename], 1)
                for op in prog.final_dma:
                    if op.eng == ename:
                        key = ("d",) + op.dma_sem
                        if seen.get(key, 0) < op.dma_val:
                            seen[key] = op.dma_val
                            e.wait_ge(dma_sems[op.dma_sem], op.dma_val)

            with nc.Block() as block:
                @block.tensor
                def _(e):
                    run_engine("pe", e)

                @block.scalar
                def _(e):
                    run_engine("act", e)

                @block.vector
                def _(e):
                    run_engine("dve", e)

                @block.gpsimd
                def _(e):
                    run_engine("pool", e)

                @block.sync
                def _(e):
                    run_engine("sp", e)
        return nc


class Rot:
    def __init__(self, bufs):
        self.bufs = bufs
        self.i = 0

    def next(self):
        b = self.bufs[self.i % len(self.bufs)]
        self.i += 1
        return b


class Ctx:
    def __init__(self, tok):
        self.nc = bass.Bass("TRN2", target_bir_lowering=False)
        self.es = contextlib.ExitStack()
        self.P = Prog(self.nc)
        self.tok = tok
        self.n = 0

    def dram_in(self, name, shape, dt):
        return self.nc.dram_tensor(name, list(shape), dt, kind="ExternalInput").ap()

    def dram_out(self, name, shape, dt):
        return self.nc.dram_tensor(name, list(shape), dt, kind="ExternalOutput").ap()

    def sb(self, name, shape, dt):
        t = self.es.enter_context(self.nc.sbuf_tensor(f"s{self.n}_" + name, list(shape), dt))
        return Buf(t[:], name)

    def sbn(self, name, n, shape, dt):
        t = self.es.enter_context(self.nc.sbuf_tensor(f"s{self.n}_" + name, [shape[0], n] + list(shape[1:]), dt))
        return [Buf(t[:, i], f"{name}{i}") for i in range(n)], Buf(t[:], name)

    def ps(self, name, shape=(128, 512), dt=F32):
        t = self.es.enter_context(self.nc.psum_tensor(f"p{self.n}_" + name, list(shape), dt))
        return Buf(t[:], name)

    def setup_common(self):
        P = self.P
        tok = self.tok
        self.ones = self.sb("ones", [128, 128], F32)
        P.op("pool", lambda e: e.memset(self.ones.ap[:], 1.0), writes=[self.ones])
        self.onesb = self.sb("onesb", [128, 128], BF16)
        P.op("pool", lambda e: e.memset(self.onesb.ap[:], 1.0), writes=[self.onesb])
        self.wrot = Rot([self.sb(f"wb{i}", [128, KT, 256], BF16) for i in range(3)])
        self.psrot = Rot([self.ps(f"pm{i}") for i in range(4)])
        self.ps_stat = Rot([self.ps(f"pst{i}") for i in range(2)])
        self.ps_misc = Rot([self.ps(f"pmi{i}") for i in range(2)])
        self.tmp = Rot([self.sb(f"tmp{i}", [128, tok], F32) for i in range(6)])
        self.sqt = Rot([self.sb(f"sq{i}", [128, tok], F32) for i in range(3)])
        self.outb = Rot([self.sb(f"ob{i}", [128, tok], BF16) for i in range(4)])
        self.rstd = self.sb("rstd", [128, tok], F32)


def inv_rms(C, ss_ps, dim, out):
    tok = C.tok
    C.P.op("dve", lambda e: e.tensor_scalar(out=out.ap[:, :tok], in0=ss_ps.ap[:, :tok], scalar1=1.0 / dim,
                                            scalar2=EPS, op0=ALU.mult, op1=ALU.add),
           reads=[ss_ps], writes=[out])
    C.P.op("act", lambda e: e.activation(out=out.ap[:, :tok], in_=out.ap[:, :tok], func=AF.Sqrt),
           reads=[out], writes=[out])
    C.P.op("dve", lambda e: e.reciprocal(out=out.ap[:, :tok], in_=out.ap[:, :tok]),
           reads=[out], writes=[out])


def norm_prep(C, src, g, hT, rstd, dim=None, router=None):
    P = C.P
    tok = C.tok
    n = len(src)
    dim = dim or 128 * n
    ss = C.ps_stat.next()
    for k in range(n):
        sq = C.sqt.next()
        P.op("act", lambda e, sq=sq, k=k: e.activation(out=sq.ap[:, :tok], in_=src[k].ap[:, :tok], func=AF.Square),
             reads=[src[k]], writes=[sq])
        P.op("pe", lambda e, sq=sq, k=k: e.matmul(ss.ap[:, :tok], lhsT=C.ones.ap[:], rhs=sq.ap[:, :tok],
                                                   start=(k == 0), stop=(k == n - 1)),
             reads=[sq, C.ones], writes=[ss])
        P.op("dve", lambda e, k=k: e.tensor_scalar(out=hT[k].ap[:, :tok], in0=src[k].ap[:, :tok],
                                                   scalar1=g.ap[:, k:k + 1], scalar2=None, op0=ALU.mult),
             reads=[src[k], g], writes=[hT[k]])
        if router is not None:
            w_r, lg = router
            xg = C.tmp.next()
            P.op("pool", lambda e, k=k, xg=xg: e.tensor_scalar(out=xg.ap[:, :tok], in0=src[k].ap[:, :tok],
                                                               scalar1=g.ap[:, k:k + 1], scalar2=None, op0=ALU.mult),
                 reads=[src[k], g], writes=[xg])
            P.op("pe", lambda e, k=k, xg=xg: e.matmul(lg.ap[0:8, :tok], lhsT=w_r.ap[:, k, :], rhs=xg.ap[:, :tok],
                                                      start=(k == 0), stop=(k == n - 1)),
                 reads=[xg, w_r], writes=[lg])
    inv_rms(C, ss, dim, rstd)


def linear(C, kt, Wd, n_tiles, epi, col0=0):
    P = C.P
    tok = C.tok
    nk = len(kt)
    j = 0
    while j < n_tiles:
        w = min(2, n_tiles - j)
        wb = C.wrot.next()
        P.dma("pool", wb.ap[:, 0:nk, 0:w * 128],
              Wd[0:nk * 128, col0 + j * 128: col0 + (j + w) * 128].rearrange("(kc p) n -> p kc n", p=128),
              writes=[wb])
        for jj in range(w):
            ps = C.psrot.next()
            for k in range(nk):
                P.op("pe", lambda e, wb=wb, jj=jj, k=k, ps=ps: e.matmul(
                    ps.ap[:, :tok], lhsT=wb.ap[:, k, jj * 128:(jj + 1) * 128], rhs=kt[k].ap[:, :tok],
                    start=(k == 0), stop=(k == nk - 1)), reads=[wb, kt[k]], writes=[ps])
            epi(j + jj, ps)
        j += w


def store_tile(C, out_dram, row0, t0, src, final=False):
    tok = C.tok
    C.P.dma("act", out_dram[row0:row0 + 128, t0:t0 + tok], src.ap[:, :tok], reads=[src], final=final)


def epi_v_store(C, cs, vdram, t0, head_of, identb):
    P = C.P
    tok = C.tok

    def epi(j, ps):
        ob = C.outb.next()
        P.op("dve", lambda e: e.tensor_tensor(out=ob.ap[:, :tok], in0=ps.ap[:, :tok], in1=cs.ap[:, :tok], op=ALU.mult),
             reads=[ps, cs], writes=[ob])
        for sub in range(tok // 128):
            pt = C.ps_misc.next()
            P.op("pe", lambda e, pt=pt, sub=sub: e.matmul(pt.ap[:, 0:128], lhsT=ob.ap[:, sub * 128:(sub + 1) * 128],
                                                          rhs=identb.ap[:], start=True, stop=True),
                 reads=[ob, identb], writes=[pt])
            vt = C.vtrot.next()
            P.op("act", lambda e, pt=pt, vt=vt: e.activation(out=vt.ap[:], in_=pt.ap[:, 0:128], func=AF.Copy),
                 reads=[pt], writes=[vt])
            P.dma("act", vdram[t0 + sub * 128:t0 + (sub + 1) * 128, head_of(j), :], vt.ap[:], reads=[vt])
    return epi


def setup_vt(C, ident_d):
    C.identb = load_const(C, "identb", ident_d, [128, 128], BF16)
    C.vtrot = Rot([C.sb(f"vt{i}", [128, 128], BF16) for i in range(3)])


def epi_raw_store(C, cs, out_dram, t0, row_of):
    P = C.P
    tok = C.tok

    def epi(j, ps):
        ob = C.outb.next()
        P.op("dve", lambda e: e.tensor_tensor(out=ob.ap[:, :tok], in0=ps.ap[:, :tok], in1=cs.ap[:, :tok], op=ALU.mult),
             reads=[ps, cs], writes=[ob])
        store_tile(C, out_dram, row_of(j), t0, ob)
    return epi


def head_norm(C, parts, cs, dim, gcols, outs, rope=None, gdeps=()):
    P = C.P
    tok = C.tok
    ys = []
    ss = C.ps_stat.next()
    for i, ps in enumerate(parts):
        y = C.tmp.next()
        P.op("dve", lambda e, y=y, ps=ps: e.tensor_tensor(out=y.ap[:, :tok], in0=ps.ap[:, :tok], in1=cs.ap[:, :tok],
                                                          op=ALU.mult), reads=[ps, cs], writes=[y])
        sq = C.sqt.next()
        P.op("act", lambda e, y=y, sq=sq: e.activation(out=sq.ap[:, :tok], in_=y.ap[:, :tok], func=AF.Square),
             reads=[y], writes=[sq])
        P.op("pe", lambda e, sq=sq, i=i: e.matmul(ss.ap[:, :tok], lhsT=C.ones.ap[:], rhs=sq.ap[:, :tok],
                                                   start=(i == 0), stop=(i == len(parts) - 1)),
             reads=[sq, C.ones], writes=[ss])
        ys.append(y)
    r = C.tmp.next()
    inv_rms(C, ss, dim, r)
    for i, y in enumerate(ys):
        rp = rope[i] if rope else None
        if rp is None:
            P.op("dve", lambda e, y=y, i=i: e.scalar_tensor_tensor(
                out=outs[i].ap[:, :tok], in0=y.ap[:, :tok], scalar=gcols[i], in1=r.ap[:, :tok],
                op0=ALU.mult, op1=ALU.mult), reads=[y, r] + list(gdeps), writes=[outs[i]])
        else:
            rotT, cos, sin, t0 = rp
            P.op("dve", lambda e, y=y, i=i: e.scalar_tensor_tensor(
                out=y.ap[:, :tok], in0=y.ap[:, :tok], scalar=gcols[i], in1=r.ap[:, :tok],
                op0=ALU.mult, op1=ALU.mult), reads=[y, r] + list(gdeps), writes=[y])
            pr = C.ps_misc.next()
            P.op("pe", lambda e, y=y, pr=pr: e.matmul(pr.ap[:, :tok], lhsT=rotT.ap[:], rhs=y.ap[:, :tok],
                                                      start=True, stop=True), reads=[y, rotT], writes=[pr])
            t1 = C.tmp.next()
            P.op("pool", lambda e, y=y, t1=t1: e.tensor_tensor(out=t1.ap[:, :tok], in0=y.ap[:, :tok],
                                                               in1=cos.ap[:, t0:t0 + tok], op=ALU.mult),
                 reads=[y, cos], writes=[t1])
            t2 = C.tmp.next()
            P.op("dve", lambda e, pr=pr, t2=t2: e.tensor_tensor(out=t2.ap[:, :tok], in0=pr.ap[:, :tok],
                                                                in1=sin.ap[:, t0:t0 + tok], op=ALU.mult),
                 reads=[pr, sin], writes=[t2])
            P.op("pool", lambda e, t1=t1, t2=t2, i=i: e.tensor_tensor(out=outs[i].ap[:, :tok], in0=t1.ap[:, :tok],
                                                                      in1=t2.ap[:, :tok], op=ALU.add),
                 reads=[t1, t2], writes=[outs[i]])


def rope_tables(C, pos_d, invf, ntok, name, gdeps=()):
    P = C.P
    posi = C.sb(name + "_pi", [128, ntok], I32)
    P.dma("sp", posi.ap[:], pos_d, writes=[posi])
    ang = C.sb(name + "_ang", [128, ntok], F32)
    P.op("dve", lambda e: e.tensor_copy(out=ang.ap[:], in_=posi.ap[:]), reads=[posi], writes=[ang])
    P.op("dve", lambda e: e.tensor_scalar(out=ang.ap[:], in0=ang.ap[:], scalar1=invf, scalar2=None, op0=ALU.mult),
         reads=[ang] + list(gdeps), writes=[ang])
    cos = C.sb(name + "_cos", [128, ntok], F32)
    sin = C.sb(name + "_sin", [128, ntok], F32)
    two_pi = 2.0 * math.pi
    sc = 1.0 - 1e-6
    ti = C.sb(name + "_ti", [128, ntok], I32)
    tf = C.sb(name + "_tf", [128, ntok], F32)
    mk = C.sb(name + "_mk", [128, ntok], F32)
    C1 = 6.28125
    C2 = 2.0 * math.pi - C1

    def reduced_sin(dst, shift):
        P.op("dve", lambda e: e.tensor_scalar(out=dst.ap[:], in0=ang.ap[:], scalar1=shift, scalar2=None, op0=ALU.add),
             reads=[ang], writes=[dst])
        P.op("dve", lambda e: e.tensor_scalar(out=tf.ap[:], in0=dst.ap[:], scalar1=1.0 / two_pi, scalar2=None,
                                              op0=ALU.mult), reads=[dst], writes=[tf])
        P.op("dve", lambda e: e.tensor_copy(out=ti.ap[:], in_=tf.ap[:]), reads=[tf], writes=[ti])
        P.op("dve", lambda e: e.tensor_copy(out=tf.ap[:], in_=ti.ap[:]), reads=[ti], writes=[tf])
        P.op("dve", lambda e: e.scalar_tensor_tensor(out=dst.ap[:], in0=tf.ap[:], scalar=-C1, in1=dst.ap[:],
                                                     op0=ALU.mult, op1=ALU.add), reads=[tf, dst], writes=[dst])
        P.op("dve", lambda e: e.scalar_tensor_tensor(out=dst.ap[:], in0=tf.ap[:], scalar=-C2, in1=dst.ap[:],
                                                     op0=ALU.mult, op1=ALU.add), reads=[tf, dst], writes=[dst])
        P.op("dve", lambda e: e.tensor_scalar(out=mk.ap[:], in0=dst.ap[:], scalar1=math.pi, scalar2=None,
                                              op0=ALU.is_gt), reads=[dst], writes=[mk])
        P.op("dve", lambda e: e.scalar_tensor_tensor(out=dst.ap[:], in0=mk.ap[:], scalar=-two_pi, in1=dst.ap[:],
                                                     op0=ALU.mult, op1=ALU.add), reads=[mk, dst], writes=[dst])
        P.op("dve", lambda e: e.tensor_scalar(out=mk.ap[:], in0=dst.ap[:], scalar1=-math.pi, scalar2=None,
                                              op0=ALU.is_lt), reads=[dst], writes=[mk])
        P.op("dve", lambda e: e.scalar_tensor_tensor(out=dst.ap[:], in0=mk.ap[:], scalar=two_pi, in1=dst.ap[:],
                                                     op0=ALU.mult, op1=ALU.add), reads=[mk, dst], writes=[dst])
        P.op("act", lambda e: e.activation(out=dst.ap[:], in_=dst.ap[:], func=AF.Sin, scale=sc),
             reads=[dst], writes=[dst])
    reduced_sin(sin, 0.0)
    reduced_sin(cos, 0.5 * math.pi)
    return cos, sin


def load_const(C, name, dram_ap, shape, dt, eng="sp"):
    b = C.sb(name, shape, dt)
    C.P.dma(eng, b.ap[:], dram_ap, writes=[b])
    return b


def build_P():
    C = Ctx(128)
    P = C.P
    memT = C.dram_in("memT", [D, 128], F32)
    g_mem = C.dram_in("g_mem", [128, KT], F32)
    g_mk = C.dram_in("g_mk", [128, 1], F32)
    w = C.dram_in("w_mem_kv", [D, 1024], F32)
    out = C.dram_out("mkvT", [1024, 128], BF16)
    with C.es:
        C.setup_common()
        tok = C.tok
        gm = load_const(C, "gm", g_mem, [128, KT], F32)
        gk = load_const(C, "gk", g_mk, [128, 1], F32)
        acc, acc_all = C.sbn("acc", KT, [128, tok], F32)
        hT, _ = C.sbn("hT", KT, [128, tok], BF16)
        P.dma("sp", acc_all.ap[:], memT.rearrange("(kc p) t -> p kc t", p=128), writes=acc + [acc_all])
        norm_prep(C, acc, gm, hT, C.rstd)

        def epi(j, ps):
            ob = C.outb.next()
            if j < 4:
                head_norm(C, [ps], C.rstd, 128, [gk.ap[:, 0:1]], [ob], gdeps=[gk])
            else:
                P.op("dve", lambda e: e.tensor_tensor(out=ob.ap[:, :tok], in0=ps.ap[:, :tok], in1=C.rstd.ap[:, :tok],
                                                      op=ALU.mult), reads=[ps, C.rstd], writes=[ob])
            store_tile(C, out, j * 128, 0, ob, final=True)
        linear(C, hT, w, 8, epi)
        P.emit()
    return C.nc


def build_A0():
    C = Ctx(TOK)
    P = C.P
    T = TOKC
    xT = C.dram_in("xT", [D, T], F32)
    posb = C.dram_in("posb", [128, T], I32)
    w_in = C.dram_in("w_in", [D, 8320], F32)
    w_uq = C.dram_in("w_uq", [1536, 4096], F32)
    w_ukv = C.dram_in("w_ukv", [512, 4096], F32)
    gmix = C.dram_in("g_mix", [128, KT], F32)
    gcq = C.dram_in("g_cq", [128, 12], F32)
    gckv = C.dram_in("g_ckv", [128, 4], F32)
    vecs = C.dram_in("vecs", [128, 8], F32)
    rots = C.dram_in("rots", [128, 2, 128], F32)
    qaT = C.dram_out("qaT", [2048, T], BF16)
    kaT = C.dram_out("kaT", [2048, T], BF16)
    vaT = C.dram_out("vaT", [2048, T], BF16)
    qbT = C.dram_out("qbT", [4096, T], BF16)
    kbnT = C.dram_out("kbnT", [2048, T], BF16)
    vbT = C.dram_out("vbT", [2048, T], BF16)
    krT = C.dram_out("krT", [128, T], BF16)
    with C.es:
        C.setup_common()
        tok = C.tok
        gm = load_const(C, "gm", gmix, [128, KT], F32)
        gq = load_const(C, "gcq", gcq, [128, 12], F32)
        gkv = load_const(C, "gckv", gckv, [128, 4], F32)
        vc = load_const(C, "vecs", vecs, [128, 8], F32)
        rt = load_const(C, "rots", rots, [128, 2, 128], F32)
        rotA = Buf(rt.ap[:, 0, :], "rotA"); rotB = Buf(rt.ap[:, 1, :], "rotB")
        rotA.last_w = rt.last_w; rotB.last_w = rt.last_w
        cosA, sinA = rope_tables(C, posb, vc.ap[:, 6:7], T, "ra", gdeps=[vc])
        cosB, sinB = rope_tables(C, posb, vc.ap[:, 7:8], T, "rb", gdeps=[vc])
        acc, acc_all = C.sbn("acc", KT, [128, tok], F32)
        hT, _ = C.sbn("hT", KT, [128, tok], BF16)
        cq, _ = C.sbn("cq", 12, [128, tok], F32)
        cqb, _ = C.sbn("cqb", 12, [128, tok], BF16)
        ckv, _ = C.sbn("ckv", 4, [128, tok], F32)
        ckvb, _ = C.sbn("ckvb", 4, [128, tok], BF16)
        r2 = C.sb("r2", [128, tok], F32)
        r3 = C.sb("r3", [128, tok], F32)
        for ch in range(T // tok):
            t0 = ch * tok
            P.dma("sp", acc_all.ap[:], xT[:, t0:t0 + tok].rearrange("(kc p) t -> p kc t", p=128),
                  writes=acc + [acc_all])
            norm_prep(C, acc, gm, hT, C.rstd)

            def epi(j, ps, t0=t0):
                if j < 32:
                    ob = C.outb.next()
                    gc = vc.ap[:, 0:1] if j < 16 else vc.ap[:, 1:2]
                    head_norm(C, [ps], C.rstd, 128, [gc], [ob], rope=[(rotA, cosA, sinA, t0)], gdeps=[vc])
                    store_tile(C, qaT if j < 16 else kaT, (j % 16) * 128, t0, ob, final=True)
                elif j < 48:
                    epi_raw_store(C, C.rstd, vaT, t0, lambda jj: (jj - 32) * 128)(j, ps)
                elif j < 64:
                    dst = cq[j - 48] if j < 60 else ckv[j - 60]
                    P.op("dve", lambda e: e.tensor_tensor(out=dst.ap[:, :tok], in0=ps.ap[:, :tok],
                                                          in1=C.rstd.ap[:, :tok], op=ALU.mult),
                         reads=[ps, C.rstd], writes=[dst])
                else:
                    ob = C.outb.next()
                    head_norm(C, [ps], C.rstd, 64, [vc.ap[:, 5:6]], [ob], rope=[(rotB, cosB, sinB, t0)], gdeps=[vc])
                    store_tile(C, krT, 0, t0, ob, final=True)
            linear(C, hT, w_in, 65, epi)
            norm_prep(C, cq, gq, cqb, r2, dim=1536)
            pend = {}

            def epi_q(j, ps, t0=t0):
                h, part = divmod(j, 2)
                if part == 0:
                    pend["nope"] = ps
                    return
                o1 = C.outb.next(); o2 = C.outb.next()
                head_norm(C, [pend["nope"], ps], r2, 192, [vc.ap[:, 2:3], vc.ap[:, 3:4]], [o1, o2],
                          rope=[None, (rotB, cosB, sinB, t0)], gdeps=[vc])
                store_tile(C, qbT, (2 * h) * 128, t0, o1, final=True)
                store_tile(C, qbT, (2 * h + 1) * 128, t0, o2, final=True)
            linear(C, cqb, w_uq, 32, epi_q)
            norm_prep(C, ckv, gkv, ckvb, r3, dim=512)

            def epi_kv(j, ps, t0=t0):
                h, part = divmod(j, 2)
                if part == 0:
                    ob = C.outb.next()
                    head_norm(C, [ps], r3, 128, [vc.ap[:, 4:5]], [ob], gdeps=[vc])
                    store_tile(C, kbnT, h * 128, t0, ob, final=True)
                else:
                    epi_raw_store(C, r3, vbT, t0, lambda jj: (jj // 2) * 128)(j, ps)
            linear(C, ckvb, w_ukv, 32, epi_kv)
        P.emit()
    return C.nc


def epi_acc(C, acc):
    P = C.P
    tok = C.tok

    def epi(j, ps):
        P.op("dve", lambda e: e.tensor_tensor(out=acc[j].ap[:, :tok], in0=ps.ap[:, :tok], in1=acc[j].ap[:, :tok],
                                              op=ALU.add), reads=[ps, acc[j]], writes=[acc[j]])
    return epi


def cross_attn(C, acc, gx, w_xq, gxq, memk, memv, w_xo, hT, kt4):
    P = C.P
    tok = C.tok
    norm_prep(C, acc, gx, hT, C.rstd)
    qx = []

    def epi_q(j, ps):
        ob = C.outb.next()
        head_norm(C, [ps], C.rstd, 128, [gxq.ap[:, 0:1]], [ob], gdeps=[gxq])
        qx.append(ob)
    linear(C, hT, w_xq, 4, epi_q)
    scale = 128 ** -0.5
    for h in range(4):
        sp = C.ps_misc.next()
        for mb in range(2):
            P.op("pe", lambda e, h=h, mb=mb, sp=sp: e.matmul(sp.ap[:, mb * tok:(mb + 1) * tok],
                                                             lhsT=memk.ap[:, h, mb * 128:(mb + 1) * 128],
                                                             rhs=qx[h].ap[:, :tok], start=True, stop=True),
                 reads=[memk, qx[h]], writes=[sp])
        pT = C.pT
        P.op("act", lambda e, sp=sp: e.activation(out=pT.ap[:, :2 * tok], in_=sp.ap[:, :2 * tok], func=AF.Exp,
                                                  scale=scale), reads=[sp], writes=[pT])
        dn = C.ps_stat.next()
        op_ = C.ps_misc.next()
        for mb in range(2):
            P.op("pe", lambda e, mb=mb, dn=dn: e.matmul(dn.ap[:, :tok], lhsT=C.onesb.ap[:],
                                                        rhs=pT.ap[:, mb * tok:(mb + 1) * tok],
                                                        start=(mb == 0), stop=(mb == 1)),
                 reads=[pT, C.onesb], writes=[dn])
        for mb in range(2):
            P.op("pe", lambda e, mb=mb, h=h, op_=op_: e.matmul(op_.ap[:, :tok], lhsT=memv.ap[:, mb, h, :],
                                                               rhs=pT.ap[:, mb * tok:(mb + 1) * tok],
                                                               start=(mb == 0), stop=(mb == 1)),
                 reads=[pT, memv], writes=[op_])
        rc = C.tmp.next()
        P.op("dve", lambda e, rc=rc, dn=dn: e.reciprocal(out=rc.ap[:, :tok], in_=dn.ap[:, :tok]),
             reads=[dn], writes=[rc])
        P.op("dve", lambda e, rc=rc, op_=op_, h=h: e.tensor_tensor(out=kt4[h].ap[:, :tok], in0=op_.ap[:, :tok],
                                                                  in1=rc.ap[:, :tok], op=ALU.mult),
             reads=[op_, rc], writes=[kt4[h]])
    linear(C, kt4, w_xo, KT, epi_acc(C, acc))


def ffn_blocks(C, acc, hT, hid, blocks):
    P = C.P
    tok = C.tok
    for (wg, wu, wd, nft, col0, row0, cse) in blocks:
        f = 0
        while f < nft:
            w = min(2, nft - f)
            wbg = C.wrot.next()
            P.dma("pool", wbg.ap[:, 0:KT, 0:w * 128],
                  wg[:, col0 + f * 128: col0 + (f + w) * 128].rearrange("(kc p) n -> p kc n", p=128), writes=[wbg])
            wbu = C.wrot.next()
            P.dma("pool", wbu.ap[:, 0:KT, 0:w * 128],
                  wu[:, col0 + f * 128: col0 + (f + w) * 128].rearrange("(kc p) n -> p kc n", p=128), writes=[wbu])
            for jj in range(w):
                pg = C.psrot.next()
                pu = C.psrot.next()
                for k in range(KT):
                    P.op("pe", lambda e, k=k, jj=jj, wbg=wbg, pg=pg: e.matmul(
                        pg.ap[:, :tok], lhsT=wbg.ap[:, k, jj * 128:(jj + 1) * 128], rhs=hT[k].ap[:, :tok],
                        start=(k == 0), stop=(k == KT - 1)), reads=[wbg, hT[k]], writes=[pg])
                for k in range(KT):
                    P.op("pe", lambda e, k=k, jj=jj, wbu=wbu, pu=pu: e.matmul(
                        pu.ap[:, :tok], lhsT=wbu.ap[:, k, jj * 128:(jj + 1) * 128], rhs=hT[k].ap[:, :tok],
                        start=(k == 0), stop=(k == KT - 1)), reads=[wbu, hT[k]], writes=[pu])
                gs = C.tmp.next()
                P.op("dve", lambda e, gs=gs, pg=pg: e.tensor_tensor(out=gs.ap[:, :tok], in0=pg.ap[:, :tok],
                                                                    in1=C.rstd.ap[:, :tok], op=ALU.mult),
                     reads=[pg, C.rstd], writes=[gs])
                P.op("act", lambda e, gs=gs: e.activation(out=gs.ap[:, :tok], in_=gs.ap[:, :tok], func=AF.Silu),
                     reads=[gs], writes=[gs])
                us = C.tmp.next()
                P.op("dve", lambda e, us=us, pu=pu, cse=cse: e.tensor_tensor(out=us.ap[:, :tok], in0=pu.ap[:, :tok],
                                                                             in1=cse.ap[:, :tok], op=ALU.mult),
                     reads=[pu, cse], writes=[us])
                P.op("pool", lambda e, gs=gs, us=us, t=hid[f + jj]: e.tensor_tensor(
                    out=t.ap[:, :tok], in0=gs.ap[:, :tok], in1=us.ap[:, :tok], op=ALU.mult),
                    reads=[gs, us], writes=[hid[f + jj]])
            f += w
        linear(C, hid[:nft], wd[row0:row0 + nft * 128, :], KT, epi_acc(C, acc))


def moe_gates(C, lg, rstd, b_r, ident, sel, cse):
    P = C.P
    tok = C.tok
    if not hasattr(C, "_mg"):
        C._mg = (C.sb("lT", [8, tok], F32), C.sb("gT", [8, tok], F32),
                 [C.sb(f"sm{i}", [128, 8], F32) for i in range(4)],
                 [C.sb(f"s1_{i}", [128, 1], F32) for i in range(6)])
    lT, gT, sm, s1 = C._mg
    P.op("dve", lambda e: e.tensor_tensor(out=lT.ap[:, :], in0=lg.ap[0:8, :tok], in1=rstd.ap[0:8, :tok], op=ALU.mult),
         reads=[lg, rstd], writes=[lT])
    P.op("dve", lambda e: e.tensor_scalar(out=lT.ap[:, :], in0=lT.ap[:, :], scalar1=b_r.ap[0:8, 0:1], scalar2=None,
                                          op0=ALU.add), reads=[lT, b_r], writes=[lT])
    for st in range(tok // 128):
        pt = C.ps_misc.next()
        P.op("pe", lambda e, st=st, pt=pt: e.matmul(pt.ap[:, 0:8], lhsT=lT.ap[:, st * 128:(st + 1) * 128],
                                                    rhs=ident.ap[0:8, 0:8], start=True, stop=True),
             reads=[lT, ident], writes=[pt])
        l, eq1, l2, eq2 = sm
        m1, m2, dd, e2, w1, w2 = s1
        P.op("dve", lambda e, pt=pt: e.tensor_copy(out=l.ap[:], in_=pt.ap[:, 0:8]), reads=[pt], writes=[l])
        P.op("dve", lambda e: e.tensor_reduce(out=m1.ap[:], in_=l.ap[:], axis=AX.X, op=ALU.max), reads=[l], writes=[m1])
        P.op("dve", lambda e: e.tensor_scalar(out=eq1.ap[:], in0=l.ap[:], scalar1=m1.ap[:, 0:1], scalar2=None,
                                              op0=ALU.is_equal), reads=[l, m1], writes=[eq1])
        P.op("dve", lambda e: e.scalar_tensor_tensor(out=l2.ap[:], in0=eq1.ap[:], scalar=-1e30, in1=l.ap[:],
                                                     op0=ALU.mult, op1=ALU.add), reads=[eq1, l], writes=[l2])
        P.op("dve", lambda e: e.tensor_reduce(out=m2.ap[:], in_=l2.ap[:], axis=AX.X, op=ALU.max), reads=[l2], writes=[m2])
        P.op("dve", lambda e: e.tensor_scalar(out=eq2.ap[:], in0=l2.ap[:], scalar1=m2.ap[:, 0:1], scalar2=None,
                                              op0=ALU.is_equal), reads=[l2, m2], writes=[eq2])
        P.op("dve", lambda e: e.tensor_tensor(out=dd.ap[:], in0=m2.ap[:], in1=m1.ap[:], op=ALU.subtract),
             reads=[m1, m2], writes=[dd])
        P.op("act", lambda e: e.activation(out=e2.ap[:], in_=dd.ap[:], func=AF.Exp), reads=[dd], writes=[e2])
        P.op("dve", lambda e: e.tensor_scalar(out=w1.ap[:], in0=e2.ap[:], scalar1=1.0, scalar2=None, op0=ALU.add),
             reads=[e2], writes=[w1])
        P.op("dve", lambda e: e.reciprocal(out=w1.ap[:], in_=w1.ap[:]), reads=[w1], writes=[w1])
        P.op("dve", lambda e: e.tensor_tensor(out=w2.ap[:], in0=e2.ap[:], in1=w1.ap[:], op=ALU.mult),
             reads=[e2, w1], writes=[w2])
        P.op("dve", lambda e: e.tensor_scalar(out=eq1.ap[:], in0=eq1.ap[:], scalar1=w1.ap[:, 0:1], scalar2=None,
                                              op0=ALU.mult), reads=[eq1, w1], writes=[eq1])
        P.op("dve", lambda e: e.scalar_tensor_tensor(out=eq2.ap[:], in0=eq2.ap[:], scalar=w2.ap[:, 0:1], in1=eq1.ap[:],
                                                     op0=ALU.mult, op1=ALU.add), reads=[eq2, w2, eq1], writes=[eq2])
        pt2 = C.ps_misc.next()
        P.op("pe", lambda e, pt2=pt2: e.matmul(pt2.ap[0:8, 0:128], lhsT=eq2.ap[:, 0:8], rhs=ident.ap[:, :],
                                               start=True, stop=True), reads=[eq2, ident], writes=[pt2])
        P.op("dve", lambda e, pt2=pt2, st=st: e.tensor_copy(out=gT.ap[:, st * 128:(st + 1) * 128],
                                                            in_=pt2.ap[0:8, 0:128]), reads=[pt2], writes=[gT])
    for ex in range(8):
        pb = C.ps_misc.next()
        P.op("pe", lambda e, ex=ex, pb=pb: e.matmul(pb.ap[:, :tok], lhsT=sel.ap[0:8, ex, :], rhs=gT.ap[:, :tok],
                                                    start=True, stop=True), reads=[sel, gT], writes=[pb])
        P.op("dve", lambda e, ex=ex, pb=pb: e.tensor_tensor(out=cse[ex].ap[:, :tok], in0=pb.ap[:, :tok],
                                                            in1=rstd.ap[:, :tok], op=ALU.mult),
             reads=[pb, rstd], writes=[cse[ex]])


def build_C(layer):
    C = Ctx(TOK)
    P = C.P
    T = TOKC
    xT = C.dram_in("xT", [D, T], F32)
    oT = C.dram_in("oT", [D, T], BF16)
    w_o = C.dram_in("w_o", [D, D], F32)
    gvec = C.dram_in("gvec", [128, 3, KT], F32)
    gxq_d = C.dram_in("g_xq", [128, 1], F32)
    w_xq = C.dram_in("w_xq", [D, 512], F32)
    w_xo = C.dram_in("w_xo", [512, D], F32)
    memk_d = C.dram_in("memk", [128, 4, 256], BF16)
    memv_d = C.dram_in("memv", [128, 2, 4, 128], BF16)
    xo = C.dram_out("xoT", [D, T], F32)
    if layer == 0:
        w_gate = C.dram_in("w_gate", [D, 14336], F32)
        w_up = C.dram_in("w_up", [D, 14336], F32)
        w_down = C.dram_in("w_down", [14336, D], F32)
        w_qkv = C.dram_in("w_qkv", [D, 3 * D], F32)
        qkvT = C.dram_out("qkvT", [3 * D, T], BF16)
    else:
        w_eg = C.dram_in("w_egate", [8, D, D], F32)
        w_eu = C.dram_in("w_eup", [8, D, D], F32)
        w_ed = C.dram_in("w_edown", [8, D, D], F32)
        w_r_d = C.dram_in("w_router", [128, KT, 8], F32)
        b_r_d = C.dram_in("b_router", [8, 1], F32)
        ident_d = C.dram_in("ident", [128, 128], F32)
        sel_d = C.dram_in("sel", [8, 8, 128], F32)
    with C.es:
        C.setup_common()
        tok = C.tok
        gv = load_const(C, "gvec", gvec, [128, 3, KT], F32)
        gx = Buf(gv.ap[:, 0, :], "gx"); gf = Buf(gv.ap[:, 1, :], "gf"); gn = Buf(gv.ap[:, 2, :], "gn")
        for b in (gx, gf, gn):
            b.last_w = gv.last_w
        gxq = load_const(C, "gxq", gxq_d, [128, 1], F32)
        memk = load_const(C, "memk", memk_d, [128, 4, 256], BF16)
        memv = load_const(C, "memv", memv_d, [128, 2, 4, 128], BF16)
        C.pT = C.sb("pT", [128, 2 * tok], BF16)
        acc, acc_all = C.sbn("acc", KT, [128, tok], F32)
        hT, _ = C.sbn("hT", KT, [128, tok], BF16)
        hid, hid_all = C.sbn("hid", KT, [128, tok], BF16)
        kt4, _ = C.sbn("kt4", 4, [128, tok], BF16)
        if layer == 1:
            w_r = load_const(C, "w_r", w_r_d, [128, KT, 8], F32)
            b_r = load_const(C, "b_r", b_r_d, [8, 1], F32)
            ident = load_const(C, "ident", ident_d, [128, 128], F32)
            sel = load_const(C, "sel", sel_d, [8, 8, 128], F32)
            cse, _ = C.sbn("cse", 8, [128, tok], F32)
        for ch in range(T // tok):
            t0 = ch * tok
            P.dma("sp", acc_all.ap[:], xT[:, t0:t0 + tok].rearrange("(kc p) t -> p kc t", p=128),
                  writes=acc + [acc_all])
            P.dma("sp", hid_all.ap[:], oT[:, t0:t0 + tok].rearrange("(kc p) t -> p kc t", p=128),
                  writes=hid + [hid_all])
            linear(C, hid, w_o, KT, epi_acc(C, acc))
            cross_attn(C, acc, gx, w_xq, gxq, memk, memv, w_xo, hT, kt4)
            if layer == 0:
                norm_prep(C, acc, gf, hT, C.rstd)
                blocks = [(w_gate, w_up, w_down, 28, b * 3584, b * 3584, C.rstd) for b in range(4)]
                ffn_blocks(C, acc, hT, hid, blocks)
                norm_prep(C, acc, gn, hT, C.rstd)
                linear(C, hT, w_qkv, 96, epi_raw_store(C, C.rstd, qkvT, t0, lambda j: j * 128))
            else:
                lg = C.ps_misc.next()
                norm_prep(C, acc, gf, hT, C.rstd, router=(w_r, lg))
                moe_gates(C, lg, C.rstd, b_r, ident, sel, cse)
                blocks = [(w_eg[ex], w_eu[ex], w_ed[ex], KT, 0, 0, cse[ex]) for ex in range(8)]
                ffn_blocks(C, acc, hT, hid, blocks)
            P.dma("act", xo[:, t0:t0 + tok].rearrange("(kc p) t -> p kc t", p=128), acc_all.ap[:],
                  reads=acc + [acc_all], final=True)
        P.emit()
    return C.nc


def cols(v, n):
    return np.ascontiguousarray(np.asarray(v, np.float32).reshape(n, 128).T)


def pad128(v):
    out = np.zeros(128, np.float32)
    out[:len(v)] = v
    return out


def rope_consts():
    theta = np.float32(500000.0)
    invA = np.zeros(128, np.float32)
    fA = theta ** (-(np.arange(0, 32, 2, dtype=np.float32)) / np.float32(32))
    invA[:16] = fA; invA[16:32] = fA
    invB = np.zeros(128, np.float32)
    fB = theta ** (-(np.arange(0, 64, 2, dtype=np.float32)) / np.float32(64))
    invB[:32] = fB; invB[32:64] = fB
    rots = np.zeros((128, 2, 128), np.float32)
    for i, half in enumerate((16, 32)):
        for m in range(half):
            rots[m + half, i, m] = -1.0
            rots[m, i, m + half] = 1.0
    return invA, invB, rots


def prep_A0(inp):
    x = inp["x"]
    xT = np.ascontiguousarray(x.reshape(-1, D).T)
    pos = inp["positions"].reshape(-1).astype(np.int32)
    w_in = np.concatenate([inp["w_in"][0], np.zeros((D, 64), np.float32)], axis=1)
    wq = inp["w_uq"][0].reshape(1536, 16, 192)
    w_uq = np.concatenate([wq, np.zeros((1536, 16, 64), np.float32)], axis=2).reshape(1536, 4096)
    w_ukv = np.ascontiguousarray(inp["w_ukv"][0])
    invA, invB, rots = rope_consts()
    gbq = inp["gb_q"][0]
    vecs = np.stack([inp["ga_q"][0], inp["ga_k"][0], gbq[:128], pad128(gbq[128:]), inp["gb_kn"][0],
                     pad128(inp["gb_kr"][0]), invA, invB], axis=1).astype(np.float32)
    shared = {"w_in": w_in, "w_uq": w_uq, "w_ukv": w_ukv, "g_mix": cols(inp["g_mix"][0], 32),
              "g_cq": cols(inp["g_cq"][0], 12), "g_ckv": cols(inp["g_ckv"][0], 4),
              "vecs": np.ascontiguousarray(vecs), "rots": rots}
    ins = []
    for c in range(NCORES):
        m = dict(shared)
        m["xT"] = np.ascontiguousarray(xT[:, c * TOKC:(c + 1) * TOKC])
        m["posb"] = np.ascontiguousarray(np.broadcast_to(pos[c * TOKC:(c + 1) * TOKC][None, :], (128, TOKC)))
        ins.append(m)
    return ins


def build_B0():
    C = Ctx(512)
    P = C.P
    NH = 8
    qa = C.dram_in("qa", [NH, 128, SEQ], BF16)
    ka = C.dram_in("ka", [NH, 128, SEQ], BF16)
    va = C.dram_in("va", [NH, SEQ, 128], BF16)
    qb = C.dram_in("qb", [NH, 256, SEQ], BF16)
    kbn = C.dram_in("kbn", [NH, 128, SEQ], BF16)
    krd = C.dram_in("kr", [128, SEQ], BF16)
    vb = C.dram_in("vb", [NH, SEQ, 128], BF16)
    mask2_d = C.dram_in("mask2", [128, 256], BF16)
    maskc_d = C.dram_in("maskc", [128, 4, 512], BF16)
    oT = C.dram_out("oT", [2 * NH, 128, SEQ], BF16)
    with C.es:
        onesb = C.sb("onesb", [128, 128], BF16)
        P.op("pool", lambda e: e.memset(onesb.ap[:], 1.0), writes=[onesb])
        mask2 = load_const(C, "mask2", mask2_d, [128, 256], BF16)
        maskc = load_const(C, "maskc", maskc_d, [128, 4, 512], BF16)
        kr = load_const(C, "kr", krd, [128, SEQ], BF16)
        psS = Rot([C.ps(f"pS{i}") for i in range(3)])
        psO = Rot([C.ps(f"pO{i}") for i in range(2)])
        psD = Rot([C.ps(f"pD{i}") for i in range(2)])
        qrot = Rot([C.sb(f"q{i}", [128, SEQ], BF16) for i in range(2)])
        krot = Rot([C.sb(f"k{i}", [128, SEQ], BF16) for i in range(2)])
        q2rot = Rot([C.sb(f"q2{i}", [128, SEQ], BF16) for i in range(2)])
        vrot = Rot([C.sb(f"v{i}", [128, 3, 16, 128], BF16) for i in range(2)])
        qd = [None, C.sb("qd1", [128, SEQ], BF16), C.sb("qd2", [128, SEQ], BF16)]
        kd = [None, C.sb("kd1", [128, SEQ], BF16), C.sb("kd2", [128, SEQ], BF16)]
        acc = C.sb("acc", [128, 2, SEQ], F32)
        rc = C.sb("rc", [128, SEQ], F32)
        obuf = Rot([C.sb(f"ob{i}", [128, SEQ], BF16) for i in range(2)])
        pTr = Rot([C.sb(f"pT{i}", [128, 512], BF16) for i in range(3)])
        rc5 = Rot([C.sb(f"rc5{i}", [128, 512], F32) for i in range(2)])
        sA = 128 ** -0.5
        sB = 192 ** -0.5
        for h in range(NH):
            q = qrot.next(); k = krot.next(); v3 = vrot.next()
            P.dma("sp", q.ap[:], qa[h], writes=[q])
            P.dma("sp", k.ap[:], ka[h], writes=[k])
            P.dma("sp", v3.ap[:, 0], va[h].rearrange("(i p) d -> p i d", p=128), writes=[v3])
            P.dma("sp", v3.ap[:, 1].rearrange("p (i r) d -> p i r d", r=4),
                  va[h].rearrange("(i p r) d -> p i r d", i=4, p=128, r=4), writes=[v3])
            P.dma("sp", v3.ap[:, 2], va[h].rearrange("(p r) d -> p r d", r=16), writes=[v3])
            for br, dil in ((1, 4), (2, 16)):
                nb = 16 // dil
                P.op("pool", lambda e, br=br, dil=dil, nb=nb, q=q: e.tensor_copy(
                    out=qd[br].ap.rearrange("p (r i j) -> p r i j", r=dil, i=nb, j=128),
                    in_=q.ap.rearrange("p (i j r) -> p r i j", i=nb, j=128, r=dil)), reads=[q], writes=[qd[br]])
                P.op("dve", lambda e, br=br, dil=dil, nb=nb, k=k: e.tensor_copy(
                    out=kd[br].ap.rearrange("p (r i j) -> p r i j", r=dil, i=nb, j=128),
                    in_=k.ap.rearrange("p (i j r) -> p r i j", i=nb, j=128, r=dil)), reads=[k], writes=[kd[br]])
            for br, dil in ((0, 1), (1, 4), (2, 16)):
                nb = 16 // dil
                qq = q if br == 0 else qd[br]
                kk = k if br == 0 else kd[br]
                accv = acc.ap.rearrange("p a (i j r) -> p a r i j", i=nb, j=128, r=dil)
                for r in range(dil):
                    for i in range(nb):
                        c0 = (r * nb + i) * 128
                        sp = psS.next()
                        if i >= 1:
                            P.op("pe", lambda e, sp=sp, kk=kk, qq=qq, c0=c0: e.matmul(
                                sp.ap[:, 0:128], lhsT=kk.ap[:, c0 - 128:c0], rhs=qq.ap[:, c0:c0 + 128],
                                start=True, stop=True), reads=[kk, qq], writes=[sp])
                        P.op("pe", lambda e, sp=sp, kk=kk, qq=qq, c0=c0: e.matmul(
                            sp.ap[:, 128:256], lhsT=kk.ap[:, c0:c0 + 128], rhs=qq.ap[:, c0:c0 + 128],
                            start=True, stop=True), reads=[kk, qq], writes=[sp])
                        lo = 0 if i >= 1 else 128
                        pT = pTr.next()
                        P.op("act", lambda e, sp=sp, pT=pT, lo=lo: e.activation(
                            out=pT.ap[:, lo:256], in_=sp.ap[:, lo:256], func=AF.Exp, scale=sA), reads=[sp], writes=[pT])
                        P.op("pool", lambda e, pT=pT, lo=lo: e.tensor_tensor(
                            out=pT.ap[:, lo:256], in0=pT.ap[:, lo:256], in1=mask2.ap[:, lo:256], op=ALU.mult),
                            reads=[pT, mask2], writes=[pT])
                        po = psO.next()
                        vi = i * dil + r
                        if i >= 1:
                            P.op("pe", lambda e, po=po, pT=pT, v3=v3, br=br, vi=vi, dil=dil: e.matmul(
                                po.ap[:, 0:128], lhsT=v3.ap[:, br, vi - dil, :], rhs=pT.ap[:, 0:128],
                                start=True, stop=False), reads=[v3, pT], writes=[po])
                        P.op("pe", lambda e, po=po, pT=pT, v3=v3, br=br, vi=vi, i=i: e.matmul(
                            po.ap[:, 0:128], lhsT=v3.ap[:, br, vi, :], rhs=pT.ap[:, 128:256],
                            start=(i == 0), stop=True), reads=[v3, pT], writes=[po])
                        if i >= 1:
                            P.op("pe", lambda e, po=po, pT=pT: e.matmul(
                                po.ap[:, 128:256], lhsT=onesb.ap[:], rhs=pT.ap[:, 0:128],
                                start=True, stop=False), reads=[onesb, pT], writes=[po])
                        P.op("pe", lambda e, po=po, pT=pT, i=i: e.matmul(
                            po.ap[:, 128:256], lhsT=onesb.ap[:], rhs=pT.ap[:, 128:256],
                            start=(i == 0), stop=True), reads=[onesb, pT], writes=[po])
                        pov = po.ap[:, 0:256].rearrange("p (a j) -> p a j", a=2)
                        if br == 0:
                            P.op("dve", lambda e, pov=pov, accv=accv, r=r, i=i: e.tensor_copy(
                                out=accv[:, :, r, i, :], in_=pov), reads=[po], writes=[acc])
                        else:
                            P.op("dve", lambda e, pov=pov, accv=accv, r=r, i=i: e.tensor_tensor(
                                out=accv[:, :, r, i, :], in0=pov, in1=accv[:, :, r, i, :], op=ALU.add),
                                reads=[po, acc], writes=[acc])
            ob = obuf.next()
            P.op("dve", lambda e: e.reciprocal(out=rc.ap[:], in_=acc.ap[:, 1, :]), reads=[acc], writes=[rc])
            P.op("dve", lambda e, ob=ob: e.tensor_tensor(out=ob.ap[:], in0=acc.ap[:, 0, :], in1=rc.ap[:], op=ALU.mult),
                 reads=[acc, rc], writes=[ob])
            P.dma("act", oT[h], ob.ap[:], reads=[ob], final=True)
        for h in range(NH):
            qn = qrot.next(); qr = q2rot.next(); kn = krot.next(); v3 = vrot.next()
            P.dma("sp", qn.ap[:], qb[h, 0:128, :], writes=[qn])
            P.dma("sp", qr.ap[:], qb[h, 128:256, :], writes=[qr])
            P.dma("sp", kn.ap[:], kbn[h], writes=[kn])
            P.dma("sp", v3.ap[:, 0], vb[h].rearrange("(i p) d -> p i d", p=128), writes=[v3])
            ob = obuf.next()
            for G in range(4):
                po = psO.next(); pd = psD.next()
                nkb = 4 * G + 4
                for kb in range(nkb):
                    sp = psS.next()
                    P.op("pe", lambda e, sp=sp, kn=kn, qn=qn, kb=kb, G=G: e.matmul(
                        sp.ap[:, :], lhsT=kn.ap[:, kb * 128:(kb + 1) * 128], rhs=qn.ap[:, G * 512:(G + 1) * 512],
                        start=True, stop=False), reads=[kn, qn], writes=[sp])
                    P.op("pe", lambda e, sp=sp, qr=qr, kb=kb, G=G: e.matmul(
                        sp.ap[:, :], lhsT=kr.ap[:, kb * 128:(kb + 1) * 128], rhs=qr.ap[:, G * 512:(G + 1) * 512],
                        start=False, stop=True), reads=[kr, qr], writes=[sp])
                    pT = pTr.next()
                    P.op("act", lambda e, sp=sp, pT=pT: e.activation(out=pT.ap[:, :], in_=sp.ap[:, :], func=AF.Exp,
                                                                     scale=sB), reads=[sp], writes=[pT])
                    if kb >= 4 * G:
                        P.op("pool", lambda e, pT=pT, o=kb - 4 * G: e.tensor_tensor(
                            out=pT.ap[:, :], in0=pT.ap[:, :], in1=maskc.ap[:, o, :], op=ALU.mult),
                            reads=[pT, maskc], writes=[pT])
                    P.op("pe", lambda e, po=po, pT=pT, v3=v3, kb=kb, nkb=nkb: e.matmul(
                        po.ap[:, :], lhsT=v3.ap[:, 0, kb, :], rhs=pT.ap[:, :], start=(kb == 0), stop=(kb == nkb - 1)),
                        reads=[v3, pT], writes=[po])
                    P.op("pe", lambda e, pd=pd, pT=pT, kb=kb, nkb=nkb: e.matmul(
                        pd.ap[:, :], lhsT=onesb.ap[:], rhs=pT.ap[:, :], start=(kb == 0), stop=(kb == nkb - 1)),
                        reads=[onesb, pT], writes=[pd])
                r5 = rc5.next()
                P.op("dve", lambda e, r5=r5, pd=pd: e.reciprocal(out=r5.ap[:], in_=pd.ap[:]), reads=[pd], writes=[r5])
                P.op("dve", lambda e, r5=r5, po=po, ob=ob, G=G: e.tensor_tensor(
                    out=ob.ap[:, G * 512:(G + 1) * 512], in0=po.ap[:], in1=r5.ap[:], op=ALU.mult),
                    reads=[po, r5], writes=[ob])
            P.dma("act", oT[NH + h], ob.ap[:], reads=[ob], final=True)
        P.emit()
    return C.nc


def build_B1(NH=16):
    C = Ctx(512)
    P = C.P
    qd_ = C.dram_in("q", [NH, 128, SEQ], BF16)
    kd_ = C.dram_in("k", [NH, 128, SEQ], BF16)
    vd_ = C.dram_in("v", [NH, SEQ, 128], BF16)
    tri_d = C.dram_in("tri", [128, 128], BF16)
    masks_d = C.dram_in("masks", [128, 4, 512], BF16)
    oT = C.dram_out("oT", [NH, 128, SEQ], BF16)
    with C.es:
        onesb = C.sb("onesb", [128, 128], BF16)
        P.op("pool", lambda e: e.memset(onesb.ap[:], 1.0), writes=[onesb])
        onef = C.sb("onef", [128, 1], F32)
        P.op("pool", lambda e: e.memset(onef.ap[:], 1.0), writes=[onef])
        tri = load_const(C, "tri", tri_d, [128, 128], BF16)
        masks = load_const(C, "masks", masks_d, [128, 4, 512], BF16)
        psS = Rot([C.ps(f"pS{i}") for i in range(2)])
        psL = Rot([C.ps(f"pL{i}") for i in range(2)])
        psC = Rot([C.ps(f"pC{i}") for i in range(2)])
        psO = Rot([C.ps(f"pO{i}") for i in range(2)])
        qrot = Rot([C.sb(f"q{i}", [128, SEQ], BF16) for i in range(2)])
        krot = Rot([C.sb(f"k{i}", [128, SEQ], BF16) for i in range(2)])
        vrot = Rot([C.sb(f"v{i}", [128, 16, 128], BF16) for i in range(2)])
        obuf = Rot([C.sb(f"ob{i}", [128, SEQ], BF16) for i in range(2)])
        ezr = Rot([C.sb(f"ez{i}", [128, 512], F32) for i in range(2)])
        Lr = Rot([C.sb(f"L{i}", [128, 512], F32) for i in range(3)])
        nlr = Rot([C.sb(f"nl{i}", [128, 512], BF16) for i in range(3)])
        tr = Rot([C.sb(f"t{i}", [128, 512], F32) for i in range(3)])
        pTr = Rot([C.sb(f"pT{i}", [128, 512], BF16) for i in range(3)])
        carry = C.sb("carry", [128, 512], F32)
        s = 128 ** -0.5
        for h in range(NH):
            q = qrot.next(); k = krot.next(); v = vrot.next()
            P.dma("sp", q.ap[:], qd_[h], writes=[q])
            P.dma("sp", k.ap[:], kd_[h], writes=[k])
            P.dma("sp", v.ap[:], vd_[h].rearrange("(i p) d -> p i d", p=128), writes=[v])
            ob = obuf.next()
            for G in range(4):
                po = psO.next()
                nkb = 4 * G + 4
                for idx, kb in enumerate(reversed(range(nkb))):
                    diag = kb >= 4 * G
                    o = kb - 4 * G
                    sp = psS.next()
                    P.op("pe", lambda e, sp=sp, k=k, q=q, kb=kb, G=G: e.matmul(
                        sp.ap[:, :], lhsT=k.ap[:, kb * 128:(kb + 1) * 128], rhs=q.ap[:, G * 512:(G + 1) * 512],
                        start=True, stop=True), reads=[k, q], writes=[sp])
                    ez = ezr.next(); L = Lr.next(); nl = nlr.next(); t = tr.next(); pT = pTr.next()
                    P.op("act", lambda e, sp=sp, ez=ez: e.activation(out=ez.ap[:], in_=sp.ap[:], func=AF.Exp, scale=-s),
                         reads=[sp], writes=[ez])
                    P.op("act", lambda e, ez=ez, L=L: e.activation(out=L.ap[:], in_=ez.ap[:], func=AF.Ln,
                                                                   bias=onef.ap[:, 0:1], scale=1.0),
                         reads=[ez, onef], writes=[L])
                    P.op("dve", lambda e, sp=sp, L=L, nl=nl: e.scalar_tensor_tensor(
                        out=nl.ap[:], in0=sp.ap[:], scalar=s, in1=L.ap[:], op0=ALU.mult, op1=ALU.add),
                        reads=[sp, L], writes=[nl])
                    if diag:
                        P.op("pool", lambda e, nl=nl, o=o: e.tensor_tensor(out=nl.ap[:], in0=nl.ap[:],
                                                                          in1=masks.ap[:, o, :], op=ALU.mult),
                             reads=[nl, masks], writes=[nl])
                    pl = psL.next(); pc = psC.next()
                    P.op("pe", lambda e, pl=pl, nl=nl: e.matmul(pl.ap[:], lhsT=tri.ap[:], rhs=nl.ap[:],
                                                                start=True, stop=True), reads=[tri, nl], writes=[pl])
                    P.op("pe", lambda e, pc=pc, nl=nl: e.matmul(pc.ap[:], lhsT=onesb.ap[:], rhs=nl.ap[:],
                                                                start=True, stop=True), reads=[onesb, nl], writes=[pc])
                    P.op("dve", lambda e, pl=pl, L=L, t=t: e.tensor_tensor(out=t.ap[:], in0=pl.ap[:], in1=L.ap[:],
                                                                           op=ALU.add), reads=[pl, L], writes=[t])
                    if idx > 0:
                        P.op("pool", lambda e, t=t: e.tensor_tensor(out=t.ap[:], in0=t.ap[:], in1=carry.ap[:],
                                                                    op=ALU.add), reads=[t, carry], writes=[t])
                    P.op("act", lambda e, t=t, pT=pT: e.activation(out=pT.ap[:], in_=t.ap[:], func=AF.Exp, scale=-1.0),
                         reads=[t], writes=[pT])
                    if diag:
                        P.op("pool", lambda e, pT=pT, o=o: e.tensor_tensor(out=pT.ap[:], in0=pT.ap[:],
                                                                          in1=masks.ap[:, o, :], op=ALU.mult),
                             reads=[pT, masks], writes=[pT])
                    P.op("pe", lambda e, po=po, pT=pT, v=v, kb=kb, idx=idx, nkb=nkb: e.matmul(
                        po.ap[:], lhsT=v.ap[:, kb, :], rhs=pT.ap[:], start=(idx == 0), stop=(idx == nkb - 1)),
                        reads=[v, pT], writes=[po])
                    if idx < nkb - 1:
                        if idx == 0:
                            P.op("dve", lambda e, pc=pc: e.tensor_copy(out=carry.ap[:], in_=pc.ap[:]),
                                 reads=[pc], writes=[carry])
                        else:
                            P.op("dve", lambda e, pc=pc: e.tensor_tensor(out=carry.ap[:], in0=pc.ap[:], in1=carry.ap[:],
                                                                         op=ALU.add), reads=[pc, carry], writes=[carry])
                P.op("dve", lambda e, po=po, ob=ob, G=G: e.tensor_copy(out=ob.ap[:, G * 512:(G + 1) * 512], in_=po.ap[:]),
                     reads=[po], writes=[ob])
            P.dma("act", oT[h], ob.ap[:], reads=[ob], final=True)
        P.emit()
    return C.nc


def attn_masks():
    kk = np.arange(128)[:, None]
    q1 = np.arange(128)[None, :]
    mask2 = np.concatenate([(kk >= q1), (kk <= q1)], axis=1).astype(NPBF)
    q5 = np.arange(512)[None, :]
    maskc = np.stack([(o * 128 + kk <= q5) for o in range(4)], axis=1).astype(NPBF)
    masks = np.stack([(o * 128 + kk < q5) for o in range(4)], axis=1).astype(NPBF)
    tri = (np.arange(128)[:, None] > np.arange(128)[None, :]).astype(NPBF)
    return mask2, maskc, masks, tri


def run(nc, ins):
    res = run_bass_kernel_spmd(nc, ins, core_ids=list(range(len(ins))))
    return res.results


def gather_T(results, name):
    return np.concatenate([np.asarray(r[name]) for r in results], axis=1)


def heads_fm(full, b, h0, nh, hw=128):
    blk = full[h0 * hw:(h0 + nh) * hw, b * SEQ:(b + 1) * SEQ]
    return np.ascontiguousarray(blk.reshape(nh, hw, SEQ))


def heads_tm(full, b, h0, nh):
    return np.ascontiguousarray(heads_fm(full, b, h0, nh).transpose(0, 2, 1))


def prep_B0(qaT, kaT, vaT, qbT, kbnT, vbT, krT):
    mask2, maskc, _, _ = attn_masks()
    ins = []
    for c in range(NCORES):
        b, j = divmod(c, 2)
        ins.append({"qa": heads_fm(qaT, b, 8 * j, 8), "ka": heads_fm(kaT, b, 8 * j, 8), "va": heads_tm(vaT, b, 8 * j, 8),
                    "qb": heads_fm(qbT, b, 8 * j, 8, 256), "kbn": heads_fm(kbnT, b, 8 * j, 8),
                    "kr": np.ascontiguousarray(krT[:, b * SEQ:(b + 1) * SEQ]), "vb": heads_tm(vbT, b, 8 * j, 8),
                    "mask2": mask2, "maskc": maskc})
    return ins


def assemble_o0(results):
    o = np.zeros((D, 4 * SEQ), NPBF)
    for c, r in enumerate(results):
        b, j = divmod(c, 2)
        t = np.asarray(r["oT"])
        o[(8 * j) * 128:(8 * j + 8) * 128, b * SEQ:(b + 1) * SEQ] = t[:8].reshape(1024, SEQ)
        o[2048 + (8 * j) * 128:2048 + (8 * j + 8) * 128, b * SEQ:(b + 1) * SEQ] = t[8:].reshape(1024, SEQ)
    return o


def prep_B1(qkvT):
    _, _, masks, tri = attn_masks()
    ins = []
    for c in range(NCORES):
        b, j = divmod(c, 2)
        ins.append({"q": heads_fm(qkvT[0:D], b, 16 * j, 16), "k": heads_fm(qkvT[D:2 * D], b, 16 * j, 16),
                    "v": heads_tm(qkvT[2 * D:3 * D], b, 16 * j, 16), "tri": tri, "masks": masks})
    return ins


def assemble_o1(results):
    o = np.zeros((D, 4 * SEQ), NPBF)
    for c, r in enumerate(results):
        b, j = divmod(c, 2)
        o[(16 * j) * 128:(16 * j + 16) * 128, b * SEQ:(b + 1) * SEQ] = np.asarray(r["oT"]).reshape(2048, SEQ)
    return o


def prep_C(layer, inp, xT_full, oT_full, mkvT):
    i = 0
    shared = {"w_o": inp["w_o_even"][0] if layer == 0 else inp["w_o_odd"][0],
              "g_xq": np.ascontiguousarray(inp["g_xq"][layer].reshape(128, 1)),
              "w_xq": np.ascontiguousarray(inp["w_xq"][layer]), "w_xo": np.ascontiguousarray(inp["w_xo"][layer])}
    gnext = inp["g_mix"][1] if layer == 0 else inp["g_mix"][1]
    shared["gvec"] = np.ascontiguousarray(np.stack([cols(inp["g_x"][layer], 32), cols(inp["g_ffn"][layer], 32),
                                                    cols(gnext, 32)], axis=1))
    if layer == 0:
        shared.update({"w_gate": inp["w_gate"][i], "w_up": inp["w_up"][i], "w_down": inp["w_down"][i],
                       "w_qkv": inp["w_qkv"][i]})
    else:
        sel = np.zeros((8, 8, 128), np.float32)
        for e in range(8):
            sel[e, e, :] = 1.0
        shared.update({"w_egate": inp["w_egate"][i], "w_eup": inp["w_eup"][i], "w_edown": inp["w_edown"][i],
                       "w_router": np.ascontiguousarray(inp["w_router"][i].reshape(32, 128, 8).transpose(1, 0, 2)),
                       "b_router": np.ascontiguousarray(inp["b_router"][i].reshape(8, 1)),
                       "ident": np.eye(128, dtype=np.float32), "sel": sel})
    ins = []
    for c in range(NCORES):
        b = c // 2
        m = dict(shared)
        m["xT"] = np.ascontiguousarray(xT_full[:, c * TOKC:(c + 1) * TOKC])
        m["oT"] = np.ascontiguousarray(oT_full[:, c * TOKC:(c + 1) * TOKC])
        mk = mkvT[0:512, b * 256:(b + 1) * 256]
        mv = mkvT[512:1024, b * 256:(b + 1) * 256]
        m["memk"] = np.ascontiguousarray(mk.reshape(4, 128, 256).transpose(1, 0, 2))
        m["memv"] = np.ascontiguousarray(mv.reshape(4, 128, 2, 128).transpose(3, 2, 0, 1))
        ins.append(m)
    return ins


def prep_P(inp):
    memT = np.ascontiguousarray(inp["mem"].reshape(-1, D).T)
    ins = []
    for c in range(NCORES):
        ins.append({"memT": np.ascontiguousarray(memT[:, c * 128:(c + 1) * 128]), "g_mem": cols(inp["g_mem"], 32),
                    "g_mk": np.ascontiguousarray(inp["g_mem_k"].reshape(128, 1).astype(np.float32)),
                    "w_mem_kv": inp["w_mem_kv"]})
    return ins


def _kernel_unfused(inp):
    inp = {k: np.asarray(v) for k, v in inp.items()}
    rP = run(build_P(), prep_P(inp))
    mkvT = gather_T(rP, "mkvT")
    rA = run(build_A0(), prep_A0(inp))
    g = {n: gather_T(rA, n) for n in ("qaT", "kaT", "vaT", "qbT", "kbnT", "vbT", "krT")}
    del rA
    rB = run(build_B0(), prep_B0(g["qaT"], g["kaT"], g["vaT"], g["qbT"], g["kbnT"], g["vbT"], g["krT"]))
    o0 = assemble_o0(rB)
    del rB, g
    xT = np.ascontiguousarray(inp["x"].reshape(-1, D).T)
    rC = run(build_C(0), prep_C(0, inp, xT, o0, mkvT))
    x3T = gather_T(rC, "xoT")
    qkvT = gather_T(rC, "qkvT")
    del rC
    rB1 = run(build_B1(), prep_B1(qkvT))
    o1 = assemble_o1(rB1)
    del rB1, qkvT
    rC1 = run(build_C(1), prep_C(1, inp, x3T, o1, mkvT))
    outT = gather_T(rC1, "xoT")
    return np.ascontiguousarray(outT.T).reshape(4, SEQ, D).astype(np.float32, copy=False)


T2 = 2 * TOKC


def body_rope(C, posb, vc_d, tabs):
    P = C.P
    vc = load_const(C, "vecs_r", vc_d, [128, 8], F32)
    for half in range(2):
        sl = slice(half * TOKC, (half + 1) * TOKC)
        for name, col, oc, os_ in (("ra", 6, 0, 1), ("rb", 7, 2, 3)):
            cos, sin = rope_tables(C, posb[:, sl], vc.ap[:, col:col + 1], TOKC, f"{name}{half}", gdeps=[vc])
            P.dma("sp", tabs[oc][0][:, sl], cos.ap[:], reads=[cos], writes=[tabs[oc][1]])
            P.dma("sp", tabs[os_][0][:, sl], sin.ap[:], reads=[sin], writes=[tabs[os_][1]])


def body_P2(C, memT, g_mem, g_mk, w, ident_d, mkT, mv):
    P = C.P
    C.setup_common()
    setup_vt(C, ident_d)
    tok = C.tok
    gm = load_const(C, "gm", g_mem, [128, KT], F32)
    gk = load_const(C, "gk", g_mk, [128, 1], F32)
    acc, acc_all = C.sbn("acc", KT, [128, tok], F32)
    hT, _ = C.sbn("hT", KT, [128, tok], BF16)
    for ch in range(2):
        t0 = ch * tok
        P.dma("sp", acc_all.ap[:], memT[:, t0:t0 + tok].rearrange("(kc p) t -> p kc t", p=128), writes=acc + [acc_all])
        norm_prep(C, acc, gm, hT, C.rstd)

        def epi(j, ps, t0=t0):
            if j < 4:
                ob = C.outb.next()
                head_norm(C, [ps], C.rstd, 128, [gk.ap[:, 0:1]], [ob], gdeps=[gk])
                C.P.dma("act", mkT[0][j * 128:(j + 1) * 128, t0:t0 + tok], ob.ap[:, :tok], reads=[ob], writes=[mkT[1]])
            else:
                epi_v_store(C, C.rstd, mv[0], t0, lambda jj: jj - 4, C.identb)(j, ps)
        linear(C, hT, w, 8, epi)
    mv[1].last_w = [o for o in P.ops["act"] if o.is_dma][-1]


def mark_written(P, bufs, eng="act", n=DMA_ROT):
    last = [o for o in P.ops[eng] if o.is_dma][-1]
    for b in bufs:
        b.last_w = last


def body_A0(C, xT, tabs, w_in, w_uq, w_ukv, gmix, gcq, gckv, vecs, rots, ident_d, outs):
    P = C.P
    qaT, kaT, va, qbT, kbnT, vb, krT = outs
    C.setup_common()
    setup_vt(C, ident_d)
    tok = C.tok
    gm = load_const(C, "gm", gmix, [128, KT], F32)
    gq = load_const(C, "gcq", gcq, [128, 12], F32)
    gkv = load_const(C, "gckv", gckv, [128, 4], F32)
    vc = load_const(C, "vecs", vecs, [128, 8], F32)
    rt = load_const(C, "rots", rots, [128, 2, 128], F32)
    rotA = Buf(rt.ap[:, 0, :], "rotA"); rotB = Buf(rt.ap[:, 1, :], "rotB")
    rotA.last_w = rt.last_w; rotB.last_w = rt.last_w
    tb = [Rot([C.sb(f"tab{i}_{k}", [128, tok], F32) for k in range(2)]) for i in range(4)]
    acc, acc_all = C.sbn("acc", KT, [128, tok], F32)
    hT, _ = C.sbn("hT", KT, [128, tok], BF16)
    cq, _ = C.sbn("cq", 12, [128, tok], F32)
    cqb, _ = C.sbn("cqb", 12, [128, tok], BF16)
    ckv, _ = C.sbn("ckv", 4, [128, tok], F32)
    ckvb, _ = C.sbn("ckvb", 4, [128, tok], BF16)
    r2 = C.sb("r2", [128, tok], F32)
    r3 = C.sb("r3", [128, tok], F32)
    for ch in range(T2 // tok):
        t0 = ch * tok
        P.dma("sp", acc_all.ap[:], xT[:, t0:t0 + tok].rearrange("(kc p) t -> p kc t", p=128), writes=acc + [acc_all])
        cur = []
        for i in range(4):
            b = tb[i].next()
            P.dma("sp", b.ap[:], tabs[i][0][:, t0:t0 + tok], reads=[tabs[i][1]], writes=[b])
            cur.append(b)
        cosA, sinA, cosB, sinB = cur
        norm_prep(C, acc, gm, hT, C.rstd)

        def epi(j, ps, t0=t0, cosA=cosA, sinA=sinA, cosB=cosB, sinB=sinB):
            if j < 32:
                ob = C.outb.next()
                gc = vc.ap[:, 0:1] if j < 16 else vc.ap[:, 1:2]
                head_norm(C, [ps], C.rstd, 128, [gc], [ob], rope=[(rotA, cosA, sinA, 0)], gdeps=[vc])
                store_tile(C, qaT[0] if j < 16 else kaT[0], (j % 16) * 128, t0, ob)
            elif j < 48:
                epi_v_store(C, C.rstd, va[0], t0, lambda jj: jj - 32, C.identb)(j, ps)
            elif j < 64:
                dst = cq[j - 48] if j < 60 else ckv[j - 60]
                P.op("dve", lambda e: e.tensor_tensor(out=dst.ap[:, :tok], in0=ps.ap[:, :tok], in1=C.rstd.ap[:, :tok],
                                                      op=ALU.mult), reads=[ps, C.rstd], writes=[dst])
            else:
                ob = C.outb.next()
                head_norm(C, [ps], C.rstd, 64, [vc.ap[:, 5:6]], [ob], rope=[(rotB, cosB, sinB, 0)], gdeps=[vc])
                store_tile(C, krT[0], 0, t0, ob)
        linear(C, hT, w_in, 65, epi)
        norm_prep(C, cq, gq, cqb, r2, dim=1536)
        pend = {}

        def epi_q(j, ps, t0=t0, cosB=cosB, sinB=sinB):
            h, part = divmod(j, 2)
            if part == 0:
                pend["nope"] = ps
                return
            o1 = C.outb.next(); o2 = C.outb.next()
            head_norm(C, [pend["nope"], ps], r2, 192, [vc.ap[:, 2:3], vc.ap[:, 3:4]], [o1, o2],
                      rope=[None, (rotB, cosB, sinB, 0)], gdeps=[vc])
            store_tile(C, qbT[0], (2 * h) * 128, t0, o1)
            store_tile(C, qbT[0], (2 * h + 1) * 128, t0, o2)
        linear(C, cqb, w_uq, 32, epi_q)
        norm_prep(C, ckv, gkv, ckvb, r3, dim=512)

        def epi_kv(j, ps, t0=t0):
            h, part = divmod(j, 2)
            if part == 0:
                ob = C.outb.next()
                head_norm(C, [ps], r3, 128, [vc.ap[:, 4:5]], [ob], gdeps=[vc])
                store_tile(C, kbnT[0], h * 128, t0, ob)
            else:
                epi_v_store(C, r3, vb[0], t0, lambda jj: jj // 2, C.identb)(j, ps)
        linear(C, ckvb, w_ukv, 32, epi_kv)


def body_B0(C, qaT, kaT, va, qbT, kbnT, vb, krT, dmask_d, maskc_d, visb_d, o0T):
    P = C.P
    NH = 16
    qa = qaT.rearrange("(h d) t -> h d t", d=128)
    ka = kaT.rearrange("(h d) t -> h d t", d=128)
    qb = qbT.rearrange("(h d) t -> h d t", d=256)
    kbn = kbnT.rearrange("(h d) t -> h d t", d=128)
    oT = o0T.rearrange("(h d) t -> h d t", d=128)
    onesb = C.sb("onesb", [128, 128], BF16)
    P.op("pool", lambda e: e.memset(onesb.ap[:], 1.0), writes=[onesb])
    zb = C.sb("zb", [128, 1], F32)
    P.op("pool", lambda e: e.memset(zb.ap[:], 0.0), writes=[zb])
    dmask = load_const(C, "dmask", dmask_d, [128, 21, 256], BF16)
    maskc = load_const(C, "maskc", maskc_d, [128, 4, 512], BF16)
    visb = load_const(C, "visb", visb_d, [128, 1], F32)
    kr = load_const(C, "kr", krT, [128, SEQ], BF16)
    psS = Rot([C.ps(f"pS{i}") for i in range(3)])
    psO = Rot([C.ps(f"pO{i}") for i in range(2)])
    psD = Rot([C.ps(f"pD{i}") for i in range(2)])
    qrot = Rot([C.sb(f"q{i}", [128, SEQ], BF16) for i in range(2)])
    krot = Rot([C.sb(f"k{i}", [128, SEQ], BF16) for i in range(2)])
    q2rot = Rot([C.sb(f"q2{i}", [128, SEQ], BF16) for i in range(2)])
    vrot = Rot([C.sb(f"v{i}", [128, 3, 16, 128], BF16) for i in range(2)])
    qd = [None, C.sb("qd1", [128, SEQ], BF16), C.sb("qd2", [128, SEQ], BF16)]
    kd = [None, C.sb("kd1", [128, SEQ], BF16), C.sb("kd2", [128, SEQ], BF16)]
    acc = C.sb("acc", [128, 2, SEQ], F32)
    rc = C.sb("rc", [128, SEQ], F32)
    obuf = Rot([C.sb(f"ob{i}", [128, SEQ], BF16) for i in range(2)])
    pTr = Rot([C.sb(f"pT{i}", [128, 512], BF16) for i in range(3)])
    rc5 = Rot([C.sb(f"rc5{i}", [128, 512], F32) for i in range(2)])
    sA = 128 ** -0.5
    sB = 192 ** -0.5
    mbase = {0: 0, 1: 16, 2: 20}
    for h in range(NH):
        q = qrot.next(); k = krot.next(); v3 = vrot.next()
        P.dma("sp", q.ap[:], qa[h], writes=[q])
        P.dma("sp", k.ap[:], ka[h], writes=[k])
        vh = va[:, h, :]
        P.dma("sp", v3.ap[:, 0], vh.rearrange("(i p) d -> p i d", p=128), writes=[v3])
        for i4 in range(4):
            P.dma("sp", v3.ap[:, 1, i4 * 4:(i4 + 1) * 4, :],
                  vh[i4 * 512:(i4 + 1) * 512, :].rearrange("(p r) d -> p r d", r=4), writes=[v3])
        P.dma("sp", v3.ap[:, 2], vh.rearrange("(p r) d -> p r d", r=16), writes=[v3])
        for br, dil in ((1, 4), (2, 16)):
            nb = 16 // dil
            P.op("pool", lambda e, br=br, dil=dil, nb=nb, q=q: e.tensor_copy(
                out=qd[br].ap.rearrange("p (r i j) -> p r i j", r=dil, i=nb, j=128),
                in_=q.ap.rearrange("p (i j r) -> p r i j", i=nb, j=128, r=dil)), reads=[q], writes=[qd[br]])
            P.op("dve", lambda e, br=br, dil=dil, nb=nb, k=k: e.tensor_copy(
                out=kd[br].ap.rearrange("p (r i j) -> p r i j", r=dil, i=nb, j=128),
                in_=k.ap.rearrange("p (i j r) -> p r i j", i=nb, j=128, r=dil)), reads=[k], writes=[kd[br]])
        for br, dil in ((0, 1), (1, 4), (2, 16)):
            nb = 16 // dil
            qq = q if br == 0 else qd[br]
            kk = k if br == 0 else kd[br]
            accv = acc.ap.rearrange("p a (i j r) -> p a r i j", i=nb, j=128, r=dil)
            for r in range(dil):
                for i in range(nb):
                    c0 = (r * nb + i) * 128
                    mt = mbase[br] + i
                    sp = psS.next()
                    if i >= 1:
                        P.op("pe", lambda e, sp=sp, kk=kk, qq=qq, c0=c0: e.matmul(
                            sp.ap[:, 0:128], lhsT=kk.ap[:, c0 - 128:c0], rhs=qq.ap[:, c0:c0 + 128],
                            start=True, stop=True), reads=[kk, qq], writes=[sp])
                    P.op("pe", lambda e, sp=sp, kk=kk, qq=qq, c0=c0: e.matmul(
                        sp.ap[:, 128:256], lhsT=kk.ap[:, c0:c0 + 128], rhs=qq.ap[:, c0:c0 + 128],
                        start=True, stop=True), reads=[kk, qq], writes=[sp])
                    lo = 0 if i >= 1 else 128
                    pT = pTr.next()
                    P.op("act", lambda e, sp=sp, pT=pT, lo=lo: e.activation(
                        out=pT.ap[:, lo:256], in_=sp.ap[:, lo:256], func=AF.Exp, scale=sA), reads=[sp], writes=[pT])
                    P.op("pool", lambda e, pT=pT, lo=lo, mt=mt: e.tensor_tensor(
                        out=pT.ap[:, lo:256], in0=pT.ap[:, lo:256], in1=dmask.ap[:, mt, lo:256], op=ALU.mult),
                        reads=[pT, dmask], writes=[pT])
                    po = psO.next()
                    vi = i * dil + r
                    if i >= 1:
                        P.op("pe", lambda e, po=po, pT=pT, v3=v3, br=br, vi=vi, dil=dil: e.matmul(
                            po.ap[:, 0:128], lhsT=v3.ap[:, br, vi - dil, :], rhs=pT.ap[:, 0:128],
                            start=True, stop=False), reads=[v3, pT], writes=[po])
                    P.op("pe", lambda e, po=po, pT=pT, v3=v3, br=br, vi=vi, i=i: e.matmul(
                        po.ap[:, 0:128], lhsT=v3.ap[:, br, vi, :], rhs=pT.ap[:, 128:256],
                        start=(i == 0), stop=True), reads=[v3, pT], writes=[po])
                    if i >= 1:
                        P.op("pe", lambda e, po=po, pT=pT: e.matmul(
                            po.ap[:, 128:256], lhsT=onesb.ap[:], rhs=pT.ap[:, 0:128],
                            start=True, stop=False), reads=[onesb, pT], writes=[po])
                    P.op("pe", lambda e, po=po, pT=pT, i=i: e.matmul(
                        po.ap[:, 128:256], lhsT=onesb.ap[:], rhs=pT.ap[:, 128:256],
                        start=(i == 0), stop=True), reads=[onesb, pT], writes=[po])
                    pov = po.ap[:, 0:256].rearrange("p (a j) -> p a j", a=2)
                    if br == 0:
                        P.op("dve", lambda e, pov=pov, accv=accv, r=r, i=i: e.tensor_copy(
                            out=accv[:, :, r, i, :], in_=pov), reads=[po], writes=[acc])
                    else:
                        P.op("dve", lambda e, pov=pov, accv=accv, r=r, i=i: e.tensor_tensor(
                            out=accv[:, :, r, i, :], in0=pov, in1=accv[:, :, r, i, :], op=ALU.add),
                            reads=[po, acc], writes=[acc])
        ob = obuf.next()
        P.op("dve", lambda e: e.reciprocal(out=rc.ap[:], in_=acc.ap[:, 1, :]), reads=[acc], writes=[rc])
        P.op("dve", lambda e, ob=ob: e.tensor_tensor(out=ob.ap[:], in0=acc.ap[:, 0, :], in1=rc.ap[:], op=ALU.mult),
             reads=[acc, rc], writes=[ob])
        P.dma("act", oT[h], ob.ap[:], reads=[ob])
    for h in range(NH):
        qn = qrot.next(); qr = q2rot.next(); kn = krot.next(); v3 = vrot.next()
        P.dma("sp", qn.ap[:], qb[h, 0:128, :], writes=[qn])
        P.dma("sp", qr.ap[:], qb[h, 128:256, :], writes=[qr])
        P.dma("sp", kn.ap[:], kbn[h], writes=[kn])
        P.dma("sp", v3.ap[:, 0], vb[:, h, :].rearrange("(i p) d -> p i d", p=128), writes=[v3])
        ob = obuf.next()
        for G in range(4):
            po = psO.next(); pd = psD.next()
            nkb = 4 * G + 4
            for kb in range(nkb):
                sp = psS.next()
                P.op("pe", lambda e, sp=sp, kn=kn, qn=qn, kb=kb, G=G: e.matmul(
                    sp.ap[:, :], lhsT=kn.ap[:, kb * 128:(kb + 1) * 128], rhs=qn.ap[:, G * 512:(G + 1) * 512],
                    start=True, stop=False), reads=[kn, qn], writes=[sp])
                P.op("pe", lambda e, sp=sp, qr=qr, kb=kb, G=G: e.matmul(
                    sp.ap[:, :], lhsT=kr.ap[:, kb * 128:(kb + 1) * 128], rhs=qr.ap[:, G * 512:(G + 1) * 512],
                    start=False, stop=True), reads=[kr, qr], writes=[sp])
                pT = pTr.next()
                bias = visb if (G >= 2 and kb < 8) else zb
                P.op("act", lambda e, sp=sp, pT=pT, bias=bias: e.activation(
                    out=pT.ap[:, :], in_=sp.ap[:, :], func=AF.Exp, bias=bias.ap[:, 0:1], scale=sB),
                    reads=[sp, bias], writes=[pT])
                if kb >= 4 * G:
                    P.op("pool", lambda e, pT=pT, o=kb - 4 * G: e.tensor_tensor(
                        out=pT.ap[:, :], in0=pT.ap[:, :], in1=maskc.ap[:, o, :], op=ALU.mult),
                        reads=[pT, maskc], writes=[pT])
                P.op("pe", lambda e, po=po, pT=pT, v3=v3, kb=kb, nkb=nkb: e.matmul(
                    po.ap[:, :], lhsT=v3.ap[:, 0, kb, :], rhs=pT.ap[:, :], start=(kb == 0), stop=(kb == nkb - 1)),
                    reads=[v3, pT], writes=[po])
                P.op("pe", lambda e, pd=pd, pT=pT, kb=kb, nkb=nkb: e.matmul(
                    pd.ap[:, :], lhsT=onesb.ap[:], rhs=pT.ap[:, :], start=(kb == 0), stop=(kb == nkb - 1)),
                    reads=[onesb, pT], writes=[pd])
            r5 = rc5.next()
            P.op("dve", lambda e, r5=r5, pd=pd: e.reciprocal(out=r5.ap[:], in_=pd.ap[:]), reads=[pd], writes=[r5])
            P.op("dve", lambda e, r5=r5, po=po, ob=ob, G=G: e.tensor_tensor(
                out=ob.ap[:, G * 512:(G + 1) * 512], in0=po.ap[:], in1=r5.ap[:], op=ALU.mult),
                reads=[po, r5], writes=[ob])
        P.dma("act", oT[NH + h], ob.ap[:], reads=[ob])


def body_C(C, layer, xT, oT, w, xo, ntok, extra):
    P = C.P
    C.setup_common()
    tok = C.tok
    gv = load_const(C, "gvec", w["gvec"], [128, 3, KT], F32)
    gx = Buf(gv.ap[:, 0, :], "gx"); gf = Buf(gv.ap[:, 1, :], "gf"); gn = Buf(gv.ap[:, 2, :], "gn")
    for b in (gx, gf, gn):
        b.last_w = gv.last_w
    gxq = load_const(C, "gxq", w["g_xq"], [128, 1], F32)
    memk = load_const(C, "memk", w["memk"].rearrange("(h d) m -> d h m", d=128), [128, 4, 256], BF16)
    memv = load_const(C, "memv", w["memv"].rearrange("(mb mp) h d -> mp mb h d", mp=128), [128, 2, 4, 128], BF16)
    C.pT = C.sb("pT", [128, 2 * tok], BF16)
    acc, acc_all = C.sbn("acc", KT, [128, tok], F32)
    hT, _ = C.sbn("hT", KT, [128, tok], BF16)
    hid, hid_all = C.sbn("hid", KT, [128, tok], BF16)
    kt4, _ = C.sbn("kt4", 4, [128, tok], BF16)
    if layer == 0:
        setup_vt(C, w["identb"])
        qkT, v1 = extra
    else:
        w_r = load_const(C, "w_r", w["w_router"], [128, KT, 8], F32)
        b_r = load_const(C, "b_r", w["b_router"], [8, 1], F32)
        ident = load_const(C, "ident", w["ident"], [128, 128], F32)
        sel = load_const(C, "sel", w["sel"], [8, 8, 128], F32)
        cse, _ = C.sbn("cse", 8, [128, tok], F32)
    for ch in range(ntok // tok):
        t0 = ch * tok
        P.dma("sp", acc_all.ap[:], xT[:, t0:t0 + tok].rearrange("(kc p) t -> p kc t", p=128), writes=acc + [acc_all])
        P.dma("sp", hid_all.ap[:], oT[:, t0:t0 + tok].rearrange("(kc p) t -> p kc t", p=128), writes=hid + [hid_all])
        linear(C, hid, w["w_o"], KT, epi_acc(C, acc))
        cross_attn(C, acc, gx, w["w_xq"], gxq, memk, memv, w["w_xo"], hT, kt4)
        if layer == 0:
            norm_prep(C, acc, gf, hT, C.rstd)
            blocks = [(w["w_gate"], w["w_up"], w["w_down"], 28, b * 3584, b * 3584, C.rstd) for b in range(4)]
            ffn_blocks(C, acc, hT, hid, blocks)
            norm_prep(C, acc, gn, hT, C.rstd)
            e_qk = epi_raw_store(C, C.rstd, qkT, t0, lambda j: j * 128)
            e_v = epi_v_store(C, C.rstd, v1, t0, lambda j: j - 64, C.identb)
            linear(C, hT, w["w_qkv"], 96, lambda j, ps: (e_qk if j < 64 else e_v)(j, ps))
            P.dma("act", xo[:, t0:t0 + tok].rearrange("(kc p) t -> p kc t", p=128), acc_all.ap[:], reads=acc + [acc_all])
        else:
            lg = C.ps_misc.next()
            norm_prep(C, acc, gf, hT, C.rstd, router=(w_r, lg))
            moe_gates(C, lg, C.rstd, b_r, ident, sel, cse)
            blocks = [(w["w_egate"][ex], w["w_eup"][ex], w["w_edown"][ex], KT, 0, 0, cse[ex]) for ex in range(8)]
            ffn_blocks(C, acc, hT, hid, blocks)
            P.dma("act", xo[:, t0:t0 + tok].rearrange("(kc p) t -> p kc t", p=128), acc_all.ap[:],
                  reads=acc + [acc_all], final=True)


def body_B1(C, qkT, v1, tri_d, masks_d, vmask_d, o1T):
    P = C.P
    NH = 32
    qd_ = qkT[0:D, :].rearrange("(h d) t -> h d t", d=128)
    kd_ = qkT[D:2 * D, :].rearrange("(h d) t -> h d t", d=128)
    oT = o1T.rearrange("(h d) t -> h d t", d=128)
    onesb = C.sb("onesb", [128, 128], BF16)
    P.op("pool", lambda e: e.memset(onesb.ap[:], 1.0), writes=[onesb])
    onef = C.sb("onef", [128, 1], F32)
    P.op("pool", lambda e: e.memset(onef.ap[:], 1.0), writes=[onef])
    tri = load_const(C, "tri", tri_d, [128, 128], BF16)
    masks = load_const(C, "masks", masks_d, [128, 4, 512], BF16)
    vmask = load_const(C, "vmask", vmask_d, [128, 512], BF16)
    psS = Rot([C.ps(f"pS{i}") for i in range(2)])
    psL = Rot([C.ps(f"pL{i}") for i in range(2)])
    psC = Rot([C.ps(f"pC{i}") for i in range(2)])
    psO = Rot([C.ps(f"pO{i}") for i in range(2)])
    qrot = Rot([C.sb(f"q{i}", [128, TOKC], BF16) for i in range(2)])
    krot = Rot([C.sb(f"k{i}", [128, SEQ], BF16) for i in range(2)])
    vrot = Rot([C.sb(f"v{i}", [128, 16, 128], BF16) for i in range(2)])
    obuf = Rot([C.sb(f"ob{i}", [128, TOKC], BF16) for i in range(2)])
    ezr = Rot([C.sb(f"ez{i}", [128, 512], F32) for i in range(2)])
    Lr = Rot([C.sb(f"L{i}", [128, 512], F32) for i in range(3)])
    nlr = Rot([C.sb(f"nl{i}", [128, 512], BF16) for i in range(3)])
    tr = Rot([C.sb(f"t{i}", [128, 512], F32) for i in range(3)])
    pTr = Rot([C.sb(f"pT{i}", [128, 512], BF16) for i in range(3)])
    carry = C.sb("carry", [128, 512], F32)
    s = 128 ** -0.5
    for h in range(NH):
        q = qrot.next(); k = krot.next(); v = vrot.next()
        P.dma("sp", q.ap[:], qd_[h][:, TOKC:T2], writes=[q])
        P.dma("sp", k.ap[:], kd_[h], writes=[k])
        P.dma("sp", v.ap[:], v1[:, h, :].rearrange("(i p) d -> p i d", p=128), writes=[v])
        ob = obuf.next()
        for G in (2, 3):
            g0 = (G - 2) * 512
            po = psO.next()
            nkb = 4 * G + 4
            for idx, kb in enumerate(reversed(range(nkb))):
                diag = kb >= 4 * G
                o = kb - 4 * G
                msk = (lambda o=o: masks.ap[:, o, :]) if diag else ((lambda: vmask.ap[:, :]) if kb < 8 else None)
                mbuf = masks if diag else vmask
                sp = psS.next()
                P.op("pe", lambda e, sp=sp, k=k, q=q, kb=kb, g0=g0: e.matmul(
                    sp.ap[:, :], lhsT=k.ap[:, kb * 128:(kb + 1) * 128], rhs=q.ap[:, g0:g0 + 512],
                    start=True, stop=True), reads=[k, q], writes=[sp])
                ez = ezr.next(); L = Lr.next(); nl = nlr.next(); t = tr.next(); pT = pTr.next()
                P.op("act", lambda e, sp=sp, ez=ez: e.activation(out=ez.ap[:], in_=sp.ap[:], func=AF.Exp, scale=-s),
                     reads=[sp], writes=[ez])
                P.op("act", lambda e, ez=ez, L=L: e.activation(out=L.ap[:], in_=ez.ap[:], func=AF.Ln,
                                                               bias=onef.ap[:, 0:1], scale=1.0),
                     reads=[ez, onef], writes=[L])
                P.op("dve", lambda e, sp=sp, L=L, nl=nl: e.scalar_tensor_tensor(
                    out=nl.ap[:], in0=sp.ap[:], scalar=s, in1=L.ap[:], op0=ALU.mult, op1=ALU.add),
                    reads=[sp, L], writes=[nl])
                if msk is not None:
                    P.op("pool", lambda e, nl=nl, msk=msk: e.tensor_tensor(out=nl.ap[:], in0=nl.ap[:], in1=msk(),
                                                                          op=ALU.mult), reads=[nl, mbuf], writes=[nl])
                pl = psL.next(); pc = psC.next()
                P.op("pe", lambda e, pl=pl, nl=nl: e.matmul(pl.ap[:], lhsT=tri.ap[:], rhs=nl.ap[:],
                                                            start=True, stop=True), reads=[tri, nl], writes=[pl])
                P.op("pe", lambda e, pc=pc, nl=nl: e.matmul(pc.ap[:], lhsT=onesb.ap[:], rhs=nl.ap[:],
                                                            start=True, stop=True), reads=[onesb, nl], writes=[pc])
                P.op("dve", lambda e, pl=pl, L=L, t=t: e.tensor_tensor(out=t.ap[:], in0=pl.ap[:], in1=L.ap[:],
                                                                       op=ALU.add), reads=[pl, L], writes=[t])
                if idx > 0:
                    P.op("pool", lambda e, t=t: e.tensor_tensor(out=t.ap[:], in0=t.ap[:], in1=carry.ap[:],
                                                                op=ALU.add), reads=[t, carry], writes=[t])
                P.op("act", lambda e, t=t, pT=pT: e.activation(out=pT.ap[:], in_=t.ap[:], func=AF.Exp, scale=-1.0),
                     reads=[t], writes=[pT])
                if msk is not None:
                    P.op("pool", lambda e, pT=pT, msk=msk: e.tensor_tensor(out=pT.ap[:], in0=pT.ap[:], in1=msk(),
                                                                          op=ALU.mult), reads=[pT, mbuf], writes=[pT])
                P.op("pe", lambda e, po=po, pT=pT, v=v, kb=kb, idx=idx, nkb=nkb: e.matmul(
                    po.ap[:], lhsT=v.ap[:, kb, :], rhs=pT.ap[:], start=(idx == 0), stop=(idx == nkb - 1)),
                    reads=[v, pT], writes=[po])
                if idx < nkb - 1:
                    if idx == 0:
                        P.op("dve", lambda e, pc=pc: e.tensor_copy(out=carry.ap[:], in_=pc.ap[:]),
                             reads=[pc], writes=[carry])
                    else:
                        P.op("dve", lambda e, pc=pc: e.tensor_tensor(out=carry.ap[:], in0=pc.ap[:], in1=carry.ap[:],
                                                                     op=ALU.add), reads=[pc, carry], writes=[carry])
            P.op("dve", lambda e, po=po, ob=ob, g0=g0: e.tensor_copy(out=ob.ap[:, g0:g0 + 512], in_=po.ap[:]),
                 reads=[po], writes=[ob])
        P.dma("act", oT[h], ob.ap[:], reads=[ob])


def build_fused():
    C = Ctx(TOK)
    nc = C.nc
    P = C.P
    i_ = C.dram_in

    def scr(name, shape, dt):
        ap = nc.dram_tensor(name, list(shape), dt, kind="Internal").ap()
        return ap, Buf(ap, name)
    xT = i_("xT", [D, T2], F32)
    posb = i_("posb", [128, T2], I32)
    memT = i_("memT", [D, 256], F32)
    g_mem = i_("g_mem", [128, KT], F32)
    g_mk = i_("g_mk", [128, 1], F32)
    w_mem_kv = i_("w_mem_kv", [D, 1024], F32)
    w_in = i_("w_in", [D, 8320], F32)
    w_uq = i_("w_uq", [1536, 4096], F32)
    w_ukv = i_("w_ukv", [512, 4096], F32)
    gmix = i_("g_mix0", [128, KT], F32)
    gcq = i_("g_cq", [128, 12], F32)
    gckv = i_("g_ckv", [128, 4], F32)
    vecs = i_("vecs", [128, 8], F32)
    rots = i_("rots", [128, 2, 128], F32)
    identb = i_("identb", [128, 128], BF16)
    ident = i_("ident", [128, 128], F32)
    sel = i_("sel", [8, 8, 128], F32)
    dmask = i_("dmask", [128, 21, 256], BF16)
    maskc = i_("maskc", [128, 4, 512], BF16)
    visb = i_("visb", [128, 1], F32)
    tri = i_("tri", [128, 128], BF16)
    masks = i_("masks", [128, 4, 512], BF16)
    vmask = i_("vmask", [128, 512], BF16)
    wl = []
    for L in range(2):
        wl.append({"gvec": i_(f"gvec{L}", [128, 3, KT], F32), "g_xq": i_(f"g_xq{L}", [128, 1], F32),
                   "w_xq": i_(f"w_xq{L}", [D, 512], F32), "w_xo": i_(f"w_xo{L}", [512, D], F32),
                   "w_o": i_(f"w_o{L}", [D, D], F32), "identb": identb, "ident": ident, "sel": sel})
    wl[0].update({"w_gate": i_("w_gate", [D, 14336], F32), "w_up": i_("w_up", [D, 14336], F32),
                  "w_down": i_("w_down", [14336, D], F32), "w_qkv": i_("w_qkv", [D, 3 * D], F32)})
    wl[1].update({"w_egate": i_("w_egate", [8, D, D], F32), "w_eup": i_("w_eup", [8, D, D], F32),
                  "w_edown": i_("w_edown", [8, D, D], F32), "w_router": i_("w_router", [128, KT, 8], F32),
                  "b_router": i_("b_router", [8, 1], F32)})
    out = C.dram_out("xoT", [D, TOKC], F32)
    tabs = [scr(f"tab{i}", [128, T2], F32) for i in range(4)]
    mkT = scr("mkT", [512, 256], BF16)
    mv = scr("mv", [256, 4, 128], BF16)
    qaT = scr("qaT", [2048, T2], BF16); kaT = scr("kaT", [2048, T2], BF16); va = scr("va", [T2, 16, 128], BF16)
    qbT = scr("qbT", [4096, T2], BF16); kbnT = scr("kbnT", [2048, T2], BF16); vb = scr("vb", [T2, 16, 128], BF16)
    krT = scr("krT", [128, T2], BF16)
    o0T = scr("o0T", [D, T2], BF16)
    x3T = scr("x3T", [D, T2], F32)
    qkT = scr("qkT", [2 * D, T2], BF16)
    v1 = scr("v1", [T2, 32, 128], BF16)
    o1T = scr("o1T", [D, TOKC], BF16)
    for L in range(2):
        wl[L]["memk"] = mkT[0]
        wl[L]["memv"] = mv[0]

    def phase(tok, fn):
        with contextlib.ExitStack() as es:
            C.es = es
            C.tok = tok
            C.n += 1
            if hasattr(C, "_mg"):
                del C._mg
            fn()
        P.fence()
    phase(TOK, lambda: body_rope(C, posb, vecs, tabs))
    phase(128, lambda: body_P2(C, memT, g_mem, g_mk, w_mem_kv, identb, mkT, mv))
    phase(TOK, lambda: body_A0(C, xT, tabs, w_in, w_uq, w_ukv, gmix, gcq, gckv, vecs, rots, identb,
                               (qaT, kaT, va, qbT, kbnT, vb, krT)))
    phase(512, lambda: body_B0(C, qaT[0], kaT[0], va[0], qbT[0], kbnT[0], vb[0], krT[0], dmask, maskc, visb, o0T[0]))
    phase(TOK, lambda: body_C(C, 0, xT, o0T[0], wl[0], x3T[0], T2, (qkT[0], v1[0])))
    phase(512, lambda: body_B1(C, qkT[0], v1[0], tri, masks, vmask, o1T[0]))
    phase(TOK, lambda: body_C(C, 1, x3T[0][:, TOKC:T2], o1T[0], wl[1], out, TOKC, None))
    P.emit()
    return nc


def dil_mask_table(vis):
    kk = np.arange(128)[:, None]
    qq = np.arange(128)[None, :]
    tiles = []
    for dil in (1, 4, 16):
        nb = 16 // dil
        hd = 1024 // dil
        for i in range(nb):
            nq = 128 * i + qq
            out = []
            for which in (0, 1):
                nk = 128 * (i - 1 + which) + kk
                band = (kk >= qq) if which == 0 else (kk <= qq)
                visible = (nq < hd) | (nk >= hd) | bool(vis)
                out.append(band & visible & (nk >= 0))
            tiles.append(np.concatenate(out, axis=1))
    return np.ascontiguousarray(np.stack(tiles, axis=1).astype(NPBF))


def prep_fused(inp):
    x = inp["x"]
    pos = inp["positions"].astype(np.int32)
    mem = inp["mem"]
    w_in = np.concatenate([inp["w_in"][0], np.zeros((D, 64), np.float32)], axis=1)
    wq = inp["w_uq"][0].reshape(1536, 16, 192)
    w_uq = np.concatenate([wq, np.zeros((1536, 16, 64), np.float32)], axis=2).reshape(1536, 4096)
    invA, invB, rots = rope_consts()
    gbq = inp["gb_q"][0]
    vecs = np.stack([inp["ga_q"][0], inp["ga_k"][0], gbq[:128], pad128(gbq[128:]), inp["gb_kn"][0],
                     pad128(inp["gb_kr"][0]), invA, invB], axis=1).astype(np.float32)
    _, maskc, masks, tri = attn_masks()
    sel = np.zeros((8, 8, 128), np.float32)
    for e in range(8):
        sel[e, e, :] = 1.0
    shared = {"g_mem": cols(inp["g_mem"], 32), "g_mk": np.ascontiguousarray(inp["g_mem_k"].reshape(128, 1)),
              "w_mem_kv": inp["w_mem_kv"], "w_in": w_in, "w_uq": w_uq, "w_ukv": np.ascontiguousarray(inp["w_ukv"][0]),
              "g_mix0": cols(inp["g_mix"][0], 32), "g_cq": cols(inp["g_cq"][0], 12), "g_ckv": cols(inp["g_ckv"][0], 4),
              "vecs": np.ascontiguousarray(vecs), "rots": rots, "identb": np.eye(128, dtype=np.float32).astype(NPBF),
              "ident": np.eye(128, dtype=np.float32), "sel": sel, "maskc": maskc, "tri": tri, "masks": masks,
              "w_gate": inp["w_gate"][0], "w_up": inp["w_up"][0], "w_down": inp["w_down"][0], "w_qkv": inp["w_qkv"][0],
              "w_egate": inp["w_egate"][0], "w_eup": inp["w_eup"][0], "w_edown": inp["w_edown"][0],
              "w_router": np.ascontiguousarray(inp["w_router"][0].reshape(32, 128, 8).transpose(1, 0, 2)),
              "b_router": np.ascontiguousarray(inp["b_router"][0].reshape(8, 1))}
    for L in range(2):
        shared[f"gvec{L}"] = np.ascontiguousarray(np.stack(
            [cols(inp["g_x"][L], 32), cols(inp["g_ffn"][L], 32), cols(inp["g_mix"][1], 32)], axis=1))
        shared[f"g_xq{L}"] = np.ascontiguousarray(inp["g_xq"][L].reshape(128, 1))
        shared[f"w_xq{L}"] = np.ascontiguousarray(inp["w_xq"][L])
        shared[f"w_xo{L}"] = np.ascontiguousarray(inp["w_xo"][L])
    shared["w_o0"] = inp["w_o_even"][0]
    shared["w_o1"] = inp["w_o_odd"][0]
    dm = [dil_mask_table(0), dil_mask_table(1)]
    ins = []
    for c in range(NCORES):
        b, j = divmod(c, 2)
        own = slice(j * TOKC, (j + 1) * TOKC)
        oth = slice((1 - j) * TOKC, (2 - j) * TOKC)
        m = dict(shared)
        xb = x[b]
        m["xT"] = np.ascontiguousarray(np.concatenate([xb[oth], xb[own]], axis=0).T)
        pb = np.concatenate([pos[b, oth], pos[b, own]])
        m["posb"] = np.ascontiguousarray(np.broadcast_to(pb[None, :], (128, T2)))
        m["memT"] = np.ascontiguousarray(mem[b].T)
        m["dmask"] = dm[j]
        m["visb"] = np.full((128, 1), 0.0 if j == 1 else -30000.0, np.float32)
        m["vmask"] = (np.ones((128, 512), np.float32) if j == 1 else np.zeros((128, 512), np.float32)).astype(NPBF)
        ins.append(m)
    return ins


def kernel_unfused(**inp):
    return _kernel_unfused(inp)


def kernel(**inp):
    inp = {k: np.asarray(v) for k, v in inp.items()}
    res = run(build_fused(), prep_fused(inp))
    out = np.zeros((4, SEQ, D), np.float32)
    for c, r in enumerate(res):
        b, j = divmod(c, 2)
        out[b, j * TOKC:(j + 1) * TOKC, :] = np.asarray(r["xoT"]).T
    return out
```

```python
import contextlib
import math
import numpy as np
import ml_dtypes
import concourse.bass as bass
import concourse.mybir as mybir
from concourse.bass_utils import run_bass_kernel_spmd

F32 = mybir.dt.float32
BF16 = mybir.dt.bfloat16
I32 = mybir.dt.int32
AF = mybir.ActivationFunctionType
ALU = mybir.AluOpType
AX = mybir.AxisListType
NPBF = ml_dtypes.bfloat16

EPS = 1e-6
NCORES = 8
D = 4096
KT = 32
SEQ = 2048
TOKC = 1024
TOK = 256
DMA_ROT = 8


class Buf:
    __slots__ = ("ap", "name", "last_w", "readers")

    def __init__(self, ap, name=""):
        self.ap = ap
        self.name = name
        self.last_w = None
        self.readers = []


class Op:
    __slots__ = ("eng", "fn", "deps", "signal", "val", "dma_sem", "dma_val", "is_dma")

    def __init__(self, eng, fn):
        self.eng = eng
        self.fn = fn
        self.deps = []
        self.signal = False
        self.val = None
        self.is_dma = False
        self.dma_sem = None
        self.dma_val = None


class Prog:
    ENGS = ("pe", "act", "dve", "pool", "sp")

    def __init__(self, nc):
        self.nc = nc
        self.ops = {e: [] for e in self.ENGS}
        self.dma_count = {e: 0 for e in self.ENGS}
        self.final_dma = []
        self.fence_ops = []
        self.fenced = set()

    def fence(self):
        f = []
        for e in self.ENGS:
            comp = [o for o in self.ops[e] if not o.is_dma]
            if comp:
                f.append(comp[-1])
            f.extend([o for o in self.ops[e] if o.is_dma][-DMA_ROT:])
        self.fence_ops = f
        self.fenced = set()

    def _add(self, eng, fn, reads, writes, is_dma=False):
        op = Op(eng, fn)
        op.is_dma = is_dma
        deps = []
        if self.fence_ops and eng not in self.fenced:
            self.fenced.add(eng)
            deps.extend(self.fence_ops)
        for b in reads:
            if b.last_w is not None:
                deps.append(b.last_w)
        for b in writes:
            if b.last_w is not None:
                deps.append(b.last_w)
            last = {}
            for r in b.readers:
                if r.is_dma:
                    deps.append(r)
                else:
                    last[r.eng] = r
            deps.extend(last.values())
        if is_dma:
            k = self.dma_count[eng]
            self.dma_count[eng] += 1
            op.dma_sem = (eng, k % DMA_ROT)
            op.dma_val = 16 * (k // DMA_ROT + 1)
        seen = set()
        for d in deps:
            if id(d) in seen or d is op:
                continue
            seen.add(id(d))
            op.deps.append(d)
        for b in writes:
            b.last_w = op
            b.readers = []
        for b in reads:
            if b.last_w is op:
                continue
            b.readers.append(op)
        self.ops[eng].append(op)
        return op

    def op(self, eng, fn, reads=(), writes=()):
        return self._add(eng, fn, list(reads), list(writes))

    def dma(self, eng, out_ap, in_ap, reads=(), writes=(), final=False):
        def fn(e, out_ap=out_ap, in_ap=in_ap):
            return e.dma_start(out=out_ap, in_=in_ap)
        op = self._add(eng, fn, list(reads), list(writes), is_dma=True)
        if final:
            self.final_dma.append(op)
        return op

    def emit(self):
        nc = self.nc
        es = contextlib.ExitStack()
        with es:
            eng_sem = {e: es.enter_context(nc.semaphore(f"es_{e}")) for e in self.ENGS}
            dma_sems = {}
            for e in self.ENGS:
                for r in range(min(DMA_ROT, self.dma_count[e])):
                    dma_sems[(e, r)] = es.enter_context(nc.semaphore(f"ds_{e}{r}"))
            for e in self.ENGS:
                for op in self.ops[e]:
                    for d in op.deps:
                        if d.is_dma:
                            continue
                        if d.eng == op.eng and not op.is_dma and d.eng == "pe":
                            continue
                        d.signal = True
            for e in self.ENGS:
                v = 0
                for op in self.ops[e]:
                    if op.is_dma:
                        continue
                    if op.signal:
                        v += 1
                        op.val = v
            prog = self

            def run_engine(ename, e):
                seen = {}
                for op in prog.ops[ename]:
                    waits = {}
                    for d in op.deps:
                        if d.is_dma:
                            key = ("d",) + d.dma_sem
                            val = d.dma_val
                        else:
                            if d.eng == ename and not op.is_dma and ename == "pe":
                                continue
                            key = ("e", d.eng)
                            val = d.val
                        if waits.get(key, 0) < val:
                            waits[key] = val
                    if op.is_dma and op.dma_val > 16:
                        key = ("d",) + op.dma_sem
                        pv = op.dma_val - 16
                        if waits.get(key, 0) < pv:
                            waits[key] = pv
                    for key, val in waits.items():
                        if seen.get(key, 0) >= val:
                            continue
                        seen[key] = val
                        sem = eng_sem[key[1]] if key[0] == "e" else dma_sems[(key[1], key[2])]
                        e.wait_ge(sem, val)
                    ins = op.fn(e)
                    if op.is_dma:
                        ins.then_inc(dma_sems[op.dma_sem], 16)
                    elif op.signal:
                        ins.then_inc(eng_sem[ename], 1)
                for op in prog.final_dma:
                    if op.eng == ename:
                        key = ("d",) + op.dma_sem
                        if seen.get(key, 0) < op.dma_val:
                            seen[key] = op.dma_val
                            e.wait_ge(dma_sems[op.dma_sem], op.dma_val)

            with nc.Block() as block:
                @block.tensor
                def _(e):
                    run_engine("pe", e)

                @block.scalar
                def _(e):
                    run_engine("act", e)

                @block.vector
                def _(e):
                    run_engine("dve", e)

                @block.gpsimd
                def _(e):
                    run_engine("pool", e)

                @block.sync
                def _(e):
                    run_engine("sp", e)
        return nc


class Rot:
    def __init__(self, bufs):
        self.bufs = bufs
        self.i = 0

    def next(self):
        b = self.bufs[self.i % len(self.bufs)]
        self.i += 1
        return b


class Ctx:
    def __init__(self, tok):
        self.nc = bass.Bass("TRN2", target_bir_lowering=False)
        self.es = contextlib.ExitStack()
        self.P = Prog(self.nc)
        self.tok = tok
        self.n = 0

    def dram_in(self, name, shape, dt):
        return self.nc.dram_tensor(name, list(shape), dt, kind="ExternalInput").ap()

    def dram_out(self, name, shape, dt):
        return self.nc.dram_tensor(name, list(shape), dt, kind="ExternalOutput").ap()

    def sb(self, name, shape, dt):
        t = self.es.enter_context(self.nc.sbuf_tensor(f"s{self.n}_" + name, list(shape), dt))
        return Buf(t[:], name)

    def sbn(self, name, n, shape, dt):
        t = self.es.enter_context(self.nc.sbuf_tensor(f"s{self.n}_" + name, [shape[0], n] + list(shape[1:]), dt))
        return [Buf(t[:, i], f"{name}{i}") for i in range(n)], Buf(t[:], name)

    def ps(self, name, shape=(128, 512), dt=F32):
        t = self.es.enter_context(self.nc.psum_tensor(f"p{self.n}_" + name, list(shape), dt))
        return Buf(t[:], name)

    def setup_common(self):
        P = self.P
        tok = self.tok
        self.ones = self.sb("ones", [128, 128], F32)
        P.op("pool", lambda e: e.memset(self.ones.ap[:], 1.0), writes=[self.ones])
        self.onesb = self.sb("onesb", [128, 128], BF16)
        P.op("pool", lambda e: e.memset(self.onesb.ap[:], 1.0), writes=[self.onesb])
        self.wrot = Rot([self.sb(f"wb{i}", [128, KT, 512], BF16) for i in range(2)])
        self.psrot = Rot([self.ps(f"pm{i}") for i in range(4)])
        self.ps_stat = Rot([self.ps(f"pst{i}") for i in range(2)])
        self.ps_misc = Rot([self.ps(f"pmi{i}") for i in range(2)])
        self.tmp = Rot([self.sb(f"tmp{i}", [128, tok], F32) for i in range(6)])
        self.sqt = Rot([self.sb(f"sq{i}", [128, tok], F32) for i in range(3)])
        self.outb = Rot([self.sb(f"ob{i}", [128, tok], BF16) for i in range(4)])
        self.rstd = self.sb("rstd", [128, tok], F32)


def inv_rms(C, ss_ps, dim, out):
    tok = C.tok
    C.P.op("dve", lambda e: e.tensor_scalar(out=out.ap[:, :tok], in0=ss_ps.ap[:, :tok], scalar1=1.0 / dim,
                                            scalar2=EPS, op0=ALU.mult, op1=ALU.add),
           reads=[ss_ps], writes=[out])
    C.P.op("act", lambda e: e.activation(out=out.ap[:, :tok], in_=out.ap[:, :tok], func=AF.Sqrt),
           reads=[out], writes=[out])
    C.P.op("dve", lambda e: e.reciprocal(out=out.ap[:, :tok], in_=out.ap[:, :tok]),
           reads=[out], writes=[out])


def norm_prep(C, src, g, hT, rstd, dim=None, router=None):
    P = C.P
    tok = C.tok
    n = len(src)
    dim = dim or 128 * n
    ss = C.ps_stat.next()
    for k in range(n):
        sq = C.sqt.next()
        P.op("act", lambda e, sq=sq, k=k: e.activation(out=sq.ap[:, :tok], in_=src[k].ap[:, :tok], func=AF.Square),
             reads=[src[k]], writes=[sq])
        P.op("pe", lambda e, sq=sq, k=k: e.matmul(ss.ap[:, :tok], lhsT=C.ones.ap[:], rhs=sq.ap[:, :tok],
                                                   start=(k == 0), stop=(k == n - 1)),
             reads=[sq, C.ones], writes=[ss])
        P.op("dve", lambda e, k=k: e.tensor_scalar(out=hT[k].ap[:, :tok], in0=src[k].ap[:, :tok],
                                                   scalar1=g.ap[:, k:k + 1], scalar2=None, op0=ALU.mult),
             reads=[src[k], g], writes=[hT[k]])
        if router is not None:
            w_r, lg = router
            xg = C.tmp.next()
            P.op("pool", lambda e, k=k, xg=xg: e.tensor_scalar(out=xg.ap[:, :tok], in0=src[k].ap[:, :tok],
                                                               scalar1=g.ap[:, k:k + 1], scalar2=None, op0=ALU.mult),
                 reads=[src[k], g], writes=[xg])
            P.op("pe", lambda e, k=k, xg=xg: e.matmul(lg.ap[0:8, :tok], lhsT=w_r.ap[:, k, :], rhs=xg.ap[:, :tok],
                                                      start=(k == 0), stop=(k == n - 1)),
                 reads=[xg, w_r], writes=[lg])
    inv_rms(C, ss, dim, rstd)


def linear(C, kt, Wd, n_tiles, epi, col0=0):
    P = C.P
    tok = C.tok
    nk = len(kt)
    j = 0
    while j < n_tiles:
        w = min(4, n_tiles - j)
        wb = C.wrot.next()
        P.dma("pool", wb.ap[:, 0:nk, 0:w * 128],
              Wd[0:nk * 128, col0 + j * 128: col0 + (j + w) * 128].rearrange("(kc p) n -> p kc n", p=128),
              writes=[wb])
        for jj in range(w):
            ps = C.psrot.next()
            for k in range(nk):
                P.op("pe", lambda e, wb=wb, jj=jj, k=k, ps=ps: e.matmul(
                    ps.ap[:, :tok], lhsT=wb.ap[:, k, jj * 128:(jj + 1) * 128], rhs=kt[k].ap[:, :tok],
                    start=(k == 0), stop=(k == nk - 1)), reads=[wb, kt[k]], writes=[ps])
            epi(j + jj, ps)
        j += w


def store_tile(C, out_dram, row0, t0, src, final=False):
    tok = C.tok
    C.P.dma("act", out_dram[row0:row0 + 128, t0:t0 + tok], src.ap[:, :tok], reads=[src], final=final)


def epi_v_store(C, cs, vdram, t0, head_of, identb):
    P = C.P
    tok = C.tok

    def epi(j, ps):
        ob = C.outb.next()
        P.op("dve", lambda e: e.tensor_tensor(out=ob.ap[:, :tok], in0=ps.ap[:, :tok], in1=cs.ap[:, :tok], op=ALU.mult),
             reads=[ps, cs], writes=[ob])
        for sub in range(tok // 128):
            pt = C.ps_misc.next()
            P.op("pe", lambda e, pt=pt, sub=sub: e.matmul(pt.ap[:, 0:128], lhsT=ob.ap[:, sub * 128:(sub + 1) * 128],
                                                          rhs=identb.ap[:], start=True, stop=True),
                 reads=[ob, identb], writes=[pt])
            vt = C.vtrot.next()
            P.op("act", lambda e, pt=pt, vt=vt: e.activation(out=vt.ap[:], in_=pt.ap[:, 0:128], func=AF.Copy),
                 reads=[pt], writes=[vt])
            P.dma("act", vdram[t0 + sub * 128:t0 + (sub + 1) * 128, head_of(j), :], vt.ap[:], reads=[vt])
    return epi


def setup_vt(C, ident_d):
    C.identb = load_const(C, "identb", ident_d, [128, 128], BF16)
    C.vtrot = Rot([C.sb(f"vt{i}", [128, 128], BF16) for i in range(3)])


def epi_raw_store(C, cs, out_dram, t0, row_of):
    P = C.P
    tok = C.tok

    def epi(j, ps):
        ob = C.outb.next()
        P.op("dve", lambda e: e.tensor_tensor(out=ob.ap[:, :tok], in0=ps.ap[:, :tok], in1=cs.ap[:, :tok], op=ALU.mult),
             reads=[ps, cs], writes=[ob])
        store_tile(C, out_dram, row_of(j), t0, ob)
    return epi


def head_norm(C, parts, cs, dim, gcols, outs, rope=None, gdeps=()):
    P = C.P
    tok = C.tok
    ys = []
    ss = C.ps_stat.next()
    for i, ps in enumerate(parts):
        y = C.tmp.next()
        P.op("dve", lambda e, y=y, ps=ps: e.tensor_tensor(out=y.ap[:, :tok], in0=ps.ap[:, :tok], in1=cs.ap[:, :tok],
                                                          op=ALU.mult), reads=[ps, cs], writes=[y])
        sq = C.sqt.next()
        P.op("act", lambda e, y=y, sq=sq: e.activation(out=sq.ap[:, :tok], in_=y.ap[:, :tok], func=AF.Square),
             reads=[y], writes=[sq])
        P.op("pe", lambda e, sq=sq, i=i: e.matmul(ss.ap[:, :tok], lhsT=C.ones.ap[:], rhs=sq.ap[:, :tok],
                                                   start=(i == 0), stop=(i == len(parts) - 1)),
             reads=[sq, C.ones], writes=[ss])
        ys.append(y)
    r = C.tmp.next()
    inv_rms(C, ss, dim, r)
    for i, y in enumerate(ys):
        rp = rope[i] if rope else None
        if rp is None:
            P.op("dve", lambda e, y=y, i=i: e.scalar_tensor_tensor(
                out=outs[i].ap[:, :tok], in0=y.ap[:, :tok], scalar=gcols[i], in1=r.ap[:, :tok],
                op0=ALU.mult, op1=ALU.mult), reads=[y, r] + list(gdeps), writes=[outs[i]])
        else:
            rotT, cos, sin, t0 = rp
            P.op("dve", lambda e, y=y, i=i: e.scalar_tensor_tensor(
                out=y.ap[:, :tok], in0=y.ap[:, :tok], scalar=gcols[i], in1=r.ap[:, :tok],
                op0=ALU.mult, op1=ALU.mult), reads=[y, r] + list(gdeps), writes=[y])
            pr = C.ps_misc.next()
            P.op("pe", lambda e, y=y, pr=pr: e.matmul(pr.ap[:, :tok], lhsT=rotT.ap[:], rhs=y.ap[:, :tok],
                                                      start=True, stop=True), reads=[y, rotT], writes=[pr])
            t1 = C.tmp.next()
            P.op("pool", lambda e, y=y, t1=t1: e.tensor_tensor(out=t1.ap[:, :tok], in0=y.ap[:, :tok],
                                                               in1=cos.ap[:, t0:t0 + tok], op=ALU.mult),
                 reads=[y, cos], writes=[t1])
            t2 = C.tmp.next()
            P.op("dve", lambda e, pr=pr, t2=t2: e.tensor_tensor(out=t2.ap[:, :tok], in0=pr.ap[:, :tok],
                                                                in1=sin.ap[:, t0:t0 + tok], op=ALU.mult),
                 reads=[pr, sin], writes=[t2])
            P.op("pool", lambda e, t1=t1, t2=t2, i=i: e.tensor_tensor(out=outs[i].ap[:, :tok], in0=t1.ap[:, :tok],
                                                                      in1=t2.ap[:, :tok], op=ALU.add),
                 reads=[t1, t2], writes=[outs[i]])


def rope_tables(C, pos_d, invf, ntok, name, gdeps=()):
    P = C.P
    posi = C.sb(name + "_pi", [128, ntok], I32)
    P.dma("sp", posi.ap[:], pos_d, writes=[posi])
    ang = C.sb(name + "_ang", [128, ntok], F32)
    P.op("dve", lambda e: e.tensor_copy(out=ang.ap[:], in_=posi.ap[:]), reads=[posi], writes=[ang])
    P.op("dve", lambda e: e.tensor_scalar(out=ang.ap[:], in0=ang.ap[:], scalar1=invf, scalar2=None, op0=ALU.mult),
         reads=[ang] + list(gdeps), writes=[ang])
    cos = C.sb(name + "_cos", [128, ntok], F32)
    sin = C.sb(name + "_sin", [128, ntok], F32)
    two_pi = 2.0 * math.pi
    sc = 1.0 - 1e-6
    ti = C.sb(name + "_ti", [128, ntok], I32)
    tf = C.sb(name + "_tf", [128, ntok], F32)
    mk = C.sb(name + "_mk", [128, ntok], F32)
    C1 = 6.28125
    C2 = 2.0 * math.pi - C1

    def reduced_sin(dst, shift):
        P.op("dve", lambda e: e.tensor_scalar(out=dst.ap[:], in0=ang.ap[:], scalar1=shift, scalar2=None, op0=ALU.add),
             reads=[ang], writes=[dst])
        P.op("dve", lambda e: e.tensor_scalar(out=tf.ap[:], in0=dst.ap[:], scalar1=1.0 / two_pi, scalar2=None,
                                              op0=ALU.mult), reads=[dst], writes=[tf])
        P.op("dve", lambda e: e.tensor_copy(out=ti.ap[:], in_=tf.ap[:]), reads=[tf], writes=[ti])
        P.op("dve", lambda e: e.tensor_copy(out=tf.ap[:], in_=ti.ap[:]), reads=[ti], writes=[tf])
        P.op("dve", lambda e: e.scalar_tensor_tensor(out=dst.ap[:], in0=tf.ap[:], scalar=-C1, in1=dst.ap[:],
                                                     op0=ALU.mult, op1=ALU.add), reads=[tf, dst], writes=[dst])
        P.op("dve", lambda e: e.scalar_tensor_tensor(out=dst.ap[:], in0=tf.ap[:], scalar=-C2, in1=dst.ap[:],
                                                     op0=ALU.mult, op1=ALU.add), reads=[tf, dst], writes=[dst])
        P.op("dve", lambda e: e.tensor_scalar(out=mk.ap[:], in0=dst.ap[:], scalar1=math.pi, scalar2=None,
                                              op0=ALU.is_gt), reads=[dst], writes=[mk])
        P.op("dve", lambda e: e.scalar_tensor_tensor(out=dst.ap[:], in0=mk.ap[:], scalar=-two_pi, in1=dst.ap[:],
                                                     op0=ALU.mult, op1=ALU.add), reads=[mk, dst], writes=[dst])
        P.op("dve", lambda e: e.tensor_scalar(out=mk.ap[:], in0=dst.ap[:], scalar1=-math.pi, scalar2=None,
                                              op0=ALU.is_lt), reads=[dst], writes=[mk])
        P.op("dve", lambda e: e.scalar_tensor_tensor(out=dst.ap[:], in0=mk.ap[:], scalar=two_pi, in1=dst.ap[:],
                                                     op0=ALU.mult, op1=ALU.add), reads=[mk, dst], writes=[dst])
        P.op("act", lambda e: e.activation(out=dst.ap[:], in_=dst.ap[:], func=AF.Sin, scale=sc),
             reads=[dst], writes=[dst])
    reduced_sin(sin, 0.0)
    reduced_sin(cos, 0.5 * math.pi)
    return cos, sin


def load_const(C, name, dram_ap, shape, dt, eng="sp"):
    b = C.sb(name, shape, dt)
    C.P.dma(eng, b.ap[:], dram_ap, writes=[b])
    return b


def build_P():
    C = Ctx(128)
    P = C.P
    memT = C.dram_in("memT", [D, 128], F32)
    g_mem = C.dram_in("g_mem", [128, KT], F32)
    g_mk = C.dram_in("g_mk", [128, 1], F32)
    w = C.dram_in("w_mem_kv", [D, 1024], F32)
    out = C.dram_out("mkvT", [1024, 128], BF16)
    with C.es:
        C.setup_common()
        tok = C.tok
        gm = load_const(C, "gm", g_mem, [128, KT], F32)
        gk = load_const(C, "gk", g_mk, [128, 1], F32)
        acc, acc_all = C.sbn("acc", KT, [128, tok], F32)
        hT, _ = C.sbn("hT", KT, [128, tok], BF16)
        P.dma("sp", acc_all.ap[:], memT.rearrange("(kc p) t -> p kc t", p=128), writes=acc + [acc_all])
        norm_prep(C, acc, gm, hT, C.rstd)

        def epi(j, ps):
            ob = C.outb.next()
            if j < 4:
                head_norm(C, [ps], C.rstd, 128, [gk.ap[:, 0:1]], [ob], gdeps=[gk])
            else:
                P.op("dve", lambda e: e.tensor_tensor(out=ob.ap[:, :tok], in0=ps.ap[:, :tok], in1=C.rstd.ap[:, :tok],
                                                      op=ALU.mult), reads=[ps, C.rstd], writes=[ob])
            store_tile(C, out, j * 128, 0, ob, final=True)
        linear(C, hT, w, 8, epi)
        P.emit()
    return C.nc


def build_A0():
    C = Ctx(TOK)
    P = C.P
    T = TOKC
    xT = C.dram_in("xT", [D, T], F32)
    posb = C.dram_in("posb", [128, T], I32)
    w_in = C.dram_in("w_in", [D, 8320], F32)
    w_uq = C.dram_in("w_uq", [1536, 4096], F32)
    w_ukv = C.dram_in("w_ukv", [512, 4096], F32)
    gmix = C.dram_in("g_mix", [128, KT], F32)
    gcq = C.dram_in("g_cq", [128, 12], F32)
    gckv = C.dram_in("g_ckv", [128, 4], F32)
    vecs = C.dram_in("vecs", [128, 8], F32)
    rots = C.dram_in("rots", [128, 2, 128], F32)
    qaT = C.dram_out("qaT", [2048, T], BF16)
    kaT = C.dram_out("kaT", [2048, T], BF16)
    vaT = C.dram_out("vaT", [2048, T], BF16)
    qbT = C.dram_out("qbT", [4096, T], BF16)
    kbnT = C.dram_out("kbnT", [2048, T], BF16)
    vbT = C.dram_out("vbT", [2048, T], BF16)
    krT = C.dram_out("krT", [128, T], BF16)
    with C.es:
        C.setup_common()
        tok = C.tok
        gm = load_const(C, "gm", gmix, [128, KT], F32)
        gq = load_const(C, "gcq", gcq, [128, 12], F32)
        gkv = load_const(C, "gckv", gckv, [128, 4], F32)
        vc = load_const(C, "vecs", vecs, [128, 8], F32)
        rt = load_const(C, "rots", rots, [128, 2, 128], F32)
        rotA = Buf(rt.ap[:, 0, :], "rotA"); rotB = Buf(rt.ap[:, 1, :], "rotB")
        rotA.last_w = rt.last_w; rotB.last_w = rt.last_w
        cosA, sinA = rope_tables(C, posb, vc.ap[:, 6:7], T, "ra", gdeps=[vc])
        cosB, sinB = rope_tables(C, posb, vc.ap[:, 7:8], T, "rb", gdeps=[vc])
        acc, acc_all = C.sbn("acc", KT, [128, tok], F32)
        hT, _ = C.sbn("hT", KT, [128, tok], BF16)
        cq, _ = C.sbn("cq", 12, [128, tok], F32)
        cqb, _ = C.sbn("cqb", 12, [128, tok], BF16)
        ckv, _ = C.sbn("ckv", 4, [128, tok], F32)
        ckvb, _ = C.sbn("ckvb", 4, [128, tok], BF16)
        r2 = C.sb("r2", [128, tok], F32)
        r3 = C.sb("r3", [128, tok], F32)
        for ch in range(T // tok):
            t0 = ch * tok
            P.dma("sp", acc_all.ap[:], xT[:, t0:t0 + tok].rearrange("(kc p) t -> p kc t", p=128),
                  writes=acc + [acc_all])
            norm_prep(C, acc, gm, hT, C.rstd)

            def epi(j, ps, t0=t0):
                if j < 32:
                    ob = C.outb.next()
                    gc = vc.ap[:, 0:1] if j < 16 else vc.ap[:, 1:2]
                    head_norm(C, [ps], C.rstd, 128, [gc], [ob], rope=[(rotA, cosA, sinA, t0)], gdeps=[vc])
                    store_tile(C, qaT if j < 16 else kaT, (j % 16) * 128, t0, ob, final=True)
                elif j < 48:
                    epi_raw_store(C, C.rstd, vaT, t0, lambda jj: (jj - 32) * 128)(j, ps)
                elif j < 64:
                    dst = cq[j - 48] if j < 60 else ckv[j - 60]
                    P.op("dve", lambda e: e.tensor_tensor(out=dst.ap[:, :tok], in0=ps.ap[:, :tok],
                                                          in1=C.rstd.ap[:, :tok], op=ALU.mult),
                         reads=[ps, C.rstd], writes=[dst])
                else:
                    ob = C.outb.next()
                    head_norm(C, [ps], C.rstd, 64, [vc.ap[:, 5:6]], [ob], rope=[(rotB, cosB, sinB, t0)], gdeps=[vc])
                    store_tile(C, krT, 0, t0, ob, final=True)
            linear(C, hT, w_in, 65, epi)
            norm_prep(C, cq, gq, cqb, r2, dim=1536)
            pend = {}

            def epi_q(j, ps, t0=t0):
                h, part = divmod(j, 2)
                if part == 0:
                    pend["nope"] = ps
                    return
                o1 = C.outb.next(); o2 = C.outb.next()
                head_norm(C, [pend["nope"], ps], r2, 192, [vc.ap[:, 2:3], vc.ap[:, 3:4]], [o1, o2],
                          rope=[None, (rotB, cosB, sinB, t0)], gdeps=[vc])
                store_tile(C, qbT, (2 * h) * 128, t0, o1, final=True)
                store_tile(C, qbT, (2 * h + 1) * 128, t0, o2, final=True)
            linear(C, cqb, w_uq, 32, epi_q)
            norm_prep(C, ckv, gkv, ckvb, r3, dim=512)

            def epi_kv(j, ps, t0=t0):
                h, part = divmod(j, 2)
                if part == 0:
                    ob = C.outb.next()
                    head_norm(C, [ps], r3, 128, [vc.ap[:, 4:5]], [ob], gdeps=[vc])
                    store_tile(C, kbnT, h * 128, t0, ob, final=True)
                else:
                    epi_raw_store(C, r3, vbT, t0, lambda jj: (jj // 2) * 128)(j, ps)
            linear(C, ckvb, w_ukv, 32, epi_kv)
        P.emit()
    return C.nc


def epi_acc(C, acc):
    P = C.P
    tok = C.tok

    def epi(j, ps):
        P.op("dve", lambda e: e.tensor_tensor(out=acc[j].ap[:, :tok], in0=ps.ap[:, :tok], in1=acc[j].ap[:, :tok],
                                              op=ALU.add), reads=[ps, acc[j]], writes=[acc[j]])
    return epi


def cross_attn(C, acc, gx, w_xq, gxq, memk, memv, w_xo, hT, kt4):
    P = C.P
    tok = C.tok
    norm_prep(C, acc, gx, hT, C.rstd)
    qx = []

    def epi_q(j, ps):
        ob = C.outb.next()
        head_norm(C, [ps], C.rstd, 128, [gxq.ap[:, 0:1]], [ob], gdeps=[gxq])
        qx.append(ob)
    linear(C, hT, w_xq, 4, epi_q)
    scale = 128 ** -0.5
    for h in range(4):
        sp = C.ps_misc.next()
        for mb in range(2):
            P.op("pe", lambda e, h=h, mb=mb, sp=sp: e.matmul(sp.ap[:, mb * tok:(mb + 1) * tok],
                                                             lhsT=memk.ap[:, h, mb * 128:(mb + 1) * 128],
                                                             rhs=qx[h].ap[:, :tok], start=True, stop=True),
                 reads=[memk, qx[h]], writes=[sp])
        pT = C.pT
        P.op("act", lambda e, sp=sp: e.activation(out=pT.ap[:, :2 * tok], in_=sp.ap[:, :2 * tok], func=AF.Exp,
                                                  scale=scale), reads=[sp], writes=[pT])
        dn = C.ps_stat.next()
        op_ = C.ps_misc.next()
        for mb in range(2):
            P.op("pe", lambda e, mb=mb, dn=dn: e.matmul(dn.ap[:, :tok], lhsT=C.onesb.ap[:],
                                                        rhs=pT.ap[:, mb * tok:(mb + 1) * tok],
                                                        start=(mb == 0), stop=(mb == 1)),
                 reads=[pT, C.onesb], writes=[dn])
        for mb in range(2):
            P.op("pe", lambda e, mb=mb, h=h, op_=op_: e.matmul(op_.ap[:, :tok], lhsT=memv.ap[:, mb, h, :],
                                                               rhs=pT.ap[:, mb * tok:(mb + 1) * tok],
                                                               start=(mb == 0), stop=(mb == 1)),
                 reads=[pT, memv], writes=[op_])
        rc = C.tmp.next()
        P.op("dve", lambda e, rc=rc, dn=dn: e.reciprocal(out=rc.ap[:, :tok], in_=dn.ap[:, :tok]),
             reads=[dn], writes=[rc])
        P.op("dve", lambda e, rc=rc, op_=op_, h=h: e.tensor_tensor(out=kt4[h].ap[:, :tok], in0=op_.ap[:, :tok],
                                                                  in1=rc.ap[:, :tok], op=ALU.mult),
             reads=[op_, rc], writes=[kt4[h]])
    linear(C, kt4, w_xo, KT, epi_acc(C, acc))


def ffn_blocks(C, acc, hT, hid, blocks):
    P = C.P
    tok = C.tok
    for (wg, wu, wd, nft, col0, row0, cse) in blocks:
        f = 0
        while f < nft:
            w = min(4, nft - f)
            wbg = C.wrot.next()
            P.dma("pool", wbg.ap[:, 0:KT, 0:w * 128],
                  wg[:, col0 + f * 128: col0 + (f + w) * 128].rearrange("(kc p) n -> p kc n", p=128), writes=[wbg])
            wbu = C.wrot.next()
            P.dma("pool", wbu.ap[:, 0:KT, 0:w * 128],
                  wu[:, col0 + f * 128: col0 + (f + w) * 128].rearrange("(kc p) n -> p kc n", p=128), writes=[wbu])
            for jj in range(w):
                pg = C.psrot.next()
                pu = C.psrot.next()
                for k in range(KT):
                    P.op("pe", lambda e, k=k, jj=jj, wbg=wbg, pg=pg: e.matmul(
                        pg.ap[:, :tok], lhsT=wbg.ap[:, k, jj * 128:(jj + 1) * 128], rhs=hT[k].ap[:, :tok],
                        start=(k == 0), stop=(k == KT - 1)), reads=[wbg, hT[k]], writes=[pg])
                for k in range(KT):
                    P.op("pe", lambda e, k=k, jj=jj, wbu=wbu, pu=pu: e.matmul(
                        pu.ap[:, :tok], lhsT=wbu.ap[:, k, jj * 128:(jj + 1) * 128], rhs=hT[k].ap[:, :tok],
                        start=(k == 0), stop=(k == KT - 1)), reads=[wbu, hT[k]], writes=[pu])
                gs = C.tmp.next()
                P.op("dve", lambda e, gs=gs, pg=pg: e.tensor_tensor(out=gs.ap[:, :tok], in0=pg.ap[:, :tok],
                                                                    in1=C.rstd.ap[:, :tok], op=ALU.mult),
                     reads=[pg, C.rstd], writes=[gs])
                P.op("act", lambda e, gs=gs: e.activation(out=gs.ap[:, :tok], in_=gs.ap[:, :tok], func=AF.Silu),
                     reads=[gs], writes=[gs])
                us = C.tmp.next()
                P.op("dve", lambda e, us=us, pu=pu, cse=cse: e.tensor_tensor(out=us.ap[:, :tok], in0=pu.ap[:, :tok],
                                                                             in1=cse.ap[:, :tok], op=ALU.mult),
                     reads=[pu, cse], writes=[us])
                P.op("pool", lambda e, gs=gs, us=us, t=hid[f + jj]: e.tensor_tensor(
                    out=t.ap[:, :tok], in0=gs.ap[:, :tok], in1=us.ap[:, :tok], op=ALU.mult),
                    reads=[gs, us], writes=[hid[f + jj]])
            f += w
        linear(C, hid[:nft], wd[row0:row0 + nft * 128, :], KT, epi_acc(C, acc))


def moe_gates(C, lg, rstd, b_r, ident, sel, cse):
    P = C.P
    tok = C.tok
    if not hasattr(C, "_mg"):
        C._mg = (C.sb("lT", [8, tok], F32), C.sb("gT", [8, tok], F32),
                 [C.sb(f"sm{i}", [128, 8], F32) for i in range(4)],
                 [C.sb(f"s1_{i}", [128, 1], F32) for i in range(6)])
    lT, gT, sm, s1 = C._mg
    P.op("dve", lambda e: e.tensor_tensor(out=lT.ap[:, :], in0=lg.ap[0:8, :tok], in1=rstd.ap[0:8, :tok], op=ALU.mult),
         reads=[lg, rstd], writes=[lT])
    P.op("dve", lambda e: e.tensor_scalar(out=lT.ap[:, :], in0=lT.ap[:, :], scalar1=b_r.ap[0:8, 0:1], scalar2=None,
                                          op0=ALU.add), reads=[lT, b_r], writes=[lT])
    for st in range(tok // 128):
        pt = C.ps_misc.next()
        P.op("pe", lambda e, st=st, pt=pt: e.matmul(pt.ap[:, 0:8], lhsT=lT.ap[:, st * 128:(st + 1) * 128],
                                                    rhs=ident.ap[0:8, 0:8], start=True, stop=True),
             reads=[lT, ident], writes=[pt])
        l, eq1, l2, eq2 = sm
        m1, m2, dd, e2, w1, w2 = s1
        P.op("dve", lambda e, pt=pt: e.tensor_copy(out=l.ap[:], in_=pt.ap[:, 0:8]), reads=[pt], writes=[l])
        P.op("dve", lambda e: e.tensor_reduce(out=m1.ap[:], in_=l.ap[:], axis=AX.X, op=ALU.max), reads=[l], writes=[m1])
        P.op("dve", lambda e: e.tensor_scalar(out=eq1.ap[:], in0=l.ap[:], scalar1=m1.ap[:, 0:1], scalar2=None,
                                              op0=ALU.is_equal), reads=[l, m1], writes=[eq1])
        P.op("dve", lambda e: e.scalar_tensor_tensor(out=l2.ap[:], in0=eq1.ap[:], scalar=-1e30, in1=l.ap[:],
                                                     op0=ALU.mult, op1=ALU.add), reads=[eq1, l], writes=[l2])
        P.op("dve", lambda e: e.tensor_reduce(out=m2.ap[:], in_=l2.ap[:], axis=AX.X, op=ALU.max), reads=[l2], writes=[m2])
        P.op("dve", lambda e: e.tensor_scalar(out=eq2.ap[:], in0=l2.ap[:], scalar1=m2.ap[:, 0:1], scalar2=None,
                                              op0=ALU.is_equal), reads=[l2, m2], writes=[eq2])
        P.op("dve", lambda e: e.tensor_tensor(out=dd.ap[:], in0=m2.ap[:], in1=m1.ap[:], op=ALU.subtract),
             reads=[m1, m2], writes=[dd])
        P.op("act", lambda e: e.activation(out=e2.ap[:], in_=dd.ap[:], func=AF.Exp), reads=[dd], writes=[e2])
        P.op("dve", lambda e: e.tensor_scalar(out=w1.ap[:], in0=e2.ap[:], scalar1=1.0, scalar2=None, op0=ALU.add),
             reads=[e2], writes=[w1])
        P.op("dve", lambda e: e.reciprocal(out=w1.ap[:], in_=w1.ap[:]), reads=[w1], writes=[w1])
        P.op("dve", lambda e: e.tensor_tensor(out=w2.ap[:], in0=e2.ap[:], in1=w1.ap[:], op=ALU.mult),
             reads=[e2, w1], writes=[w2])
        P.op("dve", lambda e: e.tensor_scalar(out=eq1.ap[:], in0=eq1.ap[:], scalar1=w1.ap[:, 0:1], scalar2=None,
                                              op0=ALU.mult), reads=[eq1, w1], writes=[eq1])
        P.op("dve", lambda e: e.scalar_tensor_tensor(out=eq2.ap[:], in0=eq2.ap[:], scalar=w2.ap[:, 0:1], in1=eq1.ap[:],
                                                     op0=ALU.mult, op1=ALU.add), reads=[eq2, w2, eq1], writes=[eq2])
        pt2 = C.ps_misc.next()
        P.op("pe", lambda e, pt2=pt2: e.matmul(pt2.ap[0:8, 0:128], lhsT=eq2.ap[:, 0:8], rhs=ident.ap[:, :],
                                               start=True, stop=True), reads=[eq2, ident], writes=[pt2])
        P.op("dve", lambda e, pt2=pt2, st=st: e.tensor_copy(out=gT.ap[:, st * 128:(st + 1) * 128],
                                                            in_=pt2.ap[0:8, 0:128]), reads=[pt2], writes=[gT])
    for ex in range(8):
        pb = C.ps_misc.next()
        P.op("pe", lambda e, ex=ex, pb=pb: e.matmul(pb.ap[:, :tok], lhsT=sel.ap[0:8, ex, :], rhs=gT.ap[:, :tok],
                                                    start=True, stop=True), reads=[sel, gT], writes=[pb])
        P.op("dve", lambda e, ex=ex, pb=pb: e.tensor_tensor(out=cse[ex].ap[:, :tok], in0=pb.ap[:, :tok],
                                                            in1=rstd.ap[:, :tok], op=ALU.mult),
             reads=[pb, rstd], writes=[cse[ex]])


def build_C(layer):
    C = Ctx(TOK)
    P = C.P
    T = TOKC
    xT = C.dram_in("xT", [D, T], F32)
    oT = C.dram_in("oT", [D, T], BF16)
    w_o = C.dram_in("w_o", [D, D], F32)
    gvec = C.dram_in("gvec", [128, 3, KT], F32)
    gxq_d = C.dram_in("g_xq", [128, 1], F32)
    w_xq = C.dram_in("w_xq", [D, 512], F32)
    w_xo = C.dram_in("w_xo", [512, D], F32)
    memk_d = C.dram_in("memk", [128, 4, 256], BF16)
    memv_d = C.dram_in("memv", [128, 2, 4, 128], BF16)
    xo = C.dram_out("xoT", [D, T], F32)
    if layer == 0:
        w_gate = C.dram_in("w_gate", [D, 14336], F32)
        w_up = C.dram_in("w_up", [D, 14336], F32)
        w_down = C.dram_in("w_down", [14336, D], F32)
        w_qkv = C.dram_in("w_qkv", [D, 3 * D], F32)
        qkvT = C.dram_out("qkvT", [3 * D, T], BF16)
    else:
        w_eg = C.dram_in("w_egate", [8, D, D], F32)
        w_eu = C.dram_in("w_eup", [8, D, D], F32)
        w_ed = C.dram_in("w_edown", [8, D, D], F32)
        w_r_d = C.dram_in("w_router", [128, KT, 8], F32)
        b_r_d = C.dram_in("b_router", [8, 1], F32)
        ident_d = C.dram_in("ident", [128, 128], F32)
        sel_d = C.dram_in("sel", [8, 8, 128], F32)
    with C.es:
        C.setup_common()
        tok = C.tok
        gv = load_const(C, "gvec", gvec, [128, 3, KT], F32)
        gx = Buf(gv.ap[:, 0, :], "gx"); gf = Buf(gv.ap[:, 1, :], "gf"); gn = Buf(gv.ap[:, 2, :], "gn")
        for b in (gx, gf, gn):
            b.last_w = gv.last_w
        gxq = load_const(C, "gxq", gxq_d, [128, 1], F32)
        memk = load_const(C, "memk", memk_d, [128, 4, 256], BF16)
        memv = load_const(C, "memv", memv_d, [128, 2, 4, 128], BF16)
        C.pT = C.sb("pT", [128, 2 * tok], BF16)
        acc, acc_all = C.sbn("acc", KT, [128, tok], F32)
        hT, _ = C.sbn("hT", KT, [128, tok], BF16)
        hid, hid_all = C.sbn("hid", KT, [128, tok], BF16)
        kt4, _ = C.sbn("kt4", 4, [128, tok], BF16)
        if layer == 1:
            w_r = load_const(C, "w_r", w_r_d, [128, KT, 8], F32)
            b_r = load_const(C, "b_r", b_r_d, [8, 1], F32)
            ident = load_const(C, "ident", ident_d, [128, 128], F32)
            sel = load_const(C, "sel", sel_d, [8, 8, 128], F32)
            cse, _ = C.sbn("cse", 8, [128, tok], F32)
        for ch in range(T // tok):
            t0 = ch * tok
            P.dma("sp", acc_all.ap[:], xT[:, t0:t0 + tok].rearrange("(kc p) t -> p kc t", p=128),
                  writes=acc + [acc_all])
            P.dma("sp", hid_all.ap[:], oT[:, t0:t0 + tok].rearrange("(kc p) t -> p kc t", p=128),
                  writes=hid + [hid_all])
            linear(C, hid, w_o, KT, epi_acc(C, acc))
            cross_attn(C, acc, gx, w_xq, gxq, memk, memv, w_xo, hT, kt4)
            if layer == 0:
                norm_prep(C, acc, gf, hT, C.rstd)
                blocks = [(w_gate, w_up, w_down, 28, b * 3584, b * 3584, C.rstd) for b in range(4)]
                ffn_blocks(C, acc, hT, hid, blocks)
                norm_prep(C, acc, gn, hT, C.rstd)
                linear(C, hT, w_qkv, 96, epi_raw_store(C, C.rstd, qkvT, t0, lambda j: j * 128))
            else:
                lg = C.ps_misc.next()
                norm_prep(C, acc, gf, hT, C.rstd, router=(w_r, lg))
                moe_gates(C, lg, C.rstd, b_r, ident, sel, cse)
                blocks = [(w_eg[ex], w_eu[ex], w_ed[ex], KT, 0, 0, cse[ex]) for ex in range(8)]
                ffn_blocks(C, acc, hT, hid, blocks)
            P.dma("act", xo[:, t0:t0 + tok].rearrange("(kc p) t -> p kc t", p=128), acc_all.ap[:],
                  reads=acc + [acc_all], final=True)
        P.emit()
    return C.nc


def cols(v, n):
    return np.ascontiguousarray(np.asarray(v, np.float32).reshape(n, 128).T)


def pad128(v):
    out = np.zeros(128, np.float32)
    out[:len(v)] = v
    return out


def rope_consts():
    theta = np.float32(500000.0)
    invA = np.zeros(128, np.float32)
    fA = theta ** (-(np.arange(0, 32, 2, dtype=np.float32)) / np.float32(32))
    invA[:16] = fA; invA[16:32] = fA
    invB = np.zeros(128, np.float32)
    fB = theta ** (-(np.arange(0, 64, 2, dtype=np.float32)) / np.float32(64))
    invB[:32] = fB; invB[32:64] = fB
    rots = np.zeros((128, 2, 128), np.float32)
    for i, half in enumerate((16, 32)):
        for m in range(half):
            rots[m + half, i, m] = -1.0
            rots[m, i, m + half] = 1.0
    return invA, invB, rots


def prep_A0(inp):
    x = inp["x"]
    xT = np.ascontiguousarray(x.reshape(-1, D).T)
    pos = inp["positions"].reshape(-1).astype(np.int32)
    w_in = np.concatenate([inp["w_in"][0], np.zeros((D, 64), np.float32)], axis=1)
    wq = inp["w_uq"][0].reshape(1536, 16, 192)
    w_uq = np.concatenate([wq, np.zeros((1536, 16, 64), np.float32)], axis=2).reshape(1536, 4096)
    w_ukv = np.ascontiguousarray(inp["w_ukv"][0])
    invA, invB, rots = rope_consts()
    gbq = inp["gb_q"][0]
    vecs = np.stack([inp["ga_q"][0], inp["ga_k"][0], gbq[:128], pad128(gbq[128:]), inp["gb_kn"][0],
                     pad128(inp["gb_kr"][0]), invA, invB], axis=1).astype(np.float32)
    shared = {"w_in": w_in, "w_uq": w_uq, "w_ukv": w_ukv, "g_mix": cols(inp["g_mix"][0], 32),
              "g_cq": cols(inp["g_cq"][0], 12), "g_ckv": cols(inp["g_ckv"][0], 4),
              "vecs": np.ascontiguousarray(vecs), "rots": rots}
    ins = []
    for c in range(NCORES):
        m = dict(shared)
        m["xT"] = np.ascontiguousarray(xT[:, c * TOKC:(c + 1) * TOKC])
        m["posb"] = np.ascontiguousarray(np.broadcast_to(pos[c * TOKC:(c + 1) * TOKC][None, :], (128, TOKC)))
        ins.append(m)
    return ins


def build_B0():
    C = Ctx(512)
    P = C.P
    NH = 8
    qa = C.dram_in("qa", [NH, 128, SEQ], BF16)
    ka = C.dram_in("ka", [NH, 128, SEQ], BF16)
    va = C.dram_in("va", [NH, SEQ, 128], BF16)
    qb = C.dram_in("qb", [NH, 256, SEQ], BF16)
    kbn = C.dram_in("kbn", [NH, 128, SEQ], BF16)
    krd = C.dram_in("kr", [128, SEQ], BF16)
    vb = C.dram_in("vb", [NH, SEQ, 128], BF16)
    mask2_d = C.dram_in("mask2", [128, 256], BF16)
    maskc_d = C.dram_in("maskc", [128, 4, 512], BF16)
    oT = C.dram_out("oT", [2 * NH, 128, SEQ], BF16)
    with C.es:
        onesb = C.sb("onesb", [128, 128], BF16)
        P.op("pool", lambda e: e.memset(onesb.ap[:], 1.0), writes=[onesb])
        mask2 = load_const(C, "mask2", mask2_d, [128, 256], BF16)
        maskc = load_const(C, "maskc", maskc_d, [128, 4, 512], BF16)
        kr = load_const(C, "kr", krd, [128, SEQ], BF16)
        psS = Rot([C.ps(f"pS{i}") for i in range(3)])
        psO = Rot([C.ps(f"pO{i}") for i in range(2)])
        psD = Rot([C.ps(f"pD{i}") for i in range(2)])
        qrot = Rot([C.sb(f"q{i}", [128, SEQ], BF16) for i in range(2)])
        krot = Rot([C.sb(f"k{i}", [128, SEQ], BF16) for i in range(2)])
        q2rot = Rot([C.sb(f"q2{i}", [128, SEQ], BF16) for i in range(2)])
        vrot = Rot([C.sb(f"v{i}", [128, 3, 16, 128], BF16) for i in range(2)])
        qd = [None, C.sb("qd1", [128, SEQ], BF16), C.sb("qd2", [128, SEQ], BF16)]
        kd = [None, C.sb("kd1", [128, SEQ], BF16), C.sb("kd2", [128, SEQ], BF16)]
        acc = C.sb("acc", [128, 2, SEQ], F32)
        rc = C.sb("rc", [128, SEQ], F32)
        obuf = Rot([C.sb(f"ob{i}", [128, SEQ], BF16) for i in range(2)])
        pTr = Rot([C.sb(f"pT{i}", [128, 512], BF16) for i in range(3)])
        rc5 = Rot([C.sb(f"rc5{i}", [128, 512], F32) for i in range(2)])
        sA = 128 ** -0.5
        sB = 192 ** -0.5
        for h in range(NH):
            q = qrot.next(); k = krot.next(); v3 = vrot.next()
            P.dma("sp", q.ap[:], qa[h], writes=[q])
            P.dma("sp", k.ap[:], ka[h], writes=[k])
            P.dma("sp", v3.ap[:, 0], va[h].rearrange("(i p) d -> p i d", p=128), writes=[v3])
            P.dma("sp", v3.ap[:, 1].rearrange("p (i r) d -> p i r d", r=4),
                  va[h].rearrange("(i p r) d -> p i r d", i=4, p=128, r=4), writes=[v3])
            P.dma("sp", v3.ap[:, 2], va[h].rearrange("(p r) d -> p r d", r=16), writes=[v3])
            for br, dil in ((1, 4), (2, 16)):
                nb = 16 // dil
                P.op("pool", lambda e, br=br, dil=dil, nb=nb, q=q: e.tensor_copy(
                    out=qd[br].ap.rearrange("p (r i j) -> p r i j", r=dil, i=nb, j=128),
                    in_=q.ap.rearrange("p (i j r) -> p r i j", i=nb, j=128, r=dil)), reads=[q], writes=[qd[br]])
                P.op("dve", lambda e, br=br, dil=dil, nb=nb, k=k: e.tensor_copy(
                    out=kd[br].ap.rearrange("p (r i j) -> p r i j", r=dil, i=nb, j=128),
                    in_=k.ap.rearrange("p (i j r) -> p r i j", i=nb, j=128, r=dil)), reads=[k], writes=[kd[br]])
            for br, dil in ((0, 1), (1, 4), (2, 16)):
                nb = 16 // dil
                qq = q if br == 0 else qd[br]
                kk = k if br == 0 else kd[br]
                accv = acc.ap.rearrange("p a (i j r) -> p a r i j", i=nb, j=128, r=dil)
                for r in range(dil):
                    for i in range(nb):
                        c0 = (r * nb + i) * 128
                        sp = psS.next()
                        if i >= 1:
                            P.op("pe", lambda e, sp=sp, kk=kk, qq=qq, c0=c0: e.matmul(
                                sp.ap[:, 0:128], lhsT=kk.ap[:, c0 - 128:c0], rhs=qq.ap[:, c0:c0 + 128],
                                start=True, stop=True), reads=[kk, qq], writes=[sp])
                        P.op("pe", lambda e, sp=sp, kk=kk, qq=qq, c0=c0: e.matmul(
                            sp.ap[:, 128:256], lhsT=kk.ap[:, c0:c0 + 128], rhs=qq.ap[:, c0:c0 + 128],
                            start=True, stop=True), reads=[kk, qq], writes=[sp])
                        lo = 0 if i >= 1 else 128
                        pT = pTr.next()
                        P.op("act", lambda e, sp=sp, pT=pT, lo=lo: e.activation(
                            out=pT.ap[:, lo:256], in_=sp.ap[:, lo:256], func=AF.Exp, scale=sA), reads=[sp], writes=[pT])
                        P.op("pool", lambda e, pT=pT, lo=lo: e.tensor_tensor(
                            out=pT.ap[:, lo:256], in0=pT.ap[:, lo:256], in1=mask2.ap[:, lo:256], op=ALU.mult),
                            reads=[pT, mask2], writes=[pT])
                        po = psO.next()
                        vi = i * dil + r
                        if i >= 1:
                            P.op("pe", lambda e, po=po, pT=pT, v3=v3, br=br, vi=vi, dil=dil: e.matmul(
                                po.ap[:, 0:128], lhsT=v3.ap[:, br, vi - dil, :], rhs=pT.ap[:, 0:128],
                                start=True, stop=False), reads=[v3, pT], writes=[po])
                        P.op("pe", lambda e, po=po, pT=pT, v3=v3, br=br, vi=vi, i=i: e.matmul(
                            po.ap[:, 0:128], lhsT=v3.ap[:, br, vi, :], rhs=pT.ap[:, 128:256],
                            start=(i == 0), stop=True), reads=[v3, pT], writes=[po])
                        if i >= 1:
                            P.op("pe", lambda e, po=po, pT=pT: e.matmul(
                                po.ap[:, 128:256], lhsT=onesb.ap[:], rhs=pT.ap[:, 0:128],
                                start=True, stop=False), reads=[onesb, pT], writes=[po])
                        P.op("pe", lambda e, po=po, pT=pT, i=i: e.matmul(
                            po.ap[:, 128:256], lhsT=onesb.ap[:], rhs=pT.ap[:, 128:256],
                            start=(i == 0), stop=True), reads=[onesb, pT], writes=[po])
                        pov = po.ap[:, 0:256].rearrange("p (a j) -> p a j", a=2)
                        if br == 0:
                            P.op("dve", lambda e, pov=pov, accv=accv, r=r, i=i: e.tensor_copy(
                                out=accv[:, :, r, i, :], in_=pov), reads=[po], writes=[acc])
                        else:
                            P.op("dve", lambda e, pov=pov, accv=accv, r=r, i=i: e.tensor_tensor(
                                out=accv[:, :, r, i, :], in0=pov, in1=accv[:, :, r, i, :], op=ALU.add),
                                reads=[po, acc], writes=[acc])
            ob = obuf.next()
            P.op("dve", lambda e: e.reciprocal(out=rc.ap[:], in_=acc.ap[:, 1, :]), reads=[acc], writes=[rc])
            P.op("dve", lambda e, ob=ob: e.tensor_tensor(out=ob.ap[:], in0=acc.ap[:, 0, :], in1=rc.ap[:], op=ALU.mult),
                 reads=[acc, rc], writes=[ob])
            P.dma("act", oT[h], ob.ap[:], reads=[ob], final=True)
        for h in range(NH):
            qn = qrot.next(); qr = q2rot.next(); kn = krot.next(); v3 = vrot.next()
            P.dma("sp", qn.ap[:], qb[h, 0:128, :], writes=[qn])
            P.dma("sp", qr.ap[:], qb[h, 128:256, :], writes=[qr])
            P.dma("sp", kn.ap[:], kbn[h], writes=[kn])
            P.dma("sp", v3.ap[:, 0], vb[h].rearrange("(i p) d -> p i d", p=128), writes=[v3])
            ob = obuf.next()
            for G in range(4):
                po = psO.next(); pd = psD.next()
                nkb = 4 * G + 4
                for kb in range(nkb):
                    sp = psS.next()
                    P.op("pe", lambda e, sp=sp, kn=kn, qn=qn, kb=kb, G=G: e.matmul(
                        sp.ap[:, :], lhsT=kn.ap[:, kb * 128:(kb + 1) * 128], rhs=qn.ap[:, G * 512:(G + 1) * 512],
                        start=True, stop=False), reads=[kn, qn], writes=[sp])
                    P.op("pe", lambda e, sp=sp, qr=qr, kb=kb, G=G: e.matmul(
                        sp.ap[:, :], lhsT=kr.ap[:, kb * 128:(kb + 1) * 128], rhs=qr.ap[:, G * 512:(G + 1) * 512],
                        start=False, stop=True), reads=[kr, qr], writes=[sp])
                    pT = pTr.next()
                    P.op("act", lambda e, sp=sp, pT=pT: e.activation(out=pT.ap[:, :], in_=sp.ap[:, :], func=AF.Exp,
                                                                     scale=sB), reads=[sp], writes=[pT])
                    if kb >= 4 * G:
                        P.op("pool", lambda e, pT=pT, o=kb - 4 * G: e.tensor_tensor(
                            out=pT.ap[:, :], in0=pT.ap[:, :], in1=maskc.ap[:, o, :], op=ALU.mult),
                            reads=[pT, maskc], writes=[pT])
                    P.op("pe", lambda e, po=po, pT=pT, v3=v3, kb=kb, nkb=nkb: e.matmul(
                        po.ap[:, :], lhsT=v3.ap[:, 0, kb, :], rhs=pT.ap[:, :], start=(kb == 0), stop=(kb == nkb - 1)),
                        reads=[v3, pT], writes=[po])
                    P.op("pe", lambda e, pd=pd, pT=pT, kb=kb, nkb=nkb: e.matmul(
                        pd.ap[:, :], lhsT=onesb.ap[:], rhs=pT.ap[:, :], start=(kb == 0), stop=(kb == nkb - 1)),
                        reads=[onesb, pT], writes=[pd])
                r5 = rc5.next()
                P.op("dve", lambda e, r5=r5, pd=pd: e.reciprocal(out=r5.ap[:], in_=pd.ap[:]), reads=[pd], writes=[r5])
                P.op("dve", lambda e, r5=r5, po=po, ob=ob, G=G: e.tensor_tensor(
                    out=ob.ap[:, G * 512:(G + 1) * 512], in0=po.ap[:], in1=r5.ap[:], op=ALU.mult),
                    reads=[po, r5], writes=[ob])
            P.dma("act", oT[NH + h], ob.ap[:], reads=[ob], final=True)
        P.emit()
    return C.nc


def build_B1(NH=16):
    C = Ctx(512)
    P = C.P
    qd_ = C.dram_in("q", [NH, 128, SEQ], BF16)
    kd_ = C.dram_in("k", [NH, 128, SEQ], BF16)
    vd_ = C.dram_in("v", [NH, SEQ, 128], BF16)
    tri_d = C.dram_in("tri", [128, 128], BF16)
    masks_d = C.dram_in("masks", [128, 4, 512], BF16)
    oT = C.dram_out("oT", [NH, 128, SEQ], BF16)
    with C.es:
        onesb = C.sb("onesb", [128, 128], BF16)
        P.op("pool", lambda e: e.memset(onesb.ap[:], 1.0), writes=[onesb])
        onef = C.sb("onef", [128, 1], F32)
        P.op("pool", lambda e: e.memset(onef.ap[:], 1.0), writes=[onef])
        tri = load_const(C, "tri", tri_d, [128, 128], BF16)
        masks = load_const(C, "masks", masks_d, [128, 4, 512], BF16)
        psS = Rot([C.ps(f"pS{i}") for i in range(2)])
        psL = Rot([C.ps(f"pL{i}") for i in range(2)])
        psC = Rot([C.ps(f"pC{i}") for i in range(2)])
        psO = Rot([C.ps(f"pO{i}") for i in range(2)])
        qrot = Rot([C.sb(f"q{i}", [128, SEQ], BF16) for i in range(2)])
        krot = Rot([C.sb(f"k{i}", [128, SEQ], BF16) for i in range(2)])
        vrot = Rot([C.sb(f"v{i}", [128, 16, 128], BF16) for i in range(2)])
        obuf = Rot([C.sb(f"ob{i}", [128, SEQ], BF16) for i in range(2)])
        ezr = Rot([C.sb(f"ez{i}", [128, 512], F32) for i in range(2)])
        Lr = Rot([C.sb(f"L{i}", [128, 512], F32) for i in range(3)])
        nlr = Rot([C.sb(f"nl{i}", [128, 512], BF16) for i in range(3)])
        tr = Rot([C.sb(f"t{i}", [128, 512], F32) for i in range(3)])
        pTr = Rot([C.sb(f"pT{i}", [128, 512], BF16) for i in range(3)])
        carry = C.sb("carry", [128, 512], F32)
        s = 128 ** -0.5
        for h in range(NH):
            q = qrot.next(); k = krot.next(); v = vrot.next()
            P.dma("sp", q.ap[:], qd_[h], writes=[q])
            P.dma("sp", k.ap[:], kd_[h], writes=[k])
            P.dma("sp", v.ap[:], vd_[h].rearrange("(i p) d -> p i d", p=128), writes=[v])
            ob = obuf.next()
            for G in range(4):
                po = psO.next()
                nkb = 4 * G + 4
                for idx, kb in enumerate(reversed(range(nkb))):
                    diag = kb >= 4 * G
                    o = kb - 4 * G
                    sp = psS.next()
                    P.op("pe", lambda e, sp=sp, k=k, q=q, kb=kb, G=G: e.matmul(
                        sp.ap[:, :], lhsT=k.ap[:, kb * 128:(kb + 1) * 128], rhs=q.ap[:, G * 512:(G + 1) * 512],
                        start=True, stop=True), reads=[k, q], writes=[sp])
                    ez = ezr.next(); L = Lr.next(); nl = nlr.next(); t = tr.next(); pT = pTr.next()
                    P.op("act", lambda e, sp=sp, ez=ez: e.activation(out=ez.ap[:], in_=sp.ap[:], func=AF.Exp, scale=-s),
                         reads=[sp], writes=[ez])
                    P.op("act", lambda e, ez=ez, L=L: e.activation(out=L.ap[:], in_=ez.ap[:], func=AF.Ln,
                                                                   bias=onef.ap[:, 0:1], scale=1.0),
                         reads=[ez, onef], writes=[L])
                    P.op("dve", lambda e, sp=sp, L=L, nl=nl: e.scalar_tensor_tensor(
                        out=nl.ap[:], in0=sp.ap[:], scalar=s, in1=L.ap[:], op0=ALU.mult, op1=ALU.add),
                        reads=[sp, L], writes=[nl])
                    if diag:
                        P.op("pool", lambda e, nl=nl, o=o: e.tensor_tensor(out=nl.ap[:], in0=nl.ap[:],
                                                                          in1=masks.ap[:, o, :], op=ALU.mult),
                             reads=[nl, masks], writes=[nl])
                    pl = psL.next(); pc = psC.next()
                    P.op("pe", lambda e, pl=pl, nl=nl: e.matmul(pl.ap[:], lhsT=tri.ap[:], rhs=nl.ap[:],
                                                                start=True, stop=True), reads=[tri, nl], writes=[pl])
                    P.op("pe", lambda e, pc=pc, nl=nl: e.matmul(pc.ap[:], lhsT=onesb.ap[:], rhs=nl.ap[:],
                                                                start=True, stop=True), reads=[onesb, nl], writes=[pc])
                    P.op("dve", lambda e, pl=pl, L=L, t=t: e.tensor_tensor(out=t.ap[:], in0=pl.ap[:], in1=L.ap[:],
                                                                           op=ALU.add), reads=[pl, L], writes=[t])
                    if idx > 0:
                        P.op("pool", lambda e, t=t: e.tensor_tensor(out=t.ap[:], in0=t.ap[:], in1=carry.ap[:],
                                                                    op=ALU.add), reads=[t, carry], writes=[t])
                    P.op("act", lambda e, t=t, pT=pT: e.activation(out=pT.ap[:], in_=t.ap[:], func=AF.Exp, scale=-1.0),
                         reads=[t], writes=[pT])
                    if diag:
                        P.op("pool", lambda e, pT=pT, o=o: e.tensor_tensor(out=pT.ap[:], in0=pT.ap[:],
                                                                          in1=masks.ap[:, o, :], op=ALU.mult),
                             reads=[pT, masks], writes=[pT])
                    P.op("pe", lambda e, po=po, pT=pT, v=v, kb=kb, idx=idx, nkb=nkb: e.matmul(
                        po.ap[:], lhsT=v.ap[:, kb, :], rhs=pT.ap[:], start=(idx == 0), stop=(idx == nkb - 1)),
                        reads=[v, pT], writes=[po])
                    if idx < nkb - 1:
                        if idx == 0:
                            P.op("dve", lambda e, pc=pc: e.tensor_copy(out=carry.ap[:], in_=pc.ap[:]),
                                 reads=[pc], writes=[carry])
                        else:
                            P.op("dve", lambda e, pc=pc: e.tensor_tensor(out=carry.ap[:], in0=pc.ap[:], in1=carry.ap[:],
                                                                         op=ALU.add), reads=[pc, carry], writes=[carry])
                P.op("dve", lambda e, po=po, ob=ob, G=G: e.tensor_copy(out=ob.ap[:, G * 512:(G + 1) * 512], in_=po.ap[:]),
                     reads=[po], writes=[ob])
            P.dma("act", oT[h], ob.ap[:], reads=[ob], final=True)
        P.emit()
    return C.nc


def attn_masks():
    kk = np.arange(128)[:, None]
    q1 = np.arange(128)[None, :]
    mask2 = np.concatenate([(kk >= q1), (kk <= q1)], axis=1).astype(NPBF)
    q5 = np.arange(512)[None, :]
    maskc = np.stack([(o * 128 + kk <= q5) for o in range(4)], axis=1).astype(NPBF)
    masks = np.stack([(o * 128 + kk < q5) for o in range(4)], axis=1).astype(NPBF)
    tri = (np.arange(128)[:, None] > np.arange(128)[None, :]).astype(NPBF)
    return mask2, maskc, masks, tri


def run(nc, ins):
    res = run_bass_kernel_spmd(nc, ins, core_ids=list(range(len(ins))))
    return res.results


def gather_T(results, name):
    return np.concatenate([np.asarray(r[name]) for r in results], axis=1)


def heads_fm(full, b, h0, nh, hw=128):
    blk = full[h0 * hw:(h0 + nh) * hw, b * SEQ:(b + 1) * SEQ]
    return np.ascontiguousarray(blk.reshape(nh, hw, SEQ))


def heads_tm(full, b, h0, nh):
    return np.ascontiguousarray(heads_fm(full, b, h0, nh).transpose(0, 2, 1))


def prep_B0(qaT, kaT, vaT, qbT, kbnT, vbT, krT):
    mask2, maskc, _, _ = attn_masks()
    ins = []
    for c in range(NCORES):
        b, j = divmod(c, 2)
        ins.append({"qa": heads_fm(qaT, b, 8 * j, 8), "ka": heads_fm(kaT, b, 8 * j, 8), "va": heads_tm(vaT, b, 8 * j, 8),
                    "qb": heads_fm(qbT, b, 8 * j, 8, 256), "kbn": heads_fm(kbnT, b, 8 * j, 8),
                    "kr": np.ascontiguousarray(krT[:, b * SEQ:(b + 1) * SEQ]), "vb": heads_tm(vbT, b, 8 * j, 8),
                    "mask2": mask2, "maskc": maskc})
    return ins


def assemble_o0(results):
    o = np.zeros((D, 4 * SEQ), NPBF)
    for c, r in enumerate(results):
        b, j = divmod(c, 2)
        t = np.asarray(r["oT"])
        o[(8 * j) * 128:(8 * j + 8) * 128, b * SEQ:(b + 1) * SEQ] = t[:8].reshape(1024, SEQ)
        o[2048 + (8 * j) * 128:2048 + (8 * j + 8) * 128, b * SEQ:(b + 1) * SEQ] = t[8:].reshape(1024, SEQ)
    return o


def prep_B1(qkvT):
    _, _, masks, tri = attn_masks()
    ins = []
    for c in range(NCORES):
        b, j = divmod(c, 2)
        ins.append({"q": heads_fm(qkvT[0:D], b, 16 * j, 16), "k": heads_fm(qkvT[D:2 * D], b, 16 * j, 16),
                    "v": heads_tm(qkvT[2 * D:3 * D], b, 16 * j, 16), "tri": tri, "masks": masks})
    return ins


def assemble_o1(results):
    o = np.zeros((D, 4 * SEQ), NPBF)
    for c, r in enumerate(results):
        b, j = divmod(c, 2)
        o[(16 * j) * 128:(16 * j + 16) * 128, b * SEQ:(b + 1) * SEQ] = np.asarray(r["oT"]).reshape(2048, SEQ)
    return o


def prep_C(layer, inp, xT_full, oT_full, mkvT):
    i = 0
    shared = {"w_o": inp["w_o_even"][0] if layer == 0 else inp["w_o_odd"][0],
              "g_xq": np.ascontiguousarray(inp["g_xq"][layer].reshape(128, 1)),
              "w_xq": np.ascontiguousarray(inp["w_xq"][layer]), "w_xo": np.ascontiguousarray(inp["w_xo"][layer])}
    gnext = inp["g_mix"][1] if layer == 0 else inp["g_mix"][1]
    shared["gvec"] = np.ascontiguousarray(np.stack([cols(inp["g_x"][layer], 32), cols(inp["g_ffn"][layer], 32),
                                                    cols(gnext, 32)], axis=1))
    if layer == 0:
        shared.update({"w_gate": inp["w_gate"][i], "w_up": inp["w_up"][i], "w_down": inp["w_down"][i],
                       "w_qkv": inp["w_qkv"][i]})
    else:
        sel = np.zeros((8, 8, 128), np.float32)
        for e in range(8):
            sel[e, e, :] = 1.0
        shared.update({"w_egate": inp["w_egate"][i], "w_eup": inp["w_eup"][i], "w_edown": inp["w_edown"][i],
                       "w_router": np.ascontiguousarray(inp["w_router"][i].reshape(32, 128, 8).transpose(1, 0, 2)),
                       "b_router": np.ascontiguousarray(inp["b_router"][i].reshape(8, 1)),
                       "ident": np.eye(128, dtype=np.float32), "sel": sel})
    ins = []
    for c in range(NCORES):
        b = c // 2
        m = dict(shared)
        m["xT"] = np.ascontiguousarray(xT_full[:, c * TOKC:(c + 1) * TOKC])
        m["oT"] = np.ascontiguousarray(oT_full[:, c * TOKC:(c + 1) * TOKC])
        mk = mkvT[0:512, b * 256:(b + 1) * 256]
        mv = mkvT[512:1024, b * 256:(b + 1) * 256]
        m["memk"] = np.ascontiguousarray(mk.reshape(4, 128, 256).transpose(1, 0, 2))
        m["memv"] = np.ascontiguousarray(mv.reshape(4, 128, 2, 128).transpose(3, 2, 0, 1))
        ins.append(m)
    return ins


def prep_P(inp):
    memT = np.ascontiguousarray(inp["mem"].reshape(-1, D).T)
    ins = []
    for c in range(NCORES):
        ins.append({"memT": np.ascontiguousarray(memT[:, c * 128:(c + 1) * 128]), "g_mem": cols(inp["g_mem"], 32),
                    "g_mk": np.ascontiguousarray(inp["g_mem_k"].reshape(128, 1).astype(np.float32)),
                    "w_mem_kv": inp["w_mem_kv"]})
    return ins


def _kernel_unfused(inp):
    inp = {k: np.asarray(v) for k, v in inp.items()}
    rP = run(build_P(), prep_P(inp))
    mkvT = gather_T(rP, "mkvT")
    rA = run(build_A0(), prep_A0(inp))
    g = {n: gather_T(rA, n) for n in ("qaT", "kaT", "vaT", "qbT", "kbnT", "vbT", "krT")}
    del rA
    rB = run(build_B0(), prep_B0(g["qaT"], g["kaT"], g["vaT"], g["qbT"], g["kbnT"], g["vbT"], g["krT"]))
    o0 = assemble_o0(rB)
    del rB, g
    xT = np.ascontiguousarray(inp["x"].reshape(-1, D).T)
    rC = run(build_C(0), prep_C(0, inp, xT, o0, mkvT))
    x3T = gather_T(rC, "xoT")
    qkvT = gather_T(rC, "qkvT")
    del rC
    rB1 = run(build_B1(), prep_B1(qkvT))
    o1 = assemble_o1(rB1)
    del rB1, qkvT
    rC1 = run(build_C(1), prep_C(1, inp, x3T, o1, mkvT))
    outT = gather_T(rC1, "xoT")
    return np.ascontiguousarray(outT.T).reshape(4, SEQ, D).astype(np.float32, copy=False)


T2 = 2 * TOKC


def body_rope(C, posb, vc_d, tabs):
    P = C.P
    vc = load_const(C, "vecs_r", vc_d, [128, 8], F32)
    for half in range(2):
        sl = slice(half * TOKC, (half + 1) * TOKC)
        for name, col, oc, os_ in (("ra", 6, 0, 1), ("rb", 7, 2, 3)):
            cos, sin = rope_tables(C, posb[:, sl], vc.ap[:, col:col + 1], TOKC, f"{name}{half}", gdeps=[vc])
            P.dma("sp", tabs[oc][0][:, sl], cos.ap[:], reads=[cos], writes=[tabs[oc][1]])
            P.dma("sp", tabs[os_][0][:, sl], sin.ap[:], reads=[sin], writes=[tabs[os_][1]])


def body_P2(C, memT, g_mem, g_mk, w, ident_d, mkT, mv):
    P = C.P
    C.setup_common()
    setup_vt(C, ident_d)
    tok = C.tok
    gm = load_const(C, "gm", g_mem, [128, KT], F32)
    gk = load_const(C, "gk", g_mk, [128, 1], F32)
    acc, acc_all = C.sbn("acc", KT, [128, tok], F32)
    hT, _ = C.sbn("hT", KT, [128, tok], BF16)
    for ch in range(2):
        t0 = ch * tok
        P.dma("sp", acc_all.ap[:], memT[:, t0:t0 + tok].rearrange("(kc p) t -> p kc t", p=128), writes=acc + [acc_all])
        norm_prep(C, acc, gm, hT, C.rstd)

        def epi(j, ps, t0=t0):
            if j < 4:
                ob = C.outb.next()
                head_norm(C, [ps], C.rstd, 128, [gk.ap[:, 0:1]], [ob], gdeps=[gk])
                C.P.dma("act", mkT[0][j * 128:(j + 1) * 128, t0:t0 + tok], ob.ap[:, :tok], reads=[ob], writes=[mkT[1]])
            else:
                epi_v_store(C, C.rstd, mv[0], t0, lambda jj: jj - 4, C.identb)(j, ps)
        linear(C, hT, w, 8, epi)
    mv[1].last_w = [o for o in P.ops["act"] if o.is_dma][-1]


def mark_written(P, bufs, eng="act", n=DMA_ROT):
    last = [o for o in P.ops[eng] if o.is_dma][-1]
    for b in bufs:
        b.last_w = last


def body_A0(C, xT, tabs, w_in, w_uq, w_ukv, gmix, gcq, gckv, vecs, rots, ident_d, outs):
    P = C.P
    qaT, kaT, va, qbT, kbnT, vb, krT = outs
    C.setup_common()
    setup_vt(C, ident_d)
    tok = C.tok
    gm = load_const(C, "gm", gmix, [128, KT], F32)
    gq = load_const(C, "gcq", gcq, [128, 12], F32)
    gkv = load_const(C, "gckv", gckv, [128, 4], F32)
    vc = load_const(C, "vecs", vecs, [128, 8], F32)
    rt = load_const(C, "rots", rots, [128, 2, 128], F32)
    rotA = Buf(rt.ap[:, 0, :], "rotA"); rotB = Buf(rt.ap[:, 1, :], "rotB")
    rotA.last_w = rt.last_w; rotB.last_w = rt.last_w
    tb = [Rot([C.sb(f"tab{i}_{k}", [128, tok], F32) for k in range(2)]) for i in range(4)]
    acc, acc_all = C.sbn("acc", KT, [128, tok], F32)
    hT, _ = C.sbn("hT", KT, [128, tok], BF16)
    cq, _ = C.sbn("cq", 12, [128, tok], F32)
    cqb, _ = C.sbn("cqb", 12, [128, tok], BF16)
    ckv, _ = C.sbn("ckv", 4, [128, tok], F32)
    ckvb, _ = C.sbn("ckvb", 4, [128, tok], BF16)
    r2 = C.sb("r2", [128, tok], F32)
    r3 = C.sb("r3", [128, tok], F32)
    for ch in range(T2 // tok):
        t0 = ch * tok
        P.dma("sp", acc_all.ap[:], xT[:, t0:t0 + tok].rearrange("(kc p) t -> p kc t", p=128), writes=acc + [acc_all])
        cur = []
        for i in range(4):
            b = tb[i].next()
            P.dma("sp", b.ap[:], tabs[i][0][:, t0:t0 + tok], reads=[tabs[i][1]], writes=[b])
            cur.append(b)
        cosA, sinA, cosB, sinB = cur
        norm_prep(C, acc, gm, hT, C.rstd)

        def epi(j, ps, t0=t0, cosA=cosA, sinA=sinA, cosB=cosB, sinB=sinB):
            if j < 32:
                ob = C.outb.next()
                gc = vc.ap[:, 0:1] if j < 16 else vc.ap[:, 1:2]
                head_norm(C, [ps], C.rstd, 128, [gc], [ob], rope=[(rotA, cosA, sinA, 0)], gdeps=[vc])
                store_tile(C, qaT[0] if j < 16 else kaT[0], (j % 16) * 128, t0, ob)
            elif j < 48:
                epi_v_store(C, C.rstd, va[0], t0, lambda jj: jj - 32, C.identb)(j, ps)
            elif j < 64:
                dst = cq[j - 48] if j < 60 else ckv[j - 60]
                P.op("dve", lambda e: e.tensor_tensor(out=dst.ap[:, :tok], in0=ps.ap[:, :tok], in1=C.rstd.ap[:, :tok],
                                                      op=ALU.mult), reads=[ps, C.rstd], writes=[dst])
            else:
                ob = C.outb.next()
                head_norm(C, [ps], C.rstd, 64, [vc.ap[:, 5:6]], [ob], rope=[(rotB, cosB, sinB, 0)], gdeps=[vc])
                store_tile(C, krT[0], 0, t0, ob)
        linear(C, hT, w_in, 65, epi)
        norm_prep(C, cq, gq, cqb, r2, dim=1536)
        pend = {}

        def epi_q(j, ps, t0=t0, cosB=cosB, sinB=sinB):
            h, part = divmod(j, 2)
            if part == 0:
                pend["nope"] = ps
                return
            o1 = C.outb.next(); o2 = C.outb.next()
            head_norm(C, [pend["nope"], ps], r2, 192, [vc.ap[:, 2:3], vc.ap[:, 3:4]], [o1, o2],
                      rope=[None, (rotB, cosB, sinB, 0)], gdeps=[vc])
            store_tile(C, qbT[0], (2 * h) * 128, t0, o1)
            store_tile(C, qbT[0], (2 * h + 1) * 128, t0, o2)
        linear(C, cqb, w_uq, 32, epi_q)
        norm_prep(C, ckv, gkv, ckvb, r3, dim=512)

        def epi_kv(j, ps, t0=t0):
            h, part = divmod(j, 2)
            if part == 0:
                ob = C.outb.next()
                head_norm(C, [ps], r3, 128, [vc.ap[:, 4:5]], [ob], gdeps=[vc])
                store_tile(C, kbnT[0], h * 128, t0, ob)
            else:
                epi_v_store(C, r3, vb[0], t0, lambda jj: jj // 2, C.identb)(j, ps)
        linear(C, ckvb, w_ukv, 32, epi_kv)


def body_B0(C, qaT, kaT, va, qbT, kbnT, vb, krT, dmask_d, maskc_d, visb_d, o0T):
    P = C.P
    NH = 16
    qa = qaT.rearrange("(h d) t -> h d t", d=128)
    ka = kaT.rearrange("(h d) t -> h d t", d=128)
    qb = qbT.rearrange("(h d) t -> h d t", d=256)
    kbn = kbnT.rearrange("(h d) t -> h d t", d=128)
    oT = o0T.rearrange("(h d) t -> h d t", d=128)
    onesb = C.sb("onesb", [128, 128], BF16)
    P.op("pool", lambda e: e.memset(onesb.ap[:], 1.0), writes=[onesb])
    zb = C.sb("zb", [128, 1], F32)
    P.op("pool", lambda e: e.memset(zb.ap[:], 0.0), writes=[zb])
    dmask = load_const(C, "dmask", dmask_d, [128, 21, 256], BF16)
    maskc = load_const(C, "maskc", maskc_d, [128, 4, 512], BF16)
    visb = load_const(C, "visb", visb_d, [128, 1], F32)
    kr = load_const(C, "kr", krT, [128, SEQ], BF16)
    psS = Rot([C.ps(f"pS{i}") for i in range(3)])
    psO = Rot([C.ps(f"pO{i}") for i in range(2)])
    psD = Rot([C.ps(f"pD{i}") for i in range(2)])
    qrot = Rot([C.sb(f"q{i}", [128, SEQ], BF16) for i in range(2)])
    krot = Rot([C.sb(f"k{i}", [128, SEQ], BF16) for i in range(2)])
    q2rot = Rot([C.sb(f"q2{i}", [128, SEQ], BF16) for i in range(2)])
    vrot = Rot([C.sb(f"v{i}", [128, 3, 16, 128], BF16) for i in range(2)])
    qd = [None, C.sb("qd1", [128, SEQ], BF16), C.sb("qd2", [128, SEQ], BF16)]
    kd = [None, C.sb("kd1", [128, SEQ], BF16), C.sb("kd2", [128, SEQ], BF16)]
    acc = C.sb("acc", [128, 2, SEQ], F32)
    rc = C.sb("rc", [128, SEQ], F32)
    obuf = Rot([C.sb(f"ob{i}", [128, SEQ], BF16) for i in range(2)])
    pTr = Rot([C.sb(f"pT{i}", [128, 512], BF16) for i in range(3)])
    rc5 = Rot([C.sb(f"rc5{i}", [128, 512], F32) for i in range(2)])
    sA = 128 ** -0.5
    sB = 192 ** -0.5
    mbase = {0: 0, 1: 16, 2: 20}
    for h in range(NH):
        q = qrot.next(); k = krot.next(); v3 = vrot.next()
        P.dma("sp", q.ap[:], qa[h], writes=[q])
        P.dma("sp", k.ap[:], ka[h], writes=[k])
        vh = va[:, h, :]
        P.dma("sp", v3.ap[:, 0], vh.rearrange("(i p) d -> p i d", p=128), writes=[v3])
        for i4 in range(4):
            P.dma("sp", v3.ap[:, 1, i4 * 4:(i4 + 1) * 4, :],
                  vh[i4 * 512:(i4 + 1) * 512, :].rearrange("(p r) d -> p r d", r=4), writes=[v3])
        P.dma("sp", v3.ap[:, 2], vh.rearrange("(p r) d -> p r d", r=16), writes=[v3])
        for br, dil in ((1, 4), (2, 16)):
            nb = 16 // dil
            P.op("pool", lambda e, br=br, dil=dil, nb=nb, q=q: e.tensor_copy(
                out=qd[br].ap.rearrange("p (r i j) -> p r i j", r=dil, i=nb, j=128),
                in_=q.ap.rearrange("p (i j r) -> p r i j", i=nb, j=128, r=dil)), reads=[q], writes=[qd[br]])
            P.op("dve", lambda e, br=br, dil=dil, nb=nb, k=k: e.tensor_copy(
                out=kd[br].ap.rearrange("p (r i j) -> p r i j", r=dil, i=nb, j=128),
                in_=k.ap.rearrange("p (i j r) -> p r i j", i=nb, j=128, r=dil)), reads=[k], writes=[kd[br]])
        for br, dil in ((0, 1), (1, 4), (2, 16)):
            nb = 16 // dil
            qq = q if br == 0 else qd[br]
            kk = k if br == 0 else kd[br]
            accv = acc.ap.rearrange("p a (i j r) -> p a r i j", i=nb, j=128, r=dil)
            for r in range(dil):
                for i in range(nb):
                    c0 = (r * nb + i) * 128
                    mt = mbase[br] + i
                    sp = psS.next()
                    if i >= 1:
                        P.op("pe", lambda e, sp=sp, kk=kk, qq=qq, c0=c0: e.matmul(
                            sp.ap[:, 0:128], lhsT=kk.ap[:, c0 - 128:c0], rhs=qq.ap[:, c0:c0 + 128],
                            start=True, stop=True), reads=[kk, qq], writes=[sp])
                    P.op("pe", lambda e, sp=sp, kk=kk, qq=qq, c0=c0: e.matmul(
                        sp.ap[:, 128:256], lhsT=kk.ap[:, c0:c0 + 128], rhs=qq.ap[:, c0:c0 + 128],
                        start=True, stop=True), reads=[kk, qq], writes=[sp])
                    lo = 0 if i >= 1 else 128
                    pT = pTr.next()
                    P.op("act", lambda e, sp=sp, pT=pT, lo=lo: e.activation(
                        out=pT.ap[:, lo:256], in_=sp.ap[:, lo:256], func=AF.Exp, scale=sA), reads=[sp], writes=[pT])
                    P.op("pool", lambda e, pT=pT, lo=lo, mt=mt: e.tensor_tensor(
                        out=pT.ap[:, lo:256], in0=pT.ap[:, lo:256], in1=dmask.ap[:, mt, lo:256], op=ALU.mult),
                        reads=[pT, dmask], writes=[pT])
                    po = psO.next()
                    vi = i * dil + r
                    if i >= 1:
                        P.op("pe", lambda e, po=po, pT=pT, v3=v3, br=br, vi=vi, dil=dil: e.matmul(
                            po.ap[:, 0:128], lhsT=v3.ap[:, br, vi - dil, :], rhs=pT.ap[:, 0:128],
                            start=True, stop=False), reads=[v3, pT], writes=[po])
                    P.op("pe", lambda e, po=po, pT=pT, v3=v3, br=br, vi=vi, i=i: e.matmul(
                        po.ap[:, 0:128], lhsT=v3.ap[:, br, vi, :], rhs=pT.ap[:, 128:256],
                        start=(i == 0), stop=True), reads=[v3, pT], writes=[po])
                    if i >= 1:
                        P.op("pe", lambda e, po=po, pT=pT: e.matmul(
                            po.ap[:, 128:256], lhsT=onesb.ap[:], rhs=pT.ap[:, 0:128],
                            start=True, stop=False), reads=[onesb, pT], writes=[po])
                    P.op("pe", lambda e, po=po, pT=pT, i=i: e.matmul(
                        po.ap[:, 128:256], lhsT=onesb.ap[:], rhs=pT.ap[:, 128:256],
                        start=(i == 0), stop=True), reads=[onesb, pT], writes=[po])
                    pov = po.ap[:, 0:256].rearrange("p (a j) -> p a j", a=2)
                    if br == 0:
                        P.op("dve", lambda e, pov=pov, accv=accv, r=r, i=i: e.tensor_copy(
                            out=accv[:, :, r, i, :], in_=pov), reads=[po], writes=[acc])
                    else:
                        P.op("dve", lambda e, pov=pov, accv=accv, r=r, i=i: e.tensor_tensor(
                            out=accv[:, :, r, i, :], in0=pov, in1=accv[:, :, r, i, :], op=ALU.add),
                            reads=[po, acc], writes=[acc])
        ob = obuf.next()
        P.op("dve", lambda e: e.reciprocal(out=rc.ap[:], in_=acc.ap[:, 1, :]), reads=[acc], writes=[rc])
        P.op("dve", lambda e, ob=ob: e.tensor_tensor(out=ob.ap[:], in0=acc.ap[:, 0, :], in1=rc.ap[:], op=ALU.mult),
             reads=[acc, rc], writes=[ob])
        P.dma("act", oT[h], ob.ap[:], reads=[ob])
    for h in range(NH):
        qn = qrot.next(); qr = q2rot.next(); kn = krot.next(); v3 = vrot.next()
        P.dma("sp", qn.ap[:], qb[h, 0:128, :], writes=[qn])
        P.dma("sp", qr.ap[:], qb[h, 128:256, :], writes=[qr])
        P.dma("sp", kn.ap[:], kbn[h], writes=[kn])
        P.dma("sp", v3.ap[:, 0], vb[:, h, :].rearrange("(i p) d -> p i d", p=128), writes=[v3])
        ob = obuf.next()
        for G in range(4):
            po = psO.next(); pd = psD.next()
            nkb = 4 * G + 4
            for kb in range(nkb):
                sp = psS.next()
                P.op("pe", lambda e, sp=sp, kn=kn, qn=qn, kb=kb, G=G: e.matmul(
                    sp.ap[:, :], lhsT=kn.ap[:, kb * 128:(kb + 1) * 128], rhs=qn.ap[:, G * 512:(G + 1) * 512],
                    start=True, stop=False), reads=[kn, qn], writes=[sp])
                P.op("pe", lambda e, sp=sp, qr=qr, kb=kb, G=G: e.matmul(
                    sp.ap[:, :], lhsT=kr.ap[:, kb * 128:(kb + 1) * 128], rhs=qr.ap[:, G * 512:(G + 1) * 512],
                    start=False, stop=True), reads=[kr, qr], writes=[sp])
                pT = pTr.next()
                bias = visb if (G >= 2 and kb < 8) else zb
                P.op("act", lambda e, sp=sp, pT=pT, bias=bias: e.activation(
                    out=pT.ap[:, :], in_=sp.ap[:, :], func=AF.Exp, bias=bias.ap[:, 0:1], scale=sB),
                    reads=[sp, bias], writes=[pT])
                if kb >= 4 * G:
                    P.op("pool", lambda e, pT=pT, o=kb - 4 * G: e.tensor_tensor(
                        out=pT.ap[:, :], in0=pT.ap[:, :], in1=maskc.ap[:, o, :], op=ALU.mult),
                        reads=[pT, maskc], writes=[pT])
                P.op("pe", lambda e, po=po, pT=pT, v3=v3, kb=kb, nkb=nkb: e.matmul(
                    po.ap[:, :], lhsT=v3.ap[:, 0, kb, :], rhs=pT.ap[:, :], start=(kb == 0), stop=(kb == nkb - 1)),
                    reads=[v3, pT], writes=[po])
                P.op("pe", lambda e, pd=pd, pT=pT, kb=kb, nkb=nkb: e.matmul(
                    pd.ap[:, :], lhsT=onesb.ap[:], rhs=pT.ap[:, :], start=(kb == 0), stop=(kb == nkb - 1)),
                    reads=[onesb, pT], writes=[pd])
            r5 = rc5.next()
            P.op("dve", lambda e, r5=r5, pd=pd: e.reciprocal(out=r5.ap[:], in_=pd.ap[:]), reads=[pd], writes=[r5])
            P.op("dve", lambda e, r5=r5, po=po, ob=ob, G=G: e.tensor_tensor(
                out=ob.ap[:, G * 512:(G + 1) * 512], in0=po.ap[:], in1=r5.ap[:], op=ALU.mult),
                reads=[po, r5], writes=[ob])
        P.dma("act", oT[NH + h], ob.ap[:], reads=[ob])


def body_C(C, layer, xT, oT, w, xo, ntok, extra):
    P = C.P
    C.setup_common()
    tok = C.tok
    gv = load_const(C, "gvec", w["gvec"], [128, 3, KT], F32)
    gx = Buf(gv.ap[:, 0, :], "gx"); gf = Buf(gv.ap[:, 1, :], "gf"); gn = Buf(gv.ap[:, 2, :], "gn")
    for b in (gx, gf, gn):
        b.last_w = gv.last_w
    gxq = load_const(C, "gxq", w["g_xq"], [128, 1], F32)
    memk = load_const(C, "memk", w["memk"].rearrange("(h d) m -> d h m", d=128), [128, 4, 256], BF16)
    memv = load_const(C, "memv", w["memv"].rearrange("(mb mp) h d -> mp mb h d", mp=128), [128, 2, 4, 128], BF16)
    C.pT = C.sb("pT", [128, 2 * tok], BF16)
    acc, acc_all = C.sbn("acc", KT, [128, tok], F32)
    hT, _ = C.sbn("hT", KT, [128, tok], BF16)
    hid, hid_all = C.sbn("hid", KT, [128, tok], BF16)
    kt4, _ = C.sbn("kt4", 4, [128, tok], BF16)
    if layer == 0:
        setup_vt(C, w["identb"])
        qkT, v1 = extra
    else:
        w_r = load_const(C, "w_r", w["w_router"], [128, KT, 8], F32)
        b_r = load_const(C, "b_r", w["b_router"], [8, 1], F32)
        ident = load_const(C, "ident", w["ident"], [128, 128], F32)
        sel = load_const(C, "sel", w["sel"], [8, 8, 128], F32)
        cse, _ = C.sbn("cse", 8, [128, tok], F32)
    for ch in range(ntok // tok):
        t0 = ch * tok
        P.dma("sp", acc_all.ap[:], xT[:, t0:t0 + tok].rearrange("(kc p) t -> p kc t", p=128), writes=acc + [acc_all])
        P.dma("sp", hid_all.ap[:], oT[:, t0:t0 + tok].rearrange("(kc p) t -> p kc t", p=128), writes=hid + [hid_all])
        linear(C, hid, w["w_o"], KT, epi_acc(C, acc))
        cross_attn(C, acc, gx, w["w_xq"], gxq, memk, memv, w["w_xo"], hT, kt4)
        if layer == 0:
            norm_prep(C, acc, gf, hT, C.rstd)
            blocks = [(w["w_gate"], w["w_up"], w["w_down"], 28, b * 3584, b * 3584, C.rstd) for b in range(4)]
            ffn_blocks(C, acc, hT, hid, blocks)
            norm_prep(C, acc, gn, hT, C.rstd)
            e_qk = epi_raw_store(C, C.rstd, qkT, t0, lambda j: j * 128)
            e_v = epi_v_store(C, C.rstd, v1, t0, lambda j: j - 64, C.identb)
            linear(C, hT, w["w_qkv"], 96, lambda j, ps: (e_qk if j < 64 else e_v)(j, ps))
            P.dma("act", xo[:, t0:t0 + tok].rearrange("(kc p) t -> p kc t", p=128), acc_all.ap[:], reads=acc + [acc_all])
        else:
            lg = C.ps_misc.next()
            norm_prep(C, acc, gf, hT, C.rstd, router=(w_r, lg))
            moe_gates(C, lg, C.rstd, b_r, ident, sel, cse)
            blocks = [(w["w_egate"][ex], w["w_eup"][ex], w["w_edown"][ex], KT, 0, 0, cse[ex]) for ex in range(8)]
            ffn_blocks(C, acc, hT, hid, blocks)
            P.dma("act", xo[:, t0:t0 + tok].rearrange("(kc p) t -> p kc t", p=128), acc_all.ap[:],
                  reads=acc + [acc_all], final=True)


def body_B1(C, qkT, v1, tri_d, masks_d, vmask_d, o1T):
    P = C.P
    NH = 32
    qd_ = qkT[0:D, :].rearrange("(h d) t -> h d t", d=128)
    kd_ = qkT[D:2 * D, :].rearrange("(h d) t -> h d t", d=128)
    oT = o1T.rearrange("(h d) t -> h d t", d=128)
    onesb = C.sb("onesb", [128, 128], BF16)
    P.op("pool", lambda e: e.memset(onesb.ap[:], 1.0), writes=[onesb])
    onef = C.sb("onef", [128, 1], F32)
    P.op("pool", lambda e: e.memset(onef.ap[:], 1.0), writes=[onef])
    tri = load_const(C, "tri", tri_d, [128, 128], BF16)
    masks = load_const(C, "masks", masks_d, [128, 4, 512], BF16)
    vmask = load_const(C, "vmask", vmask_d, [128, 512], BF16)
    psS = Rot([C.ps(f"pS{i}") for i in range(2)])
    psL = Rot([C.ps(f"pL{i}") for i in range(2)])
    psC = Rot([C.ps(f"pC{i}") for i in range(2)])
    psO = Rot([C.ps(f"pO{i}") for i in range(2)])
    qrot = Rot([C.sb(f"q{i}", [128, TOKC], BF16) for i in range(2)])
    krot = Rot([C.sb(f"k{i}", [128, SEQ], BF16) for i in range(2)])
    vrot = Rot([C.sb(f"v{i}", [128, 16, 128], BF16) for i in range(2)])
    obuf = Rot([C.sb(f"ob{i}", [128, TOKC], BF16) for i in range(2)])
    ezr = Rot([C.sb(f"ez{i}", [128, 512], F32) for i in range(2)])
    Lr = Rot([C.sb(f"L{i}", [128, 512], F32) for i in range(3)])
    nlr = Rot([C.sb(f"nl{i}", [128, 512], BF16) for i in range(3)])
    tr = Rot([C.sb(f"t{i}", [128, 512], F32) for i in range(3)])
    pTr = Rot([C.sb(f"pT{i}", [128, 512], BF16) for i in range(3)])
    carry = C.sb("carry", [128, 512], F32)
    s = 128 ** -0.5
    for h in range(NH):
        q = qrot.next(); k = krot.next(); v = vrot.next()
        P.dma("sp", q.ap[:], qd_[h][:, TOKC:T2], writes=[q])
        P.dma("sp", k.ap[:], kd_[h], writes=[k])
        P.dma("sp", v.ap[:], v1[:, h, :].rearrange("(i p) d -> p i d", p=128), writes=[v])
        ob = obuf.next()
        for G in (2, 3):
            g0 = (G - 2) * 512
            po = psO.next()
            nkb = 4 * G + 4
            for idx, kb in enumerate(reversed(range(nkb))):
                diag = kb >= 4 * G
                o = kb - 4 * G
                msk = (lambda o=o: masks.ap[:, o, :]) if diag else ((lambda: vmask.ap[:, :]) if kb < 8 else None)
                mbuf = masks if diag else vmask
                sp = psS.next()
                P.op("pe", lambda e, sp=sp, k=k, q=q, kb=kb, g0=g0: e.matmul(
                    sp.ap[:, :], lhsT=k.ap[:, kb * 128:(kb + 1) * 128], rhs=q.ap[:, g0:g0 + 512],
                    start=True, stop=True), reads=[k, q], writes=[sp])
                ez = ezr.next(); L = Lr.next(); nl = nlr.next(); t = tr.next(); pT = pTr.next()
                P.op("act", lambda e, sp=sp, ez=ez: e.activation(out=ez.ap[:], in_=sp.ap[:], func=AF.Exp, scale=-s),
                     reads=[sp], writes=[ez])
                P.op("act", lambda e, ez=ez, L=L: e.activation(out=L.ap[:], in_=ez.ap[:], func=AF.Ln,
                                                               bias=onef.ap[:, 0:1], scale=1.0),
                     reads=[ez, onef], writes=[L])
                P.op("dve", lambda e, sp=sp, L=L, nl=nl: e.scalar_tensor_tensor(
                    out=nl.ap[:], in0=sp.ap[:], scalar=s, in1=L.ap[:], op0=ALU.mult, op1=ALU.add),
                    reads=[sp, L], writes=[nl])
                if msk is not None:
                    P.op("pool", lambda e, nl=nl, msk=msk: e.tensor_tensor(out=nl.ap[:], in0=nl.ap[:], in1=msk(),
                                                                          op=ALU.mult), reads=[nl, mbuf], writes=[nl])
                pl = psL.next(); pc = psC.next()
                P.op("pe", lambda e, pl=pl, nl=nl: e.matmul(pl.ap[:], lhsT=tri.ap[:], rhs=nl.ap[:],
                                                            start=True, stop=True), reads=[tri, nl], writes=[pl])
                P.op("pe", lambda e, pc=pc, nl=nl: e.matmul(pc.ap[:], lhsT=onesb.ap[:], rhs=nl.ap[:],
                                                            start=True, stop=True), reads=[onesb, nl], writes=[pc])
                P.op("dve", lambda e, pl=pl, L=L, t=t: e.tensor_tensor(out=t.ap[:], in0=pl.ap[:], in1=L.ap[:],
                                                                       op=ALU.add), reads=[pl, L], writes=[t])
                if idx > 0:
                    P.op("pool", lambda e, t=t: e.tensor_tensor(out=t.ap[:], in0=t.ap[:], in1=carry.ap[:],
                                                                op=ALU.add), reads=[t, carry], writes=[t])
                P.op("act", lambda e, t=t, pT=pT: e.activation(out=pT.ap[:], in_=t.ap[:], func=AF.Exp, scale=-1.0),
                     reads=[t], writes=[pT])
                if msk is not None:
                    P.op("pool", lambda e, pT=pT, msk=msk: e.tensor_tensor(out=pT.ap[:], in0=pT.ap[:], in1=msk(),
                                                                          op=ALU.mult), reads=[pT, mbuf], writes=[pT])
                P.op("pe", lambda e, po=po, pT=pT, v=v, kb=kb, idx=idx, nkb=nkb: e.matmul(
                    po.ap[:], lhsT=v.ap[:, kb, :], rhs=pT.ap[:], start=(idx == 0), stop=(idx == nkb - 1)),
                    reads=[v, pT], writes=[po])
                if idx < nkb - 1:
                    if idx == 0:
                        P.op("dve", lambda e, pc=pc: e.tensor_copy(out=carry.ap[:], in_=pc.ap[:]),
                             reads=[pc], writes=[carry])
                    else:
                        P.op("dve", lambda e, pc=pc: e.tensor_tensor(out=carry.ap[:], in0=pc.ap[:], in1=carry.ap[:],
                                                                     op=ALU.add), reads=[pc, carry], writes=[carry])
            P.op("dve", lambda e, po=po, ob=ob, g0=g0: e.tensor_copy(out=ob.ap[:, g0:g0 + 512], in_=po.ap[:]),
                 reads=[po], writes=[ob])
        P.dma("act", oT[h], ob.ap[:], reads=[ob])


def build_fused():
    C = Ctx(TOK)
    nc = C.nc
    P = C.P
    i_ = C.dram_in

    def scr(name, shape, dt):
        ap = nc.dram_tensor(name, list(shape), dt, kind="Internal").ap()
        return ap, Buf(ap, name)
    xT = i_("xT", [D, T2], F32)
    posb = i_("posb", [128, T2], I32)
    memT = i_("memT", [D, 256], F32)
    g_mem = i_("g_mem", [128, KT], F32)
    g_mk = i_("g_mk", [128, 1], F32)
    w_mem_kv = i_("w_mem_kv", [D, 1024], F32)
    w_in = i_("w_in", [D, 8320], F32)
    w_uq = i_("w_uq", [1536, 4096], F32)
    w_ukv = i_("w_ukv", [512, 4096], F32)
    gmix = i_("g_mix0", [128, KT], F32)
    gcq = i_("g_cq", [128, 12], F32)
    gckv = i_("g_ckv", [128, 4], F32)
    vecs = i_("vecs", [128, 8], F32)
    rots = i_("rots", [128, 2, 128], F32)
    identb = i_("identb", [128, 128], BF16)
    ident = i_("ident", [128, 128], F32)
    sel = i_("sel", [8, 8, 128], F32)
    dmask = i_("dmask", [128, 21, 256], BF16)
    maskc = i_("maskc", [128, 4, 512], BF16)
    visb = i_("visb", [128, 1], F32)
    tri = i_("tri", [128, 128], BF16)
    masks = i_("masks", [128, 4, 512], BF16)
    vmask = i_("vmask", [128, 512], BF16)
    wl = []
    for L in range(2):
        wl.append({"gvec": i_(f"gvec{L}", [128, 3, KT], F32), "g_xq": i_(f"g_xq{L}", [128, 1], F32),
                   "w_xq": i_(f"w_xq{L}", [D, 512], F32), "w_xo": i_(f"w_xo{L}", [512, D], F32),
                   "w_o": i_(f"w_o{L}", [D, D], F32), "identb": identb, "ident": ident, "sel": sel})
    wl[0].update({"w_gate": i_("w_gate", [D, 14336], F32), "w_up": i_("w_up", [D, 14336], F32),
                  "w_down": i_("w_down", [14336, D], F32), "w_qkv": i_("w_qkv", [D, 3 * D], F32)})
    wl[1].update({"w_egate": i_("w_egate", [8, D, D], F32), "w_eup": i_("w_eup", [8, D, D], F32),
                  "w_edown": i_("w_edown", [8, D, D], F32), "w_router": i_("w_router", [128, KT, 8], F32),
                  "b_router": i_("b_router", [8, 1], F32)})
    out = C.dram_out("xoT", [D, TOKC], F32)
    tabs = [scr(f"tab{i}", [128, T2], F32) for i in range(4)]
    mkT = scr("mkT", [512, 256], BF16)
    mv = scr("mv", [256, 4, 128], BF16)
    qaT = scr("qaT", [2048, T2], BF16); kaT = scr("kaT", [2048, T2], BF16); va = scr("va", [T2, 16, 128], BF16)
    qbT = scr("qbT", [4096, T2], BF16); kbnT = scr("kbnT", [2048, T2], BF16); vb = scr("vb", [T2, 16, 128], BF16)
    krT = scr("krT", [128, T2], BF16)
    o0T = scr("o0T", [D, T2], BF16)
    x3T = scr("x3T", [D, T2], F32)
    qkT = scr("qkT", [2 * D, T2], BF16)
    v1 = scr("v1", [T2, 32, 128], BF16)
    o1T = scr("o1T", [D, TOKC], BF16)
    for L in range(2):
        wl[L]["memk"] = mkT[0]
        wl[L]["memv"] = mv[0]

    def phase(tok, fn):
        with contextlib.ExitStack() as es:
            C.es = es
            C.tok = tok
            C.n += 1
            if hasattr(C, "_mg"):
                del C._mg
            fn()
        P.fence()
    phase(TOK, lambda: body_rope(C, posb, vecs, tabs))
    phase(128, lambda: body_P2(C, memT, g_mem, g_mk, w_mem_kv, identb, mkT, mv))
    phase(TOK, lambda: body_A0(C, xT, tabs, w_in, w_uq, w_ukv, gmix, gcq, gckv, vecs, rots, identb,
                               (qaT, kaT, va, qbT, kbnT, vb, krT)))
    phase(512, lambda: body_B0(C, qaT[0], kaT[0], va[0], qbT[0], kbnT[0], vb[0], krT[0], dmask, maskc, visb, o0T[0]))
    phase(TOK, lambda: body_C(C, 0, xT, o0T[0], wl[0], x3T[0], T2, (qkT[0], v1[0])))
    phase(512, lambda: body_B1(C, qkT[0], v1[0], tri, masks, vmask, o1T[0]))
    phase(TOK, lambda: body_C(C, 1, x3T[0][:, TOKC:T2], o1T[0], wl[1], out, TOKC, None))
    P.emit()
    return nc


def dil_mask_table(vis):
    kk = np.arange(128)[:, None]
    qq = np.arange(128)[None, :]
    tiles = []
    for dil in (1, 4, 16):
        nb = 16 // dil
        hd = 1024 // dil
        for i in range(nb):
            nq = 128 * i + qq
            out = []
            for which in (0, 1):
                nk = 128 * (i - 1 + which) + kk
                band = (kk >= qq) if which == 0 else (kk <= qq)
                visible = (nq < hd) | (nk >= hd) | bool(vis)
                out.append(band & visible & (nk >= 0))
            tiles.append(np.concatenate(out, axis=1))
    return np.ascontiguousarray(np.stack(tiles, axis=1).astype(NPBF))


def prep_fused(inp):
    x = inp["x"]
    pos = inp["positions"].astype(np.int32)
    mem = inp["mem"]
    w_in = np.concatenate([inp["w_in"][0], np.zeros((D, 64), np.float32)], axis=1)
    wq = inp["w_uq"][0].reshape(1536, 16, 192)
    w_uq = np.concatenate([wq, np.zeros((1536, 16, 64), np.float32)], axis=2).reshape(1536, 4096)
    invA, invB, rots = rope_consts()
    gbq = inp["gb_q"][0]
    vecs = np.stack([inp["ga_q"][0], inp["ga_k"][0], gbq[:128], pad128(gbq[128:]), inp["gb_kn"][0],
                     pad128(inp["gb_kr"][0]), invA, invB], axis=1).astype(np.float32)
    _, maskc, masks, tri = attn_masks()
    sel = np.zeros((8, 8, 128), np.float32)
    for e in range(8):
        sel[e, e, :] = 1.0
    shared = {"g_mem": cols(inp["g_mem"], 32), "g_mk": np.ascontiguousarray(inp["g_mem_k"].reshape(128, 1)),
              "w_mem_kv": inp["w_mem_kv"], "w_in": w_in, "w_uq": w_uq, "w_ukv": np.ascontiguousarray(inp["w_ukv"][0]),
              "g_mix0": cols(inp["g_mix"][0], 32), "g_cq": cols(inp["g_cq"][0], 12), "g_ckv": cols(inp["g_ckv"][0], 4),
              "vecs": np.ascontiguousarray(vecs), "rots": rots, "identb": np.eye(128, dtype=np.float32).astype(NPBF),
              "ident": np.eye(128, dtype=np.float32), "sel": sel, "maskc": maskc, "tri": tri, "masks": masks,
              "w_gate": inp["w_gate"][0], "w_up": inp["w_up"][0], "w_down": inp["w_down"][0], "w_qkv": inp["w_qkv"][0],
              "w_egate": inp["w_egate"][0], "w_eup": inp["w_eup"][0], "w_edown": inp["w_edown"][0],
              "w_router": np.ascontiguousarray(inp["w_router"][0].reshape(32, 128, 8).transpose(1, 0, 2)),
              "b_router": np.ascontiguousarray(inp["b_router"][0].reshape(8, 1))}
    for L in range(2):
        shared[f"gvec{L}"] = np.ascontiguousarray(np.stack(
            [cols(inp["g_x"][L], 32), cols(inp["g_ffn"][L], 32), cols(inp["g_mix"][1], 32)], axis=1))
        shared[f"g_xq{L}"] = np.ascontiguousarray(inp["g_xq"][L].reshape(128, 1))
        shared[f"w_xq{L}"] = np.ascontiguousarray(inp["w_xq"][L])
        shared[f"w_xo{L}"] = np.ascontiguousarray(inp["w_xo"][L])
    shared["w_o0"] = inp["w_o_even"][0]
    shared["w_o1"] = inp["w_o_odd"][0]
    dm = [dil_mask_table(0), dil_mask_table(1)]
    ins = []
    for c in range(NCORES):
        b, j = divmod(c, 2)
        own = slice(j * TOKC, (j + 1) * TOKC)
        oth = slice((1 - j) * TOKC, (2 - j) * TOKC)
        m = dict(shared)
        xb = x[b]
        m["xT"] = np.ascontiguousarray(np.concatenate([xb[oth], xb[own]], axis=0).T)
        pb = np.concatenate([pos[b, oth], pos[b, own]])
        m["posb"] = np.ascontiguousarray(np.broadcast_to(pb[None, :], (128, T2)))
        m["memT"] = np.ascontiguousarray(mem[b].T)
        m["dmask"] = dm[j]
        m["visb"] = np.full((128, 1), 0.0 if j == 1 else -30000.0, np.float32)
        m["vmask"] = (np.ones((128, 512), np.float32) if j == 1 else np.zeros((128, 512), np.float32)).astype(NPBF)
        ins.append(m)
    return ins


def kernel_unfused(**inp):
    return _kernel_unfused(inp)


def kernel(**inp):
    inp = {k: np.asarray(v) for k, v in inp.items()}
    res = run(build_fused(), prep_fused(inp))
    out = np.zeros((4, SEQ, D), np.float32)
    for c, r in enumerate(res):
        b, j = divmod(c, 2)
        out[b, j * TOKC:(j + 1) * TOKC, :] = np.asarray(r["xoT"]).T
    return out
```
